# Optimizing a Trainium2 kernel written in Bass

```python
import jax, jax.numpy as jnp
from jax import lax
import numpy as np

D_MODEL = 4096
BATCH = 2
SEQ = 4096
DEPTH = 4

D_MIX = D_MODEL
N_MIXERS = 4
D_GROUP = D_MIX // N_MIXERS
HEAD_DIM = 128
ATT_HEADS = D_GROUP // HEAD_DIM
IDX_HEADS = 16
IDX_DIM = 64
TOPK_MAX = 256
Q_BLOCK = 128
ROPE_THETA = 10000.0
GDN_DK = 128
GDN_DV = 128
GDN_HEADS = D_GROUP // GDN_DV
GLA_DK = 64
GLA_DV = 128
GLA_HEADS = D_GROUP // GLA_DV
GLA_RANK = 16
GLA_GATE_NORM = 16.0
SSD_HEADDIM = 64
SSD_HEADS = D_GROUP // SSD_HEADDIM
SSD_STATE = 128
SSD_GROUPS = 2
SSD_XBC = D_GROUP + 2 * SSD_GROUPS * SSD_STATE
CONV_WIDTH = 4
CHUNK = 64
N_EXPERT_GROUPS = 4
EXPERTS_PER_GROUP = 8
N_EXPERTS = N_EXPERT_GROUPS * EXPERTS_PER_GROUP
TOPK_IN_GROUP = 2
EXPERT_FF = 256
ADA_RANK = 256
ALPHA = (2.0 * DEPTH) ** 0.25
BETA_INIT = (8.0 * DEPTH) ** -0.25
EPS = 1e-6

PROJ_WIDTHS = (
    D_GROUP, D_GROUP, D_GROUP, IDX_HEADS * IDX_DIM, IDX_DIM, IDX_HEADS,
    GDN_HEADS * GDN_DK, GDN_HEADS * GDN_DK, GDN_HEADS * GDN_DV, GDN_HEADS, GDN_HEADS, GDN_HEADS * GDN_DV,
    GLA_HEADS * GLA_DK, GLA_HEADS * GLA_DK, GLA_HEADS * GLA_DV, GLA_RANK, GLA_HEADS * GLA_DV,
    D_GROUP, D_GROUP, SSD_GROUPS * SSD_STATE, SSD_GROUPS * SSD_STATE, SSD_HEADS,
)
D_PROJ = sum(PROJ_WIDTHS)
PROJ_OFFSETS = tuple(sum(PROJ_WIDTHS[:i + 1]) for i in range(len(PROJ_WIDTHS) - 1))

kernel_name = "hybrid_headgroup_dsa_gdn_gla_ssd_hmoe"


def layer_norm(x, g, b):
    xf = x.astype(jnp.float32)
    mu = jnp.mean(xf, -1, keepdims=True)
    var = jnp.mean(jnp.square(xf - mu), -1, keepdims=True)
    return ((xf - mu) * lax.rsqrt(var + EPS) * g + b).astype(x.dtype)


def rms_norm(x, w):
    xf = x.astype(jnp.float32)
    return (xf * lax.rsqrt(jnp.mean(xf * xf, -1, keepdims=True) + EPS) * w).astype(x.dtype)


def l2_norm(x):
    xf = x.astype(jnp.float32)
    return (xf * lax.rsqrt(jnp.sum(xf * xf, -1, keepdims=True) + EPS)).astype(x.dtype)


def rope_tables(positions, dim):
    inv = 1.0 / (ROPE_THETA ** (jnp.arange(0, dim, 2, dtype=jnp.float32) / dim))
    ang = positions.astype(jnp.float32)[..., None] * inv
    return jnp.cos(ang), jnp.sin(ang)


def apply_rope(x, cos, sin):
    x1, x2 = jnp.split(x, 2, axis=-1)
    c = cos[:, :, None, :].astype(x.dtype)
    s = sin[:, :, None, :].astype(x.dtype)
    return jnp.concatenate([x1 * c - x2 * s, x2 * c + x1 * s], axis=-1)


def causal_conv(x, w, b=None):
    K = w.shape[0]
    L = x.shape[1]
    xp = jnp.pad(x, ((0, 0), (K - 1, 0), (0, 0)))
    y = xp[:, 0:L] * w[0]
    for i in range(1, K):
        y = y + xp[:, i:i + L] * w[i]
    if b is not None:
        y = y + b
    return y


def to_chunks(t):
    B, L, H = t.shape[:3]
    t = t.astype(jnp.float32).reshape(B, L // CHUNK, CHUNK, H, *t.shape[3:])
    return jnp.moveaxis(t, 3, 1)


def from_chunks(o):
    n, B, H, C, d = o.shape
    return jnp.transpose(o, (1, 0, 3, 2, 4)).reshape(B, n * C, H, d)


def seg_decay(gc):
    causal = jnp.tril(jnp.ones((CHUNK, CHUNK), bool))
    seg = gc[..., :, None] - gc[..., None, :]
    return jnp.where(causal, jnp.exp(jnp.where(causal, seg, 0.0)), 0.0)


def chunk_gated_delta(q, k, v, beta, g):
    out_dtype = v.dtype
    q, k, v = to_chunks(q), to_chunks(k), to_chunks(v)
    beta, g = to_chunks(beta), to_chunks(g)
    dv = v.shape[-1]
    gc = jnp.cumsum(g, axis=-1)
    decay = seg_decay(gc)
    strict = jnp.tril(jnp.ones((CHUNK, CHUNK), bool), -1)
    kb = k * beta[..., None]
    a_mat = jnp.where(strict, jnp.einsum('bhnid,bhnjd->bhnij', kb, k) * decay, 0.0)
    rhs = jnp.concatenate([v * beta[..., None], kb * jnp.exp(gc)[..., None]], axis=-1)
    sol = lax.linalg.triangular_solve(a_mat + jnp.eye(CHUNK, dtype=jnp.float32), rhs,
                                      left_side=True, lower=True, unit_diagonal=True)
    u0, w = sol[..., :dv], sol[..., dv:]
    qk = jnp.einsum('bhnid,bhnjd->bhnij', q, k) * decay
    qg = q * jnp.exp(gc)[..., None]
    kd = k * jnp.exp(gc[..., -1:] - gc)[..., None]
    glast = jnp.exp(gc[..., -1])

    def step(S, inp):
        qg_n, kd_n, u0_n, w_n, qk_n, gl_n = inp
        u = u0_n - jnp.einsum('bhck,bhkv->bhcv', w_n, S)
        o = jnp.einsum('bhck,bhkv->bhcv', qg_n, S) + jnp.einsum('bhij,bhjv->bhiv', qk_n, u)
        S = S * gl_n[..., None, None] + jnp.einsum('bhck,bhcv->bhkv', kd_n, u)
        return S, o

    B, H = q.shape[:2]
    S0 = jnp.zeros((B, H, q.shape[-1], dv), jnp.float32)
    xs = tuple(jnp.moveaxis(t, 2, 0) for t in (qg, kd, u0, w, qk, glast))
    _, o = lax.scan(step, S0, xs)
    return from_chunks(o).astype(out_dtype)


def chunk_gla(q, k, v, gk):
    out_dtype = v.dtype
    q, k, v, gk = to_chunks(q), to_chunks(k), to_chunks(v), to_chunks(gk)
    b = jnp.cumsum(gk, axis=-2)
    qe = q * jnp.exp(b)
    ke = k * jnp.exp(-b)
    causal = jnp.tril(jnp.ones((CHUNK, CHUNK), bool))
    attn = jnp.where(causal, jnp.einsum('bhnid,bhnjd->bhnij', qe, ke), 0.0)
    o_intra = jnp.einsum('bhnij,bhnjv->bhniv', attn, v)
    blast = b[..., -1, :]
    kd = k * jnp.exp(blast[..., None, :] - b)

    def step(S, inp):
        qe_n, kd_n, v_n, oi_n, bl_n = inp
        o = jnp.einsum('bhck,bhkv->bhcv', qe_n, S) + oi_n
        S = S * jnp.exp(bl_n)[..., None] + jnp.einsum('bhck,bhcv->bhkv', kd_n, v_n)
        return S, o

    B, H = q.shape[:2]
    S0 = jnp.zeros((B, H, q.shape[-1], v.shape[-1]), jnp.float32)
    xs = tuple(jnp.moveaxis(t, 2, 0) for t in (qe, kd, v, o_intra, blast))
    _, o = lax.scan(step, S0, xs)
    return from_chunks(o).astype(out_dtype)


def chunk_ssd(cq, bk, xv, g):
    out_dtype = xv.dtype
    cq, bk, xv, g = to_chunks(cq), to_chunks(bk), to_chunks(xv), to_chunks(g)
    gc = jnp.cumsum(g, axis=-1)
    scores = jnp.einsum('bhnid,bhnjd->bhnij', cq, bk) * seg_decay(gc)
    o_intra = jnp.einsum('bhnij,bhnjp->bhnip', scores, xv)
    cg = cq * jnp.exp(gc)[..., None]
    bd = bk * jnp.exp(gc[..., -1:] - gc)[..., None]
    glast = jnp.exp(gc[..., -1])

    def step(S, inp):
        cg_n, bd_n, x_n, oi_n, gl_n = inp
        o = jnp.einsum('bhcn,bhnp->bhcp', cg_n, S) + oi_n
        S = S * gl_n[..., None, None] + jnp.einsum('bhcn,bhcp->bhnp', bd_n, x_n)
        return S, o

    B, H = cq.shape[:2]
    S0 = jnp.zeros((B, H, cq.shape[-1], xv.shape[-1]), jnp.float32)
    xs = tuple(jnp.moveaxis(t, 2, 0) for t in (cg, bd, xv, o_intra, glast))
    _, o = lax.scan(step, S0, xs)
    return from_chunks(o).astype(out_dtype)


def dsa_attention(q, k, v, q_idx, k_idx, w_idx):
    B, L, H, Dh = q.shape
    k_sel = min(TOPK_MAX, L // 4)
    nb = L // Q_BLOCK
    key_pos = jnp.arange(L)

    def to_blocks(t):
        return jnp.swapaxes(t.reshape(B, nb, Q_BLOCK, *t.shape[2:]), 0, 1)

    def block(args):
        qb, qib, wb, t0 = args
        qpos = t0 + jnp.arange(Q_BLOCK)
        admissible = key_pos[None, :] <= qpos[:, None]
        logits = jnp.einsum('bqhd,bsd->bqhs', qib, k_idx)
        score = jnp.einsum('bqhs,bqh->bqs', jax.nn.relu(logits), wb).astype(jnp.float32)
        score = jnp.where(admissible[None], score, -jnp.inf)
        _, sel = lax.top_k(score, k_sel)
        k_g = jax.vmap(lambda kk, ii: kk[ii])(k, sel)
        v_g = jax.vmap(lambda vv, ii: vv[ii])(v, sel)
        s = jnp.einsum('bqhd,bqkhd->bhqk', qb, k_g).astype(jnp.float32) * (Dh ** -0.5)
        valid = sel <= qpos[None, :, None]
        s = jnp.where(valid[:, None], s, -jnp.inf)
        p = jax.nn.softmax(s, axis=-1).astype(v.dtype)
        return jnp.einsum('bhqk,bqkhd->bqhd', p, v_g)

    out = lax.map(block, (to_blocks(q), to_blocks(q_idx), to_blocks(w_idx),
                          jnp.arange(nb) * Q_BLOCK))
    return jnp.swapaxes(out, 0, 1).reshape(B, L, H * Dh)


def hybrid_mixer(h, cos_a, sin_a, cos_i, sin_i, w_in, w_out, idx_kn_g, idx_kn_b,
                 gdn_conv_w, gdn_a_log, gdn_dt_bias, gdn_norm_w,
                 gla_w_up, gla_b_up, gla_norm_w,
                 ssd_conv_w, ssd_conv_b, ssd_a_log, ssd_dt_bias, ssd_d, ssd_norm_w):
    B, L, _ = h.shape
    proj = jnp.einsum('bld,dp->blp', h, w_in)
    (a_q, a_k, a_v, a_qi, a_ki, a_wi,
     b_q, b_k, b_v, b_beta, b_a, b_z,
     c_q, c_k, c_v, c_gk, c_g,
     d_z, d_x, d_b, d_c, d_dt) = jnp.split(proj, PROJ_OFFSETS, axis=-1)

    q = apply_rope(a_q.reshape(B, L, ATT_HEADS, HEAD_DIM), cos_a, sin_a)
    k = apply_rope(a_k.reshape(B, L, ATT_HEADS, HEAD_DIM), cos_a, sin_a)
    v = a_v.reshape(B, L, ATT_HEADS, HEAD_DIM)
    qi = apply_rope(a_qi.reshape(B, L, IDX_HEADS, IDX_DIM), cos_i, sin_i)
    ki = apply_rope(layer_norm(a_ki, idx_kn_g, idx_kn_b)[:, :, None, :], cos_i, sin_i)[:, :, 0]
    wi = a_wi * (IDX_HEADS ** -0.5 * IDX_DIM ** -0.5)
    out_a = dsa_attention(q, k, v, qi, ki, wi)

    qkv = jax.nn.silu(causal_conv(jnp.concatenate([b_q, b_k, b_v], -1), gdn_conv_w))
    gq, gk_, gv = jnp.split(qkv, [GDN_HEADS * GDN_DK, 2 * GDN_HEADS * GDN_DK], axis=-1)
    gq = l2_norm(gq.reshape(B, L, GDN_HEADS, GDN_DK)) * (GDN_DK ** -0.5)
    gk_ = l2_norm(gk_.reshape(B, L, GDN_HEADS, GDN_DK))
    gv = gv.reshape(B, L, GDN_HEADS, GDN_DV)
    beta = jax.nn.sigmoid(b_beta)
    g_dec = -jnp.exp(gdn_a_log) * jax.nn.softplus(b_a + gdn_dt_bias)
    o_b = chunk_gated_delta(gq, gk_, gv, beta, g_dec)
    o_b = rms_norm(o_b, gdn_norm_w) * jax.nn.silu(b_z.reshape(B, L, GDN_HEADS, GDN_DV))
    out_b = o_b.reshape(B, L, D_GROUP)

    gk_log = jax.nn.log_sigmoid(jnp.einsum('blr,rk->blk', c_gk, gla_w_up) + gla_b_up) / GLA_GATE_NORM
    o_c = chunk_gla(c_q.reshape(B, L, GLA_HEADS, GLA_DK) * (GLA_DK ** -0.5),
                    c_k.reshape(B, L, GLA_HEADS, GLA_DK),
                    c_v.reshape(B, L, GLA_HEADS, GLA_DV),
                    gk_log.reshape(B, L, GLA_HEADS, GLA_DK))
    o_c = rms_norm(o_c, gla_norm_w) * jax.nn.silu(c_g.reshape(B, L, GLA_HEADS, GLA_DV))
    out_c = o_c.reshape(B, L, D_GROUP)

    xbc = jax.nn.silu(causal_conv(jnp.concatenate([d_x, d_b, d_c], -1), ssd_conv_w, ssd_conv_b))
    sx, sb, sc = jnp.split(xbc, [D_GROUP, D_GROUP + SSD_GROUPS * SSD_STATE], axis=-1)
    xh = sx.reshape(B, L, SSD_HEADS, SSD_HEADDIM)
    heads_per_group = SSD_HEADS // SSD_GROUPS
    bh = jnp.repeat(sb.reshape(B, L, SSD_GROUPS, SSD_STATE), heads_per_group, axis=2)
    ch = jnp.repeat(sc.reshape(B, L, SSD_GROUPS, SSD_STATE), heads_per_group, axis=2)
    dt = jax.nn.softplus(d_dt + ssd_dt_bias)
    a_dec = -jnp.exp(ssd_a_log)
    y = chunk_ssd(ch, bh, xh * dt[..., None], dt * a_dec)
    y = (y + xh * ssd_d[:, None]).reshape(B, L, D_GROUP) * jax.nn.silu(d_z)
    y = rms_norm(y.reshape(B, L, SSD_GROUPS, D_GROUP // SSD_GROUPS),
                 ssd_norm_w.reshape(SSD_GROUPS, D_GROUP // SSD_GROUPS))
    out_d = y.reshape(B, L, D_GROUP)

    mixed = jnp.concatenate([out_a, out_b, out_c, out_d], axis=-1)
    return jnp.einsum('blm,md->bld', mixed, w_out)


def hier_moe(h, router_g_w, router_g_b, router_e_w, router_e_b, w_gate, w_up, w_down):
    B, L, D = h.shape
    t = h.reshape(B * L, D)
    n = t.shape[0]
    g_logits = (t @ router_g_w + router_g_b).astype(jnp.float32)
    g_prob = jax.nn.softmax(g_logits, axis=-1)
    g_sel = jnp.argmax(g_logits, axis=-1)
    p_g = jnp.take_along_axis(g_prob, g_sel[:, None], axis=-1)
    e_logits = (t @ router_e_w + router_e_b).astype(jnp.float32).reshape(n, N_EXPERT_GROUPS, EXPERTS_PER_GROUP)
    e_in = jnp.take_along_axis(e_logits, g_sel[:, None, None], axis=1)[:, 0]
    top_v, top_i = lax.top_k(e_in, TOPK_IN_GROUP)
    top_w = jax.nn.softmax(top_v, axis=-1) * p_g
    expert_id = g_sel[:, None] * EXPERTS_PER_GROUP + top_i
    gates = jnp.sum(jax.nn.one_hot(expert_id, N_EXPERTS, dtype=jnp.float32) * top_w[..., None], axis=1)
    hg = jnp.einsum('nd,edf->nef', t, w_gate)
    hu = jnp.einsum('nd,edf->nef', t, w_up)
    act = jax.nn.silu(hg) * hu * gates[..., None].astype(t.dtype)
    return jnp.einsum('nef,efd->nd', act, w_down).reshape(B, L, D)


def setup_inputs(seed: int = 0) -> dict:
    key = jax.random.key(seed)
    ks = iter(jax.random.split(key, 48))
    f32 = jnp.float32

    def nrm(shape, std):
        return jax.random.normal(next(ks), shape, f32) * std

    def unif(shape, lo, hi):
        return jax.random.uniform(next(ks), shape, f32, lo, hi)

    def dt_bias_init(shape):
        dt = jnp.exp(unif(shape, float(np.log(1e-3)), float(np.log(1e-1))))
        return dt + jnp.log(-jnp.expm1(-dt))

    Ld = DEPTH
    x = nrm((BATCH, SEQ, D_MODEL), 1.0)
    c = nrm((BATCH, D_MODEL), 1.0)
    positions = (jax.random.randint(next(ks), (BATCH, 1), 0, 1024, jnp.int32)
                 + jnp.arange(SEQ, dtype=jnp.int32)[None, :])
    return {
        "x": x,
        "c": c,
        "positions": positions,
        "ada_down": nrm((Ld, D_MODEL, ADA_RANK), D_MODEL ** -0.5),
        "ada_up": nrm((Ld, ADA_RANK, 6 * D_MODEL), 0.1 * ADA_RANK ** -0.5),
        "ada_bias": nrm((Ld, 6 * D_MODEL), 0.01),
        "w_in": nrm((Ld, D_MODEL, D_PROJ), D_MODEL ** -0.5),
        "w_out": nrm((Ld, D_MIX, D_MODEL), BETA_INIT * (2.0 / (D_MIX + D_MODEL)) ** 0.5),
        "idx_kn_g": 1.0 + nrm((Ld, IDX_DIM), 0.01),
        "idx_kn_b": nrm((Ld, IDX_DIM), 0.01),
        "gdn_conv_w": nrm((Ld, CONV_WIDTH, 3 * D_GROUP), CONV_WIDTH ** -0.5),
        "gdn_a_log": jnp.log(unif((Ld, GDN_HEADS), 1.0, 16.0)),
        "gdn_dt_bias": dt_bias_init((Ld, GDN_HEADS)),
        "gdn_norm_w": 1.0 + nrm((Ld, GDN_DV), 0.01),
        "gla_w_up": nrm((Ld, GLA_RANK, GLA_HEADS * GLA_DK), GLA_RANK ** -0.5),
        "gla_b_up": nrm((Ld, GLA_HEADS * GLA_DK), 0.1),
        "gla_norm_w": 1.0 + nrm((Ld, GLA_DV), 0.01),
        "ssd_conv_w": nrm((Ld, CONV_WIDTH, SSD_XBC), CONV_WIDTH ** -0.5),
        "ssd_conv_b": nrm((Ld, SSD_XBC), 0.01),
        "ssd_a_log": jnp.log(unif((Ld, SSD_HEADS), 1.0, 16.0)),
        "ssd_dt_bias": dt_bias_init((Ld, SSD_HEADS)),
        "ssd_d": 1.0 + nrm((Ld, SSD_HEADS), 0.01),
        "ssd_norm_w": 1.0 + nrm((Ld, D_GROUP), 0.01),
        "ln1_g": 1.0 + nrm((Ld, D_MODEL), 0.01),
        "ln1_b": nrm((Ld, D_MODEL), 0.01),
        "router_g_w": nrm((Ld, D_MODEL, N_EXPERT_GROUPS), D_MODEL ** -0.5),
        "router_g_b": nrm((Ld, N_EXPERT_GROUPS), 0.01),
        "router_e_w": nrm((Ld, D_MODEL, N_EXPERTS), D_MODEL ** -0.5),
        "router_e_b": nrm((Ld, N_EXPERTS), 0.01),
        "exp_w_gate": nrm((Ld, N_EXPERTS, D_MODEL, EXPERT_FF), D_MODEL ** -0.5),
        "exp_w_up": nrm((Ld, N_EXPERTS, D_MODEL, EXPERT_FF), D_MODEL ** -0.5),
        "exp_w_down": nrm((Ld, N_EXPERTS, EXPERT_FF, D_MODEL), BETA_INIT * (2.0 / (EXPERT_FF + D_MODEL)) ** 0.5),
        "ln2_g": 1.0 + nrm((Ld, D_MODEL), 0.01),
        "ln2_b": nrm((Ld, D_MODEL), 0.01),
    }


def reference(x, c, positions, ada_down, ada_up, ada_bias, w_in, w_out, idx_kn_g, idx_kn_b,
              gdn_conv_w, gdn_a_log, gdn_dt_bias, gdn_norm_w, gla_w_up, gla_b_up, gla_norm_w,
              ssd_conv_w, ssd_conv_b, ssd_a_log, ssd_dt_bias, ssd_d, ssd_norm_w,
              ln1_g, ln1_b, router_g_w, router_g_b, router_e_w, router_e_b,
              exp_w_gate, exp_w_up, exp_w_down, ln2_g, ln2_b):
    cos_a, sin_a = rope_tables(positions, HEAD_DIM)
    cos_i, sin_i = rope_tables(positions, IDX_DIM)
    c_act = jax.nn.silu(c)
    for l in range(DEPTH):
        mod = (c_act @ ada_down[l]) @ ada_up[l] + ada_bias[l]
        shift1, scale1, gate1, shift2, scale2, gate2 = jnp.split(mod[:, None, :], 6, axis=-1)
        h = x * (1.0 + scale1) + shift1
        mix = hybrid_mixer(h, cos_a, sin_a, cos_i, sin_i, w_in[l], w_out[l], idx_kn_g[l], idx_kn_b[l],
                           gdn_conv_w[l], gdn_a_log[l], gdn_dt_bias[l], gdn_norm_w[l],
                           gla_w_up[l], gla_b_up[l], gla_norm_w[l],
                           ssd_conv_w[l], ssd_conv_b[l], ssd_a_log[l], ssd_dt_bias[l], ssd_d[l], ssd_norm_w[l])
        x = layer_norm(ALPHA * x + (1.0 + gate1) * mix, ln1_g[l], ln1_b[l])
        h = x * (1.0 + scale2) + shift2
        moe = hier_moe(h, router_g_w[l], router_g_b[l], router_e_w[l], router_e_b[l],
                       exp_w_gate[l], exp_w_up[l], exp_w_down[l])
        x = layer_norm(ALPHA * x + (1.0 + gate2) * moe, ln2_g[l], ln2_b[l])
    return x
```

```python
import math
import numpy as np
import concourse.bass as bass
import concourse.mybir as mybir
from concourse.bass_utils import run_bass_kernel_spmd
from contextlib import ExitStack

F32 = mybir.dt.float32
BF16 = mybir.dt.bfloat16
I32 = mybir.dt.int32
ALU = mybir.AluOpType
AF = mybir.ActivationFunctionType
AX = mybir.AxisListType
ENGS = ("pe", "act", "dve", "pool", "sp")
EPOCH = 30000
NEG = -1.0e30


class T:
    def __init__(self, name, h):
        self.name = name
        self.t = h
        self.st = {}

    def __getitem__(self, k):
        return self.t[k]


class Prog:
    def __init__(self, nc, n_dma_slots=32):
        self.nc = nc
        self.es = ExitStack()
        self.ops = {e: [] for e in ENGS}
        self.cnt = {e: 0 for e in ENGS}
        self.known = {e: {} for e in ENGS}
        self.sem = {}
        self.dma_slots = []
        for i in range(n_dma_slots):
            k = ("d", i)
            self.sem[k] = self.es.enter_context(nc.semaphore("d%d" % i))
            self.dma_slots.append([k, 0])
        self.dma_rr = 0
        self.n_t = 0
        self.n_ops = 0

    def _semkey(self, eng):
        k = (eng, self.cnt[eng] // EPOCH)
        if k not in self.sem:
            self.sem[k] = self.es.enter_context(self.nc.semaphore("s_%s_%d" % k))
        return k

    def sb(self, name, shape, dt=F32, stack=None):
        self.n_t += 1
        h = (stack or self.es).enter_context(self.nc.sbuf_tensor("%s_%d" % (name, self.n_t), list(shape), dt))
        return T(name, h)

    def ps(self, name, shape, dt=F32, stack=None):
        self.n_t += 1
        h = (stack or self.es).enter_context(self.nc.psum_tensor("%s_%d" % (name, self.n_t), list(shape), dt))
        t = T(name, h)
        t.psum = True
        return t

    def _fix(self, R, W):
        R2, W2 = [], []
        for a in R:
            t = a if isinstance(a, T) else a[0]
            if getattr(t, "psum", False):
                W2.append(t)
            else:
                R2.append(a)
        for a in W:
            t = a if isinstance(a, T) else a[0]
            W2.append(t if getattr(t, "psum", False) else a)
        return R2, W2

    @staticmethod
    def _norm(acc):
        out = []
        for a in acc:
            if isinstance(a, T):
                out.append((a, (None,)))
            else:
                out.append((a[0], tuple(a[1:]) if len(a) > 1 else (None,)))
        return out

    @staticmethod
    def _subs(t, subs):
        if None in subs:
            return list(t.st.keys()) + ([None] if None not in t.st else [])
        return list(subs) + [None]

    def _deps(self, reads, writes):
        deps = []
        for t, subs in self._norm(reads):
            for s in self._subs(t, subs):
                st = t.st.get(s)
                if st and st[0] is not None:
                    deps.append(st[0])
        for t, subs in self._norm(writes):
            for s in self._subs(t, subs):
                st = t.st.get(s)
                if st:
                    if st[0] is not None:
                        deps.append(st[0])
                    deps.extend(st[1].items())
        return deps

    def _commit(self, reads, writes, tok):
        for t, subs in self._norm(reads):
            for s in subs:
                st = t.st.setdefault(s, [None, {}])
                if st[1].get(tok[0], 0) < tok[1]:
                    st[1][tok[0]] = tok[1]
        for t, subs in self._norm(writes):
            if None in subs:
                t.st = {None: [tok, {}]}
            else:
                for s in subs:
                    t.st[s] = [tok, {}]

    def _waits(self, eng, deps):
        kn = self.known[eng]
        need = {}
        for k, v in deps:
            if k[0] == "pe" and eng == "pe":
                continue
            if kn.get(k, 0) < v:
                need[k] = max(need.get(k, 0), v)
        for k, v in need.items():
            kn[k] = v
        return list(need.items())

    def op(self, eng, fn, R=(), W=()):
        R, W = self._fix(R, W)
        deps = self._deps(R, W)
        waits = self._waits(eng, deps)
        k = self._semkey(eng)
        self.cnt[eng] += 1
        tok = (k, self.cnt[eng] - k[1] * EPOCH)
        self.ops[eng].append((waits, fn, (k, 1)))
        self._commit(R, W, tok)
        self.n_ops += 1
        return tok

    def dma(self, eng, out, in_, R=(), W=(), **kw):
        R, W = self._fix(R, W)
        deps = self._deps(R, W)
        slot = self.dma_slots[self.dma_rr]
        self.dma_rr = (self.dma_rr + 1) % len(self.dma_slots)
        if slot[1] > 0:
            deps.append((slot[0], slot[1]))
        waits = self._waits(eng, deps)
        slot[1] += 16
        tok = (slot[0], slot[1])

        def fn(e, out=out, in_=in_, kw=kw):
            return e.dma_start(out=out, in_=in_, **kw)
        self.ops[eng].append((waits, fn, (slot[0], 16)))
        self._commit(R, W, tok)
        self.n_ops += 1
        return tok

    def barrier(self):
        toks = []
        for e in ENGS:
            if self.cnt[e] > 0:
                k = (e, (self.cnt[e] - 1) // EPOCH)
                toks.append((k, self.cnt[e] - k[1] * EPOCH))
        for slot in self.dma_slots:
            if slot[1] > 0:
                toks.append((slot[0], slot[1]))
        for e in ENGS:
            w = self._waits(e, toks)
            if w:
                self.ops[e].append((w, None, None))

    def finish(self, toks, eng="sp"):
        self.ops[eng].append((self._waits(eng, list(toks)), None, None))

    def emit(self):
        nc = self.nc
        engmap = {"pe": "tensor", "act": "scalar", "dve": "vector", "pool": "gpsimd", "sp": "sync"}
        with nc.Block() as block:
            for e in ENGS:
                ops = self.ops[e]
                if not ops:
                    continue

                def body(engine, ops=ops):
                    for waits, fn, inc in ops:
                        for k, v in waits:
                            engine.wait_ge(self.sem[k], v)
                        if fn is not None:
                            fn(engine).then_inc(self.sem[inc[0]], inc[1])
                getattr(block, engmap[e])(body)
        self.es.close()

    def mm(self, out, lhsT, rhs, start=True, stop=True, R=(), W=()):
        return self.op("pe", lambda e: e.matmul(out, lhsT, rhs, start=start, stop=stop), R, W)

    def tr(self, out, in_, ident, R=(), W=()):
        return self.op("pe", lambda e: e.transpose(out, in_, ident), R, W)

    def act(self, out, in_, func, scale=1.0, bias=0.0, accum=None, R=(), W=()):
        if accum is None:
            return self.op("act", lambda e: e.activation(out, in_, func, scale=scale, bias=bias), R, W)
        return self.op("act", lambda e: e.activation(out, in_, func, scale=scale, bias=bias, accum_out=accum), R, W)

    def ts(self, eng, out, in0, s1, s2, op0, op1=None, accum=None, R=(), W=()):
        if op1 is None:
            op1 = ALU.bypass
        if accum is None:
            return self.op(eng, lambda e: e.tensor_scalar(out, in0, s1, s2, op0, op1), R, W)
        return self.op(eng, lambda e: e.tensor_scalar(out, in0, s1, s2, op0, op1, accum_out=accum), R, W)

    def tt(self, eng, out, in0, in1, op, R=(), W=()):
        return self.op(eng, lambda e: e.tensor_tensor(out, in0, in1, op), R, W)

    def stt(self, eng, out, in0, scalar, in1, op0, op1, R=(), W=()):
        return self.op(eng, lambda e: e.scalar_tensor_tensor(out, in0, scalar, in1, op0, op1), R, W)

    def cp(self, eng, out, in_, R=(), W=()):
        if eng == "act":
            return self.act(out, in_, AF.Copy, R=R, W=W)
        return self.op(eng, lambda e: e.tensor_copy(out, in_), R, W)

    def memset(self, eng, ap, val, W=()):
        return self.op(eng, lambda e: e.memset(ap, val), (), W)


IDX_HEADS, IDX_DIM = 16, 64
N_EXP, EXP_FF, N_GRP, EPG = 32, 256, 4, 8
ROPE_THETA = 10000.0
EPS = 1e-6


def derive(DM, S, DEPTH):
    G = DM // 4
    c = dict(DM=DM, S=S, KD=DM // 128, G=G, HA=G // 128, HB=G // 128, HC=G // 128, HD=G // 64,
             XBC=G + 512, KSEL=min(256, S // 4), NT=S // 128, DMIX=DM, KM=DM // 128)
    widths = (G, G, G, IDX_HEADS * IDX_DIM, IDX_DIM, IDX_HEADS,
              G, G, G, G // 128, G // 128, G,
              G // 2, G // 2, G, 16, G,
              G, G, 256, 256, G // 64)
    names = ("a_q", "a_k", "a_v", "a_qi", "a_ki", "a_wi", "b_q", "b_k", "b_v", "b_beta", "b_a", "b_z",
             "c_q", "c_k", "c_v", "c_gk", "c_g", "d_z", "d_x", "d_b", "d_c", "d_dt")
    off = {}
    o = 0
    for n, w in zip(names, widths):
        off[n] = (o, w)
        o += w
    c["off"] = off
    c["DP"] = o
    c["ALPHA"] = (2.0 * DEPTH) ** 0.25
    return c


WEIGHTS = ("ada_down", "ada_up", "ada_bias", "w_in", "w_out", "idx_kn_g", "idx_kn_b",
           "gdn_conv_w", "gdn_a_log", "gdn_dt_bias", "gdn_norm_w", "gla_w_up", "gla_b_up", "gla_norm_w",
           "ssd_conv_w", "ssd_conv_b", "ssd_a_log", "ssd_dt_bias", "ssd_d", "ssd_norm_w",
           "ln1_g", "ln1_b", "router_g_w", "router_g_b", "router_e_w", "router_e_b",
           "exp_w_gate", "exp_w_up", "exp_w_down", "ln2_g", "ln2_b")


def wshapes(c):
    DM, G = c["DM"], c["G"]
    return dict(ada_down=[DM, 256], ada_up=[256, 6 * DM], ada_bias=[6 * DM], w_in=[DM, c["DP"]], w_out=[DM, DM],
                idx_kn_g=[64], idx_kn_b=[64], gdn_conv_w=[4, 3 * G], gdn_a_log=[c["HB"]], gdn_dt_bias=[c["HB"]],
                gdn_norm_w=[128], gla_w_up=[16, G // 2], gla_b_up=[G // 2], gla_norm_w=[128],
                ssd_conv_w=[4, c["XBC"]], ssd_conv_b=[c["XBC"]], ssd_a_log=[c["HD"]], ssd_dt_bias=[c["HD"]],
                ssd_d=[c["HD"]], ssd_norm_w=[G], ln1_g=[DM], ln1_b=[DM], router_g_w=[DM, 4], router_g_b=[4],
                router_e_w=[DM, 32], router_e_b=[32], exp_w_gate=[32, DM, 256], exp_w_up=[32, DM, 256],
                exp_w_down=[32, 256, DM], ln2_g=[DM], ln2_b=[DM])


def host_consts():
    P = 128
    i = np.arange(P)
    U = (i[:, None] <= i[None, :]).astype(np.float32)
    cs = {"c_ident": np.eye(P, dtype=np.float32), "c_U": U, "c_NU": ((1.0 - U) * NEG).astype(np.float32),
          "c_ones": np.ones((P, P), np.float32), "c_NL": np.ascontiguousarray(((1.0 - U) * NEG).T.astype(np.float32))}
    LM = np.zeros((7, P, P), np.float32)
    for k in range(7):
        b = 1 << k
        r = (i // b)
        LM[k] = ((r[:, None] % 2 == 1) & (r[None, :] == r[:, None] - 1)).astype(np.float32)
    cs["c_LM"] = np.ascontiguousarray(LM.transpose(1, 0, 2))
    cs["c_LMT"] = np.ascontiguousarray(LM.transpose(2, 0, 1))
    sel = np.zeros((32, 32, P), np.float32)
    for e in range(32):
        sel[e, e, :] = 1.0
    cs["c_sel"] = np.ascontiguousarray(sel.transpose(1, 0, 2))
    return cs


def build(DM, S, NL, DEPTH, debug=False, STAGES=9, BIS=0, MIX="abcd"):
    c = derive(DM, S, DEPTH)
    KD, G, NT, DP = c["KD"], c["G"], c["NT"], c["DP"]
    HA, HB, HC, HD, XBC, KSEL = c["HA"], c["HB"], c["HC"], c["HD"], c["XBC"], c["KSEL"]
    off = c["off"]
    ALPHA = c["ALPHA"]
    nc = bass.Bass("TRN2", target_bir_lowering=False)
    P = Prog(nc)
    ES = P.es

    def din(name, shape, dt=F32):
        return T(name, nc.dram_tensor(name, list(shape), dt, kind="ExternalInput"))

    def dscr(name, shape, dt=F32, out=False):
        return T(name, nc.dram_tensor(name, list(shape), dt, kind="ExternalOutput" if out else "Internal"))

    x_in = din("x", [S, DM])
    c_in = din("c", [1, DM])
    pos_in = din("pos", [S, 1], I32)
    Wt = {n: din(n, [NL] + s) for n, s in wshapes(c).items()}
    CS = {n: din(n, list(a.shape)) for n, a in host_consts().items()}
    y_out = dscr("y", [S, DM], out=True)
    xa = dscr("xa", [S, DM]); xb = dscr("xb", [S, DM])
    proj = dscr("proj", [S, DP], out=debug)
    mixed = din("mixed_in", [S, DM]) if (debug and STAGES == 2) else dscr("mixed", [S, DM], out=debug)
    x1 = dscr("x1", [S, DM], out=debug)
    hg_d = dscr("hg", [S, N_EXP * EXP_FF]); hu_d = dscr("hu", [S, N_EXP * EXP_FF])
    modv = dscr("modv", [1, 6 * DM], out=debug)
    rope = dscr("rope", [S, 192], out=debug)

    ident = P.sb("ident", [128, 128]); Uc = P.sb("U", [128, 128]); NUc = P.sb("NU", [128, 128])
    ones = P.sb("ones", [128, 128])
    for tl, nm in ((ident, "c_ident"), (Uc, "c_U"), (NUc, "c_NU"), (ones, "c_ones")):
        P.dma("sp", tl[:], CS[nm][:, :], W=[tl])

    PS = [P.ps("ps%d" % i, [128, 512]) for i in range(8)]
    psrr = [0]

    psmod = [8]

    def nps():
        psrr[0] = (psrr[0] + 1) % psmod[0]
        return PS[psrr[0]]

    evrr = [0]

    def ev_eng():
        evrr[0] ^= 1
        return "act" if evrr[0] else "dve"

    from contextlib import contextmanager

    @contextmanager
    def scope():
        P.barrier()
        with ExitStack() as st:
            yield st
            P.barrier()

    castq = T("castq", None)
    castq_n = [0]

    def col_layout(st, src_row_ap, name):
        rows = P.sb(name + "r", [KD, 128], F32, st)
        P.dma("sp", rows[:], src_row_ap.rearrange("o (k p) -> (o k) p", p=128), W=[rows])
        pst = nps()
        P.tr(pst[:, 0:KD], rows[:], ident[0:KD, 0:KD], R=[rows, ident], W=[pst])
        tl = P.sb(name, [128, KD], F32, st)
        P.cp("dve", tl[:], pst[:, 0:KD], R=[pst], W=[tl])
        return tl

    def cast_weights(src_ap_fn, K, N, name):
        kd = K // 128
        NTL = 512 if kd <= 32 else 256
        NTL = min(NTL, N)
        nt = (N + NTL - 1) // NTL
        scr = dscr(name, [nt, 128, kd, NTL], BF16)
        for t in range(nt):
            w = min(NTL, N - t * NTL)
            src = src_ap_fn(t * NTL, w).rearrange("(kd p) n -> p kd n", p=128)
            for k0 in range(0, kd, 32):
                k1 = min(kd, k0 + 32)
                castq_n[0] += 1
                P.dma("pool", scr[t][:, k0:k1, 0:w], src[:, k0:k1, :], W=[(scr, t), (castq, castq_n[0] % 2)])
        return scr, NTL, nt

    def gemm(A, K, Wscr, NTL, nt, N, epilogue, prologue=None, name="g"):
        kd = K // 128
        TB = min(S, max(128, (32768 // kd) // 128 * 128))
        P.barrier()
        with scope() as st:
            AT = P.sb(name + "AT", [128, kd, TB], BF16, st)
            Wsb = [P.sb(name + "W%d" % i, [128, kd, NTL], BF16, st) for i in range(2)]
            xs = [P.sb(name + "xs%d" % i, [128, 2048], F32, st) for i in range(2)]
            k = 0
            for tb in range(0, S, TB):
                for tt_ in range(TB // 128):
                    t0 = tb + tt_ * 128
                    for c0 in range(0, K, 2048):
                        cw = min(2048, K - c0)
                        xt = xs[k % 2]; k += 1
                        P.dma("sp", xt[:, 0:cw], A[t0:t0 + 128, c0:c0 + cw], R=[A], W=[xt])
                        for j in range(cw // 128):
                            kk = (c0 // 128) + j
                            if j % 4 == 0:
                                pst = nps()
                            P.tr(pst[:, (j % 4) * 128:(j % 4 + 1) * 128], xt[:, j * 128:(j + 1) * 128], ident[:],
                                 R=[xt, ident], W=[(pst, j % 4)])
                            if prologue is None:
                                if j % 4 == 3:
                                    P.cp(ev_eng(), AT[:, kk - 3:kk + 1, tt_ * 128:(tt_ + 1) * 128],
                                         pst[:, :].rearrange("p (a b) -> p a b", a=4), R=[pst], W=[(AT, tt_)])
                            else:
                                prologue(kk, AT[:, kk, tt_ * 128:(tt_ + 1) * 128], pst[:, (j % 4) * 128:(j % 4 + 1) * 128],
                                         [(pst, j % 4)], [(AT, tt_)])
                for t in range(0 if BIS == 3 else (nt - 1 if BIS == 4 else nt)):
                    w = min(NTL, N - t * NTL)
                    ws = Wsb[t % 2]
                    P.dma("sp", ws[:], Wscr[t], R=[(Wscr, t)], W=[ws])
                    for tt_ in range(TB // 128):
                        pst = nps()
                        for kk in range(kd):
                            P.mm(pst[:, 0:NTL], AT[:, kk, tt_ * 128:(tt_ + 1) * 128], ws[:, kk, :], start=(kk == 0),
                                 stop=(kk == kd - 1), R=[(AT, tt_), ws], W=[pst])
                        epilogue(tb + tt_ * 128, t * NTL, w, pst, [pst])

    def store_epilogue(dst, st):
        bufs = [P.sb("ob%d" % i, [128, 512], F32, st) for i in range(3)]
        cnt = [0]

        def ep(t0, n0, w, pst, R):
            ob = bufs[cnt[0] % 3]; cnt[0] += 1
            P.cp(ev_eng(), ob[:, 0:w], pst[:, 0:w], R=R, W=[ob])
            P.dma("act", dst[t0:t0 + 128, n0:n0 + w], ob[:, 0:w], R=[ob], W=[dst])
        return ep

    def bcast_row(dst_tile, src_dram_row_ap, n, eng="sp"):
        P.dma(eng, dst_tile[:, 0:n], src_dram_row_ap.to_broadcast([128, n]), W=[dst_tile])

    def stage_mod(l):
        P.barrier()
        with scope() as st:
            cT = P.sb("cT", [128, KD, 2], F32, st)
            P.memset("dve", cT[:], 0.0, W=[cT])
            cc = col_layout(st, c_in.t.ap(), "ccol")
            P.cp("dve", cT[:, :, 0:1], cc[:].rearrange("p (k o) -> p k o", o=1), R=[cc], W=[cT])
            P.act(cT[:], cT[:], AF.Silu, R=[cT], W=[cT])
            ad = P.sb("ad", [128, KD, 256], F32, st)
            P.dma("sp", ad[:], Wt["ada_down"][l].rearrange("(k p) r -> p k r", p=128), W=[ad])
            vT = P.sb("vT", [128, 2], F32, st)
            for rh in range(2):
                pst = nps()
                for kk in range(KD):
                    P.mm(pst[:, 0:2], ad[:, kk, rh * 128:(rh + 1) * 128], cT[:, kk, :], start=(kk == 0), stop=(kk == KD - 1),
                         R=[ad, cT], W=[pst])
                P.cp("dve", vT[:, rh:rh + 1], pst[:, 0:1], R=[pst], W=[vT])
            au = [P.sb("au%d" % i, [128, 2, 512], F32, st) for i in range(2)]
            bb = [P.sb("bb%d" % i, [1, 512], F32, st) for i in range(2)]
            rw = [P.sb("rw%d" % i, [1, 512], F32, st) for i in range(2)]
            for mc in range(6 * DM // 512):
                a_, b_, r_ = au[mc % 2], bb[mc % 2], rw[mc % 2]
                P.dma("sp", a_[:], Wt["ada_up"][l][:, mc * 512:(mc + 1) * 512].rearrange("(r p) n -> p r n", p=128), W=[a_])
                P.dma("sp", b_[:], Wt["ada_bias"][l:l + 1, mc * 512:(mc + 1) * 512], W=[b_])
                pst = nps()
                for rh in range(2):
                    P.mm(pst[0:1, :], vT[:, rh:rh + 1], a_[:, rh, :], start=(rh == 0), stop=(rh == 1), R=[vT, a_], W=[pst])
                P.tt("dve", r_[:], pst[0:1, :], b_[:], ALU.add, R=[pst, b_], W=[r_])
                P.dma("act", modv[0:1, mc * 512:(mc + 1) * 512], r_[:], R=[r_], W=[modv])

    def mod_cols(st, part, plus_one):
        P.barrier()
        tl = col_layout(st, modv.t.ap()[0:1, part * DM:(part + 1) * DM], "mc%d" % part)
        if plus_one:
            P.ts("dve", tl[:], tl[:], 1.0, None, ALU.add, R=[tl], W=[tl])
        return tl

    def stage_proj(l, xin):
        with scope() as st:
            wscr, NTL, nt = cast_weights(lambda n0, w: Wt["w_in"][l][:, n0:n0 + w], DM, DP, "win_bf%d" % l)
            s1 = mod_cols(st, 1, True)
            sh1 = mod_cols(st, 0, False)

            def prologue(kk, out_ap, ps_ap, R, W):
                P.act(out_ap, ps_ap, AF.Identity, scale=s1[:, kk:kk + 1], bias=sh1[:, kk:kk + 1], R=R + [s1, sh1], W=W)
            if BIS != 1:
                gemm(xin, DM, wscr, NTL, nt, DP, store_epilogue(proj, st), None if BIS in (2, 3, 4) else prologue, name="pj")

    def conv_silu(t, c0, ncols, wbc, bias_bc, win, acc, tmp):
        for i in range(4):
            r0 = t * 128 - 3 + i
            wt = win[i]
            if r0 < 0:
                P.memset("pool", wt[:, 0:ncols], 0.0, W=[wt])
                P.dma("sp", wt[-r0:128, 0:ncols], proj[0:128 + r0, c0:c0 + ncols], R=[proj], W=[wt])
            else:
                P.dma("sp", wt[:, 0:ncols], proj[r0:r0 + 128, c0:c0 + ncols], R=[proj], W=[wt])
        P.tt("pool", acc[:, 0:ncols], win[0][:, 0:ncols], wbc[0][:, 0:ncols], ALU.mult, R=[win[0], wbc[0]], W=[acc])
        for i in range(1, 4):
            P.tt("pool", tmp[:, 0:ncols], win[i][:, 0:ncols], wbc[i][:, 0:ncols], ALU.mult, R=[win[i], wbc[i]], W=[tmp])
            P.tt("dve", acc[:, 0:ncols], acc[:, 0:ncols], tmp[:, 0:ncols], ALU.add, R=[acc, tmp], W=[acc])
        if bias_bc is not None:
            P.tt("dve", acc[:, 0:ncols], acc[:, 0:ncols], bias_bc[:, 0:ncols], ALU.add, R=[acc, bias_bc], W=[acc])
        P.act(acc[:, 0:ncols], acc[:, 0:ncols], AF.Silu, R=[acc], W=[acc])

    def ldp(tile, t, name, eng="sp"):
        o, w = off[name]
        kw = {"allow_slow_non_contiguous": True} if w == 1 else {}
        P.dma(eng, tile[:, 0:w], proj[t * 128:(t + 1) * 128, o:o + w], R=[proj], W=[tile], **kw)

    def bc3(ap2, h, d):
        return ap2.rearrange("p (h o) -> p h o", o=1).to_broadcast([128, h, d])

    def v3(ap2, h):
        return ap2.rearrange("p (h d) -> p h d", h=h)

    def decayT(g, h0, nh, dst, ug, nug):
        pst = nps()
        for k in range(nh):
            h = h0 + k
            P.ts("pool", ug[:], Uc[:], g[:, h:h + 1], None, ALU.mult, R=[Uc, g], W=[ug])
            P.ts("dve", nug[:], Uc[:], g[:, h:h + 1], -1.0, ALU.mult, ALU.mult, R=[Uc, g], W=[nug])
            sl = pst[:, k * 128:(k + 1) * 128]
            P.mm(sl, ones[:], ug[:], start=True, stop=False, R=[ones, ug], W=[pst])
            P.mm(sl, nug[:], ones[:], start=False, stop=False, R=[nug, ones], W=[pst])
            P.mm(sl, ident[:], NUc[:], start=False, stop=True, R=[ident, NUc], W=[pst])
        P.act(dst[:, 0:nh * 128], pst[:, 0:nh * 128], AF.Exp, R=[pst], W=[dst])

    def stage_ssd(l):
        HG = HD // 2
        with scope() as st:
            wbc = [P.sb("dw%d" % i, [128, XBC], F32, st) for i in range(4)]
            for i in range(4):
                bcast_row(wbc[i], Wt["ssd_conv_w"][l, i:i + 1, :], XBC)
            cb = P.sb("dcb", [128, XBC], F32, st); bcast_row(cb, Wt["ssd_conv_b"][l:l + 1, :], XBC)
            dtb = P.sb("ddtb", [128, HD], F32, st); bcast_row(dtb, Wt["ssd_dt_bias"][l:l + 1, :], HD)
            adec = P.sb("dadec", [128, HD], F32, st); bcast_row(adec, Wt["ssd_a_log"][l:l + 1, :], HD)
            P.act(adec[:], adec[:], AF.Exp, R=[adec], W=[adec])
            P.ts("dve", adec[:], adec[:], -1.0, None, ALU.mult, R=[adec], W=[adec])
            dsk = P.sb("ddsk", [128, HD], F32, st); bcast_row(dsk, Wt["ssd_d"][l:l + 1, :], HD)
            nw = P.sb("dnw", [128, G], F32, st); bcast_row(nw, Wt["ssd_norm_w"][l:l + 1, :], G)
            win = [P.sb("dwin%d" % i, [128, XBC], F32, st) for i in range(4)]
            acc = P.sb("dacc", [128, XBC], F32, st); tmp = P.sb("dtmp", [128, XBC], F32, st)
            zt = P.sb("dz", [128, G], F32, st); dtt = P.sb("ddt", [128, HD], F32, st)
            g = P.sb("dg", [128, HD], F32, st); gc = P.sb("dgc", [128, HD], F32, st); gcl = P.sb("dgcl", [128, HD], F32, st)
            eg = P.sb("deg", [128, HD], F32, st); egl = P.sb("degl", [128, HD], F32, st); gl = P.sb("dgl", [128, HD], F32, st)
            xdt = P.sb("dxdt", [128, G], F32, st)
            BCT = P.sb("dBCT", [128, 4, 128], F32, st)
            CBT = P.sb("dCBT", [128, 2, 128], F32, st)
            ug = P.sb("dug", [128, 128], F32, st); nug = P.sb("dnug", [128, 128], F32, st)
            dec = P.sb("ddec", [128, 512], F32, st)
            ST = P.sb("dST", [128, HD, 128], F32, st)
            Bd = [P.sb("dBd%d" % i, [128, 128], F32, st) for i in range(2)]
            S_ = P.sb("dS", [128, G], F32, st)
            P.memset("dve", S_[:], 0.0, W=[S_])
            o_ = P.sb("do", [128, G], F32, st); t2 = P.sb("dt2", [128, G], F32, st)
            sm = P.sb("dsm", [128, 8], F32, st)
            for t in range(NT):
                conv_silu(t, off["d_x"][0], XBC, wbc, cb, win, acc, tmp)
                ldp(zt, t, "d_z"); ldp(dtt, t, "d_dt")
                P.tt("dve", dtt[:], dtt[:], dtb[:], ALU.add, R=[dtt, dtb], W=[dtt])
                P.act(dtt[:], dtt[:], AF.Exp, R=[dtt], W=[dtt])
                P.act(dtt[:], dtt[:], AF.Ln, bias=1.0, R=[dtt], W=[dtt])
                P.tt("dve", g[:], dtt[:], adec[:], ALU.mult, R=[dtt, adec], W=[g])
                P.tt("dve", v3(xdt[:], HD), v3(acc[:, 0:G], HD), bc3(dtt[:], HD, 64), ALU.mult, R=[acc, dtt], W=[xdt])
                pst = nps()
                P.mm(pst[:, 0:HD], Uc[:], g[:], R=[Uc, g], W=[pst])
                P.mm(pst[:, 64:64 + HD], ones[:], g[:], R=[ones, g], W=[pst])
                P.cp("dve", gc[:], pst[:, 0:HD], R=[pst], W=[gc])
                P.cp("dve", gcl[:], pst[:, 64:64 + HD], R=[pst], W=[gcl])
                P.act(eg[:], gc[:], AF.Exp, R=[gc], W=[eg])
                P.act(gl[:], gcl[:], AF.Exp, R=[gcl], W=[gl])
                P.tt("dve", egl[:], gcl[:], gc[:], ALU.subtract, R=[gcl, gc], W=[egl])
                P.act(egl[:], egl[:], AF.Exp, R=[egl], W=[egl])
                pst = nps()
                for k in range(4):
                    P.tr(pst[:, k * 128:(k + 1) * 128], acc[:, G + k * 128:G + (k + 1) * 128], ident[:], R=[acc, ident], W=[pst])
                P.cp("act", BCT[:], v3(pst[:, :], 4), R=[pst], W=[BCT])
                pst = nps()
                for gr in range(2):
                    P.mm(pst[:, gr * 128:(gr + 1) * 128], BCT[:, gr, :], BCT[:, 2 + gr, :], R=[BCT], W=[pst])
                P.cp("act", CBT[:], v3(pst[:, 0:256], 2), R=[pst], W=[CBT])
                for h0 in range(0, HD, 4):
                    nh = min(4, HD - h0)
                    decayT(g, h0, nh, dec, ug, nug)
                    for k in range(nh):
                        h = h0 + k
                        P.tt("dve" if k % 2 else "pool", ST[:, h, :], dec[:, k * 128:(k + 1) * 128], CBT[:, h // HG, :], ALU.mult,
                             R=[dec, CBT], W=[(ST, h)])
                p_in = [nps() for _ in range((HD * 64 + 511) // 512)]
                for h in range(HD):
                    P.mm(p_in[(h * 64) // 512][:, (h * 64) % 512:(h * 64) % 512 + 64], ST[:, h, :], xdt[:, h * 64:(h + 1) * 64],
                         R=[(ST, h), xdt], W=[p_in[(h * 64) // 512]])
                p_x = [nps() for _ in range(2)]
                for gr in range(2):
                    P.mm(p_x[gr][:, 0:HG * 64], BCT[:, 2 + gr, :], S_[:, gr * HG * 64:(gr + 1) * HG * 64], R=[BCT, S_], W=[p_x[gr]])
                for gr in range(2):
                    cs = slice(gr * HG * 64, (gr + 1) * HG * 64)
                    P.tt("dve", v3(o_[:, cs], HG), v3(p_x[gr][:, 0:HG * 64], HG), bc3(eg[:, gr * HG:(gr + 1) * HG], HG, 64), ALU.mult,
                         R=[p_x[gr], eg], W=[o_])
                for bi, pb in enumerate(p_in):
                    w = min(512, HD * 64 - bi * 512)
                    P.tt("dve", o_[:, bi * 512:bi * 512 + w], o_[:, bi * 512:bi * 512 + w], pb[:, 0:w], ALU.add, R=[o_, pb], W=[o_])
                p_s = [nps() for _ in range((HD * 64 + 511) // 512)]
                for h in range(HD):
                    bd = Bd[h % 2]
                    P.ts("pool" if h % 2 else "dve", bd[:], acc[:, G + (h // HG) * 128:G + (h // HG + 1) * 128], egl[:, h:h + 1], None, ALU.mult,
                         R=[acc, egl], W=[bd])
                    P.mm(p_s[(h * 64) // 512][:, (h * 64) % 512:(h * 64) % 512 + 64], bd[:], xdt[:, h * 64:(h + 1) * 64],
                         R=[bd, xdt], W=[p_s[(h * 64) // 512]])
                P.tt("dve", v3(S_[:], HD), v3(S_[:], HD), bc3(gl[:], HD, 64), ALU.mult, R=[S_, gl], W=[S_])
                for bi, pb in enumerate(p_s):
                    w = min(512, HD * 64 - bi * 512)
                    P.tt("dve", S_[:, bi * 512:bi * 512 + w], S_[:, bi * 512:bi * 512 + w], pb[:, 0:w], ALU.add, R=[S_, pb], W=[S_])
                P.tt("pool", v3(t2[:], HD), v3(acc[:, 0:G], HD), bc3(dsk[:], HD, 64), ALU.mult, R=[acc, dsk], W=[t2])
                P.tt("dve", o_[:], o_[:], t2[:], ALU.add, R=[o_, t2], W=[o_])
                P.act(zt[:], zt[:], AF.Silu, R=[zt], W=[zt])
                P.tt("dve", o_[:], o_[:], zt[:], ALU.mult, R=[o_, zt], W=[o_])
                for gr in range(2):
                    cs = slice(gr * G // 2, (gr + 1) * G // 2)
                    P.act(t2[:, cs], o_[:, cs], AF.Square, accum=sm[:, gr:gr + 1], R=[o_], W=[t2, sm])
                P.ts("dve", sm[:, 2:4], sm[:, 0:2], 2.0 / G, EPS, ALU.mult, ALU.add, R=[sm], W=[sm])
                P.act(sm[:, 4:6], sm[:, 2:4], AF.Sqrt, R=[sm], W=[sm])
                P.op("dve", lambda e: e.reciprocal(sm[:, 6:8], sm[:, 4:6]), R=[sm], W=[sm])
                for gr in range(2):
                    cs = slice(gr * G // 2, (gr + 1) * G // 2)
                    P.ts("dve", o_[:, cs], o_[:, cs], sm[:, 6 + gr:7 + gr], None, ALU.mult, R=[o_, sm], W=[o_])
                P.tt("pool", o_[:], o_[:], nw[:], ALU.mult, R=[o_, nw], W=[o_])
                P.dma("act", mixed[t * 128:(t + 1) * 128, 3 * G:4 * G], o_[:], R=[o_], W=[(mixed, t, 3)])

    def stage_gla(l):
        G2 = G // 2
        with scope() as st:
            wup = P.sb("cwup", [16, G2], F32, st)
            P.dma("sp", wup[:], Wt["gla_w_up"][l], W=[wup])
            bup = P.sb("cbup", [128, G2], F32, st); bcast_row(bup, Wt["gla_b_up"][l:l + 1, :], G2)
            nw = P.sb("cnw", [128, 128], F32, st); bcast_row(nw, Wt["gla_norm_w"][l:l + 1, :], 128)
            q = P.sb("cq", [128, G2], F32, st); k = P.sb("ck", [128, G2], F32, st); v = P.sb("cv", [128, G], F32, st)
            gkr = P.sb("cgkr", [128, 16], F32, st); gz = P.sb("cgz", [128, G], F32, st)
            gkT = P.sb("cgkT", [16, 128], F32, st)
            gk = P.sb("cgk", [128, G2], F32, st); b = P.sb("cb", [128, G2], F32, st)
            e1 = P.sb("ce1", [128, G2], F32, st); e2 = P.sb("ce2", [128, G2], F32, st); e3 = P.sb("ce3", [128, G2], F32, st)
            qe = P.sb("cqe", [128, G2], F32, st); ke = P.sb("cke", [128, G2], F32, st); kd = P.sb("ckd", [128, G2], F32, st)
            qeT = P.sb("cqeT", [64, HC, 128], F32, st); keT = P.sb("ckeT", [64, HC, 128], F32, st)
            ebt = P.sb("cebt", [64, HC, 2], F32, st)
            one2 = P.sb("cone2", [128, 2], F32, st); P.memset("dve", one2[:], 1.0, W=[one2])
            aT = [P.sb("caT%d" % i, [128, 128], F32, st) for i in range(2)]
            S_ = P.sb("cS", [64, HC * 128], F32, st); P.memset("dve", S_[:], 0.0, W=[S_])
            o_ = P.sb("co", [128, G], F32, st); t2 = P.sb("ct2", [128, G], F32, st)
            sm = P.sb("csm", [128, 4 * HC], F32, st)
            for t in range(NT):
                ldp(q, t, "c_q"); ldp(k, t, "c_k"); ldp(v, t, "c_v"); ldp(gkr, t, "c_gk"); ldp(gz, t, "c_g")
                pst = nps()
                P.tr(pst[0:16, 0:128], gkr[:], ident[:], R=[gkr, ident], W=[pst])
                P.cp("act", gkT[:], pst[0:16, 0:128], R=[pst], W=[gkT])
                pst = nps()
                P.mm(pst[:, 0:G2], gkT[:], wup[:], R=[gkT, wup], W=[pst])
                P.tt("dve", gk[:], pst[:, 0:G2], bup[:], ALU.add, R=[pst, bup], W=[gk])
                P.act(gk[:], gk[:], AF.Exp, scale=-1.0, R=[gk], W=[gk])
                P.act(gk[:], gk[:], AF.Ln, bias=1.0, R=[gk], W=[gk])
                P.ts("dve", gk[:], gk[:], -1.0 / 16.0, None, ALU.mult, R=[gk], W=[gk])
                pst = nps(); pst2 = nps()
                P.mm(pst[:, 0:G2], Uc[:], gk[:], R=[Uc, gk], W=[pst])
                P.mm(pst2[:, 0:G2], ones[:], gk[:], R=[ones, gk], W=[pst2])
                P.cp("dve", b[:], pst[:, 0:G2], R=[pst], W=[b])
                P.act(e1[:], b[:], AF.Exp, R=[b], W=[e1])
                P.act(e2[:], b[:], AF.Exp, scale=-1.0, R=[b], W=[e2])
                P.tt("dve", e3[:], pst2[:, 0:G2], b[:], ALU.subtract, R=[pst2, b], W=[e3])
                P.act(e3[:], e3[:], AF.Exp, R=[e3], W=[e3])
                P.stt("dve", qe[:], q[:], 0.125, e1[:], ALU.mult, ALU.mult, R=[q, e1], W=[qe])
                P.tt("pool", ke[:], k[:], e2[:], ALU.mult, R=[k, e2], W=[ke])
                P.tt("pool", kd[:], k[:], e3[:], ALU.mult, R=[k, e3], W=[kd])
                for src, dstT in ((qe, qeT), (ke, keT)):
                    for h0 in range(0, HC, 4):
                        nh = min(4, HC - h0)
                        pst = nps()
                        for kk in range(nh):
                            P.tr(pst[0:64, kk * 128:(kk + 1) * 128], src[:, (h0 + kk) * 64:(h0 + kk + 1) * 64], ident[:], R=[src, ident], W=[pst])
                        P.cp("act", dstT[:, h0:h0 + nh, :], v3(pst[0:64, 0:nh * 128], nh), R=[pst], W=[dstT])
                for h in range(HC):
                    pst = nps()
                    P.mm(pst[0:64, 0:2], gk[:, h * 64:(h + 1) * 64], one2[:], R=[gk, one2], W=[pst])
                    P.act(ebt[:, h, :], pst[0:64, 0:2], AF.Exp, R=[pst], W=[ebt])
                for h0 in range(0, HC, 4):
                    nh = min(4, HC - h0)
                    p_o = nps()
                    for kk in range(nh):
                        h = h0 + kk
                        pst = nps()
                        P.mm(pst[:, 0:128], keT[:, h, :], qeT[:, h, :], R=[keT, qeT], W=[pst])
                        at = aT[h % 2]
                        P.tt("dve", at[:], pst[:, 0:128], Uc[:], ALU.mult, R=[pst, Uc], W=[at])
                        osl = p_o[:, kk * 128:(kk + 1) * 128]
                        P.mm(osl, at[:], v[:, h * 128:(h + 1) * 128], start=True, stop=False, R=[at, v], W=[p_o])
                        P.mm(osl, qeT[:, h, :], S_[:, h * 128:(h + 1) * 128], start=False, stop=True, R=[qeT, S_], W=[p_o])
                    P.cp("act", o_[:, h0 * 128:(h0 + nh) * 128], p_o[:, 0:nh * 128], R=[p_o], W=[o_])
                    for kk in range(nh):
                        h = h0 + kk
                        pst = nps()
                        P.mm(pst[0:64, 0:128], kd[:, h * 64:(h + 1) * 64], v[:, h * 128:(h + 1) * 128], R=[kd, v], W=[pst])
                        P.stt("dve", S_[:, h * 128:(h + 1) * 128], S_[:, h * 128:(h + 1) * 128], ebt[:, h, 0:1], pst[0:64, 0:128],
                              ALU.mult, ALU.add, R=[S_, ebt, pst], W=[S_])
                P.tt("pool", t2[:], o_[:], o_[:], ALU.mult, R=[o_], W=[t2])
                P.op("dve", lambda e: e.reduce_sum(sm[:, 0:HC], v3(t2[:], HC), AX.X), R=[t2], W=[sm])
                P.ts("dve", sm[:, HC:2 * HC], sm[:, 0:HC], 1.0 / 128.0, EPS, ALU.mult, ALU.add, R=[sm], W=[sm])
                P.act(sm[:, 2 * HC:3 * HC], sm[:, HC:2 * HC], AF.Sqrt, R=[sm], W=[sm])
                P.op("dve", lambda e: e.reciprocal(sm[:, 3 * HC:4 * HC], sm[:, 2 * HC:3 * HC]), R=[sm], W=[sm])
                P.tt("dve", v3(o_[:], HC), v3(o_[:], HC), bc3(sm[:, 3 * HC:4 * HC], HC, 128), ALU.mult, R=[o_, sm], W=[o_])
                P.tt("pool", v3(o_[:], HC), v3(o_[:], HC), nw[:].rearrange("p (o d) -> p o d", o=1).to_broadcast([128, HC, 128]), ALU.mult,
                     R=[o_, nw], W=[o_])
                P.act(gz[:], gz[:], AF.Silu, R=[gz], W=[gz])
                P.tt("dve", o_[:], o_[:], gz[:], ALU.mult, R=[o_, gz], W=[o_])
                P.dma("act", mixed[t * 128:(t + 1) * 128, 2 * G:3 * G], o_[:], R=[o_], W=[(mixed, t, 2)])

    def stage_gdn(l):
        with scope() as st:
            CW = 3 * G
            wbc = [P.sb("bw%d" % i, [128, CW], F32, st) for i in range(4)]
            for i in range(4):
                bcast_row(wbc[i], Wt["gdn_conv_w"][l, i:i + 1, :], CW)
            dtb = P.sb("bdtb", [128, HB], F32, st); bcast_row(dtb, Wt["gdn_dt_bias"][l:l + 1, :], HB)
            adec = P.sb("badec", [128, HB], F32, st); bcast_row(adec, Wt["gdn_a_log"][l:l + 1, :], HB)
            P.act(adec[:], adec[:], AF.Exp, R=[adec], W=[adec])
            P.ts("dve", adec[:], adec[:], -1.0, None, ALU.mult, R=[adec], W=[adec])
            nw = P.sb("bnw", [128, 128], F32, st); bcast_row(nw, Wt["gdn_norm_w"][l:l + 1, :], 128)
            LM = P.sb("bLM", [128, 7, 128], F32, st); LMT = P.sb("bLMT", [128, 7, 128], F32, st)
            P.dma("sp", LM[:], CS["c_LM"][:, :, :], W=[LM]); P.dma("sp", LMT[:], CS["c_LMT"][:, :, :], W=[LMT])
            win = [P.sb("bwin%d" % i, [128, CW], F32, st) for i in range(4)]
            acc = P.sb("bacc", [128, CW], F32, st); tmp = P.sb("btmp", [128, CW], F32, st)
            zt = P.sb("bz", [128, G], F32, st); beta = P.sb("bbeta", [128, HB], F32, st); ba = P.sb("bba", [128, HB], F32, st)
            g = P.sb("bg", [128, HB], F32, st); gc = P.sb("bgc", [128, HB], F32, st); gcl = P.sb("bgcl", [128, HB], F32, st)
            eg = P.sb("beg", [128, HB], F32, st); egl = P.sb("begl", [128, HB], F32, st); gl = P.sb("bgl", [128, HB], F32, st)
            rn = P.sb("brn", [128, 4 * 2 * HB], F32, st)
            qn = P.sb("bqn", [128, G], F32, st); kn = P.sb("bkn", [128, G], F32, st)
            kb = P.sb("bkb", [128, G], F32, st); vb = P.sb("bvb", [128, G], F32, st)
            qg = P.sb("bqg", [128, G], F32, st); kbg = P.sb("bkbg", [128, G], F32, st); kdd = P.sb("bkdd", [128, G], F32, st)
            ug = P.sb("bug", [128, 128], F32, st); nug = P.sb("bnug", [128, 128], F32, st)
            dec = P.sb("bdec", [128, 512], F32, st)
            TT = P.sb("bTT", [128, 4, 128], F32, st)
            AQ = P.sb("bAQ", [128, 2, 128], F32, st)
            A_ = P.sb("bA", [128, 128], F32, st)
            LA = P.sb("bLA", [128, 7, 128], F32, st); LAT = P.sb("bLAT", [128, 7, 128], F32, st)
            DE = P.sb("bDE", [128, 2, 128], F32, st); X = P.sb("bX", [128, 2, 128], F32, st)
            u0 = P.sb("bu0", [128, 128], F32, st); wT = P.sb("bwT", [128, 128], F32, st); u = P.sb("bu", [128, 128], F32, st)
            S_ = P.sb("bS", [128, HB * 128], F32, st); P.memset("dve", S_[:], 0.0, W=[S_])
            o_ = P.sb("bo", [128, G], F32, st); t2 = P.sb("bt2", [128, G], F32, st)
            sm = P.sb("bsm", [128, 4 * HB], F32, st)
            for t in range(NT):
                conv_silu(t, off["b_q"][0], CW, wbc, None, win, acc, tmp)
                ldp(zt, t, "b_z"); ldp(beta, t, "b_beta"); ldp(ba, t, "b_a")
                P.act(beta[:], beta[:], AF.Sigmoid, R=[beta], W=[beta])
                P.tt("dve", ba[:], ba[:], dtb[:], ALU.add, R=[ba, dtb], W=[ba])
                P.act(ba[:], ba[:], AF.Exp, R=[ba], W=[ba])
                P.act(ba[:], ba[:], AF.Ln, bias=1.0, R=[ba], W=[ba])
                P.tt("dve", g[:], ba[:], adec[:], ALU.mult, R=[ba, adec], W=[g])
                P.tt("pool", tmp[:, 0:2 * G], acc[:, 0:2 * G], acc[:, 0:2 * G], ALU.mult, R=[acc], W=[tmp])
                P.op("dve", lambda e: e.reduce_sum(rn[:, 0:2 * HB], v3(tmp[:, 0:2 * G], 2 * HB), AX.X), R=[tmp], W=[rn])
                P.ts("dve", rn[:, 2 * HB:4 * HB], rn[:, 0:2 * HB], EPS, None, ALU.add, R=[rn], W=[rn])
                P.act(rn[:, 4 * HB:6 * HB], rn[:, 2 * HB:4 * HB], AF.Sqrt, R=[rn], W=[rn])
                P.op("dve", lambda e: e.reciprocal(rn[:, 6 * HB:8 * HB], rn[:, 4 * HB:6 * HB]), R=[rn], W=[rn])
                P.ts("dve", rn[:, 6 * HB:7 * HB], rn[:, 6 * HB:7 * HB], 128.0 ** -0.5, None, ALU.mult, R=[rn], W=[rn])
                P.tt("dve", v3(qn[:], HB), v3(acc[:, 0:G], HB), bc3(rn[:, 6 * HB:7 * HB], HB, 128), ALU.mult, R=[acc, rn], W=[qn])
                P.tt("dve", v3(kn[:], HB), v3(acc[:, G:2 * G], HB), bc3(rn[:, 7 * HB:8 * HB], HB, 128), ALU.mult, R=[acc, rn], W=[kn])
                P.tt("pool", v3(kb[:], HB), v3(kn[:], HB), bc3(beta[:], HB, 128), ALU.mult, R=[kn, beta], W=[kb])
                P.tt("pool", v3(vb[:], HB), v3(acc[:, 2 * G:3 * G], HB), bc3(beta[:], HB, 128), ALU.mult, R=[acc, beta], W=[vb])
                pst = nps()
                P.mm(pst[:, 0:HB], Uc[:], g[:], R=[Uc, g], W=[pst])
                P.mm(pst[:, 64:64 + HB], ones[:], g[:], R=[ones, g], W=[pst])
                P.cp("dve", gc[:], pst[:, 0:HB], R=[pst], W=[gc])
                P.cp("dve", gcl[:], pst[:, 64:64 + HB], R=[pst], W=[gcl])
                P.act(eg[:], gc[:], AF.Exp, R=[gc], W=[eg])
                P.act(gl[:], gcl[:], AF.Exp, R=[gcl], W=[gl])
                P.tt("dve", egl[:], gcl[:], gc[:], ALU.subtract, R=[gcl, gc], W=[egl])
                P.act(egl[:], egl[:], AF.Exp, R=[egl], W=[egl])
                P.tt("dve", v3(qg[:], HB), v3(qn[:], HB), bc3(eg[:], HB, 128), ALU.mult, R=[qn, eg], W=[qg])
                P.tt("pool", v3(kbg[:], HB), v3(kb[:], HB), bc3(eg[:], HB, 128), ALU.mult, R=[kb, eg], W=[kbg])
                P.tt("pool", v3(kdd[:], HB), v3(kn[:], HB), bc3(egl[:], HB, 128), ALU.mult, R=[kn, egl], W=[kdd])
                for h0 in range(0, HB, 4):
                    nh = min(4, HB - h0)
                    decayT(g, h0, nh, dec, ug, nug)
                    for kk in range(nh):
                        h = h0 + kk
                        hs = slice(h * 128, (h + 1) * 128)
                        pst = nps()
                        for i_, src in enumerate((kn, kb, qn, qg)):
                            P.tr(pst[:, i_ * 128:(i_ + 1) * 128], src[:, hs], ident[:], R=[src, ident], W=[pst])
                        P.cp("act", TT[:], v3(pst[:, :], 4), R=[pst], W=[TT])
                        pst = nps()
                        P.mm(pst[:, 0:256], TT[:, 0, :], TT[:, 1:3, :], R=[TT], W=[pst])
                        P.tt("dve", AQ[:], v3(pst[:, 0:256], 2),
                             dec[:, kk * 128:(kk + 1) * 128].rearrange("p (o d) -> p o d", o=1).to_broadcast([128, 2, 128]), ALU.mult,
                             R=[pst, dec], W=[AQ])
                        pst = nps()
                        P.tr(pst[:, 0:128], AQ[:, 0, :], ident[:], R=[AQ, ident], W=[pst])
                        P.cp("act", A_[:], pst[:, 0:128], R=[pst], W=[A_])
                        P.tt("pool", LA[:], LM[:], A_[:].rearrange("p (o d) -> p o d", o=1).to_broadcast([128, 7, 128]), ALU.mult,
                             R=[LM, A_], W=[LA])
                        P.tt("dve", LAT[:], LMT[:], AQ[:, 0, :].rearrange("p (o d) -> p o d", o=1).to_broadcast([128, 7, 128]), ALU.mult,
                             R=[LMT, AQ], W=[LAT])
                        P.tt("dve", DE[:, 0, :], ident[:], LA[:, 0, :], ALU.subtract, R=[ident, LA], W=[DE])
                        P.tt("dve", DE[:, 1, :], ident[:], LAT[:, 0, :], ALU.subtract, R=[ident, LAT], W=[DE])
                        for lv in range(1, 7):
                            pst = nps()
                            P.mm(pst[:, 0:128], LAT[:, lv, :], DE[:, 0, :], R=[LAT, DE], W=[pst])
                            P.mm(pst[:, 128:256], LA[:, lv, :], DE[:, 1, :], R=[LA, DE], W=[pst])
                            P.cp("act", X[:], v3(pst[:, 0:256], 2), R=[pst], W=[X])
                            pst = nps()
                            P.mm(pst[:, 0:128], DE[:, 1, :], X[:, 0, :], R=[DE, X], W=[pst])
                            P.mm(pst[:, 128:256], DE[:, 0, :], X[:, 1, :], R=[DE, X], W=[pst])
                            P.tt("dve", DE[:], DE[:], v3(pst[:, 0:256], 2), ALU.subtract, R=[DE, pst], W=[DE])
                        pst = nps(); pst2 = nps()
                        P.mm(pst[:, 0:128], DE[:, 1, :], vb[:, hs], R=[DE, vb], W=[pst])
                        P.mm(pst2[:, 0:128], kbg[:, hs], DE[:, 1, :], R=[kbg, DE], W=[pst2])
                        P.cp("act", u0[:], pst[:, 0:128], R=[pst], W=[u0])
                        P.cp("dve", wT[:], pst2[:, 0:128], R=[pst2], W=[wT])
                        pst = nps()
                        P.mm(pst[:, 0:128], wT[:], S_[:, hs], R=[wT, S_], W=[pst])
                        P.tt("dve", u[:], u0[:], pst[:, 0:128], ALU.subtract, R=[u0, pst], W=[u])
                        pst = nps()
                        P.mm(pst[:, 0:128], TT[:, 3, :], S_[:, hs], start=True, stop=False, R=[TT, S_], W=[pst])
                        P.mm(pst[:, 0:128], AQ[:, 1, :], u[:], start=False, stop=True, R=[AQ, u], W=[pst])
                        P.cp("act", o_[:, hs], pst[:, 0:128], R=[pst], W=[o_])
                        pst = nps()
                        P.mm(pst[:, 0:128], kdd[:, hs], u[:], R=[kdd, u], W=[pst])
                        P.stt("dve", S_[:, hs], S_[:, hs], gl[:, h:h + 1], pst[:, 0:128], ALU.mult, ALU.add, R=[S_, gl, pst], W=[S_])
                P.tt("pool", t2[:], o_[:], o_[:], ALU.mult, R=[o_], W=[t2])
                P.op("dve", lambda e: e.reduce_sum(sm[:, 0:HB], v3(t2[:], HB), AX.X), R=[t2], W=[sm])
                P.ts("dve", sm[:, HB:2 * HB], sm[:, 0:HB], 1.0 / 128.0, EPS, ALU.mult, ALU.add, R=[sm], W=[sm])
                P.act(sm[:, 2 * HB:3 * HB], sm[:, HB:2 * HB], AF.Sqrt, R=[sm], W=[sm])
                P.op("dve", lambda e: e.reciprocal(sm[:, 3 * HB:4 * HB], sm[:, 2 * HB:3 * HB]), R=[sm], W=[sm])
                P.tt("dve", v3(o_[:], HB), v3(o_[:], HB), bc3(sm[:, 3 * HB:4 * HB], HB, 128), ALU.mult, R=[o_, sm], W=[o_])
                P.tt("pool", v3(o_[:], HB), v3(o_[:], HB), nw[:].rearrange("p (o d) -> p o d", o=1).to_broadcast([128, HB, 128]), ALU.mult,
                     R=[o_, nw], W=[o_])
                P.act(zt[:], zt[:], AF.Silu, R=[zt], W=[zt])
                P.tt("dve", o_[:], o_[:], zt[:], ALU.mult, R=[o_, zt], W=[o_])
                P.dma("act", mixed[t * 128:(t + 1) * 128, G:2 * G], o_[:], R=[o_], W=[(mixed, t, 1)])

    def stage_rope_once(l):
        if l != 0:
            return
        TWO_PI = 2.0 * math.pi
        with scope() as st:
            ii = P.sb("rii", [128, 96], I32, st)
            inv = P.sb("rinv", [128, 96], F32, st)
            P.op("pool", lambda e: e.iota(ii[:, 0:64], [[1, 64]], base=0, channel_multiplier=0), W=[ii])
            P.op("pool", lambda e: e.iota(ii[:, 64:96], [[1, 32]], base=0, channel_multiplier=0), W=[ii])
            P.cp("dve", inv[:], ii[:], R=[ii], W=[inv])
            P.act(inv[:, 0:64], inv[:, 0:64], AF.Exp, scale=-2.0 * math.log(ROPE_THETA) / 128.0, R=[inv], W=[inv])
            P.act(inv[:, 64:96], inv[:, 64:96], AF.Exp, scale=-2.0 * math.log(ROPE_THETA) / 64.0, R=[inv], W=[inv])
            pi_ = P.sb("rpi", [128, 1], I32, st); pf = P.sb("rpf", [128, 1], F32, st)
            y = P.sb("ry", [128, 96], F32, st); ki = P.sb("rki", [128, 96], I32, st); kf = P.sb("rkf", [128, 96], F32, st)
            m1 = P.sb("rm1", [128, 96], F32, st)
            tab = P.sb("rtab", [128, 192], F32, st)
            for t in range(NT):
                P.dma("sp", pi_[:], pos_in[t * 128:(t + 1) * 128, :], W=[pi_])
                P.cp("dve", pf[:], pi_[:], R=[pi_], W=[pf])
                for which, shift in ((0, 0.25), (1, 0.0)):
                    P.ts("dve", y[:], inv[:], pf[:, 0:1], 1.0 / TWO_PI, ALU.mult, ALU.mult, R=[inv, pf], W=[y])
                    if shift:
                        P.ts("dve", y[:], y[:], shift, None, ALU.add, R=[y], W=[y])
                    P.cp("dve", ki[:], y[:], R=[y], W=[ki])
                    P.cp("dve", kf[:], ki[:], R=[ki], W=[kf])
                    P.tt("dve", y[:], y[:], kf[:], ALU.subtract, R=[y, kf], W=[y])
                    P.ts("dve", m1[:], y[:], 0.5, None, ALU.is_gt, R=[y], W=[m1])
                    P.tt("dve", y[:], y[:], m1[:], ALU.subtract, R=[y, m1], W=[y])
                    P.ts("dve", m1[:], y[:], -0.5, None, ALU.is_lt, R=[y], W=[m1])
                    P.tt("dve", y[:], y[:], m1[:], ALU.add, R=[y, m1], W=[y])
                    P.act(tab[:, which * 64:which * 64 + 64], y[:, 0:64], AF.Sin, scale=TWO_PI, R=[y], W=[tab])
                    P.act(tab[:, 128 + which * 32:160 + which * 32], y[:, 64:96], AF.Sin, scale=TWO_PI, R=[y], W=[tab])
                P.dma("act", rope[t * 128:(t + 1) * 128, :], tab[:], R=[tab], W=[(rope, t)])

    def rope_apply(dst, src, cs, sn, H, half, t1, t2_):
        def bc(ap):
            return ap.rearrange("p (o d) -> p o d", o=1).to_broadcast([128, H, half])
        s4 = src.rearrange("p (h two d) -> p h two d", h=H, two=2)
        d4 = dst.rearrange("p (h two d) -> p h two d", h=H, two=2)
        a1 = t1.rearrange("p (h d) -> p h d", h=H); a2 = t2_.rearrange("p (h d) -> p h d", h=H)
        return s4, d4, a1, a2, bc(cs), bc(sn)

    def stage_attn(l):
        NIT = 26
        psmod[0] = 7
        p_o = PS[7]
        with scope() as st:
            kT = P.sb("akT", [128, HA, S], BF16, st)
            vA = P.sb("avA", [128, NT, HA, 128], BF16, st)
            kiT = P.sb("akiT", [64, S], BF16, st)
            NL = P.sb("aNL", [128, 128], F32, st); P.dma("sp", NL[:], CS["c_NL"][:, :], W=[NL])
            lng = P.sb("alng", [128, 64], F32, st); lnb = P.sb("alnb", [128, 64], F32, st)
            bcast_row(lng, Wt["idx_kn_g"][l:l + 1, :], 64); bcast_row(lnb, Wt["idx_kn_b"][l:l + 1, :], 64)
            rp = [P.sb("arp%d" % i, [128, 192], F32, st) for i in range(2)]
            xa_ = P.sb("axa", [128, G], F32, st); xr = P.sb("axr", [128, G], F32, st)
            t1 = P.sb("at1", [128, 512], F32, st); t2_ = P.sb("at2", [128, 512], F32, st)
            qi = P.sb("aqi", [128, 1024], F32, st)
            vt = qi
            kit = P.sb("akit", [128, 64], F32, st); kir = P.sb("akir", [128, 64], F32, st)
            sm = P.sb("asm", [128, 16], F32, st)

            def do_rope(dst, src, H, half, cs, sn, W_):
                s4, d4, a1, a2, cb, sb_ = rope_apply(dst, src, cs, sn, H, half, t1[:, 0:H * half], t2_[:, 0:H * half])
                P.tt("dve", a1, s4[:, :, 0, :], cb, ALU.mult, R=[W_[0], W_[2]], W=[t1])
                P.tt("pool", a2, s4[:, :, 1, :], sb_, ALU.mult, R=[W_[0], W_[2]], W=[t2_])
                P.tt("dve", d4[:, :, 0, :], a1, a2, ALU.subtract, R=[t1, t2_], W=[W_[1]])
                P.tt("dve", a1, s4[:, :, 1, :], cb, ALU.mult, R=[W_[0], W_[2]], W=[t1])
                P.tt("pool", a2, s4[:, :, 0, :], sb_, ALU.mult, R=[W_[0], W_[2]], W=[t2_])
                P.tt("dve", d4[:, :, 1, :], a1, a2, ALU.add, R=[t1, t2_], W=[W_[1]])

            for t in range(NT):
                r_ = rp[t % 2]
                P.dma("sp", r_[:], rope[t * 128:(t + 1) * 128, :], R=[(rope, t)], W=[r_])
                ldp(xa_, t, "a_k"); ldp(vt, t, "a_v"); ldp(kit, t, "a_ki")
                do_rope(xr[:], xa_[:], HA, 64, r_[:, 0:64], r_[:, 64:128], (xa_, xr, r_))
                for h0 in range(0, HA, 4):
                    nh = min(4, HA - h0)
                    pst = nps()
                    for kk in range(nh):
                        P.tr(pst[:, kk * 128:(kk + 1) * 128], xr[:, (h0 + kk) * 128:(h0 + kk + 1) * 128], ident[:], R=[xr, ident], W=[pst])
                    P.cp("act", kT[:, h0:h0 + nh, t * 128:(t + 1) * 128], v3(pst[:, 0:nh * 128], nh), R=[pst], W=[(kT, t)])
                P.cp("pool", vA[:, t, :, :], v3(vt[:, 0:G], HA), R=[vt], W=[(vA, t)])
                P.act(kir[:], kit[:], AF.Identity, accum=sm[:, 0:1], R=[kit], W=[kir, sm])
                P.act(kir[:], kit[:], AF.Square, accum=sm[:, 1:2], R=[kit], W=[kir, sm])
                P.ts("dve", sm[:, 2:3], sm[:, 0:1], 1.0 / 64.0, None, ALU.mult, R=[sm], W=[sm])
                P.tt("dve", sm[:, 3:4], sm[:, 2:3], sm[:, 2:3], ALU.mult, R=[sm], W=[sm])
                P.stt("dve", sm[:, 4:5], sm[:, 1:2], 1.0 / 64.0, sm[:, 3:4], ALU.mult, ALU.subtract, R=[sm], W=[sm])
                P.ts("dve", sm[:, 4:5], sm[:, 4:5], EPS, None, ALU.add, R=[sm], W=[sm])
                P.act(sm[:, 5:6], sm[:, 4:5], AF.Sqrt, R=[sm], W=[sm])
                P.op("dve", lambda e: e.reciprocal(sm[:, 6:7], sm[:, 5:6]), R=[sm], W=[sm])
                P.ts("dve", kit[:], kit[:], sm[:, 2:3], sm[:, 6:7], ALU.subtract, ALU.mult, R=[kit, sm], W=[kit])
                P.tt("dve", kit[:], kit[:], lng[:], ALU.mult, R=[kit, lng], W=[kit])
                P.tt("dve", kit[:], kit[:], lnb[:], ALU.add, R=[kit, lnb], W=[kit])
                do_rope(kir[:], kit[:], 1, 32, r_[:, 128:160], r_[:, 160:192], (kit, kir, r_))
                pst = nps()
                P.tr(pst[0:64, 0:128], kir[:], ident[:], R=[kir, ident], W=[pst])
                P.cp("act", kiT[:, t * 128:(t + 1) * 128], pst[0:64, 0:128], R=[pst], W=[(kiT, t)])
            if BIS == 6:
                NTQ = 0
            else:
                NTQ = NT
            qT = P.sb("aqT", [128, HA, 128], BF16, st)
            qir = P.sb("aqir", [128, 1024], F32, st)
            qiT = P.sb("aqiT", [64, 16, 128], BF16, st)
            wi = P.sb("awi", [128, 16], F32, st)
            acc = P.sb("aacc", [128, S], F32, st)
            maskb = P.sb("amask", [128, S], BF16, st)
            rl = [P.sb("arl%d" % i, [128, 512], F32, st) for i in range(2)]
            bs = P.sb("abs", [128, 8], F32, st)
            mxc = P.sb("amxc", [128, 16], F32, st)
            pT = [P.sb("apT%d" % i, [128, 4, 128], BF16, st) for i in range(2)]
            o_ = xa_
            for qb in range(NTQ):
                Sk = (qb + 1) * 128
                r_ = rp[qb % 2]
                P.dma("sp", r_[:], rope[qb * 128:(qb + 1) * 128, :], R=[(rope, qb)], W=[r_])
                ldp(xa_, qb, "a_q"); ldp(qi, qb, "a_qi"); ldp(wi, qb, "a_wi")
                do_rope(xr[:], xa_[:], HA, 64, r_[:, 0:64], r_[:, 64:128], (xa_, xr, r_))
                for h0 in range(0, HA, 4):
                    nh = min(4, HA - h0)
                    pst = nps()
                    for kk in range(nh):
                        P.tr(pst[:, kk * 128:(kk + 1) * 128], xr[:, (h0 + kk) * 128:(h0 + kk + 1) * 128], ident[:], R=[xr, ident], W=[pst])
                    P.act(qT[:, h0:h0 + nh, :], v3(pst[:, 0:nh * 128], nh), AF.Copy, scale=128.0 ** -0.5, R=[pst], W=[qT])
                do_rope(qir[:], qi[:], 16, 32, r_[:, 128:160], r_[:, 160:192], (qi, qir, r_))
                for h0 in range(0, 16, 4):
                    pst = nps()
                    for kk in range(4):
                        P.tr(pst[0:64, kk * 128:(kk + 1) * 128], qir[:, (h0 + kk) * 64:(h0 + kk + 1) * 64], ident[:], R=[qir, ident], W=[pst])
                    P.cp("act", qiT[:, h0:h0 + 4, :], v3(pst[0:64, 0:512], 4), R=[pst], W=[qiT])
                P.ts("dve", wi[:], wi[:], 1.0 / 32.0, None, ALU.mult, R=[wi], W=[wi])
                k_ = 0
                for c0 in range(0, Sk, 512):
                    w = min(512, Sk - c0)
                    for hi in range(16):
                        pst = nps()
                        P.mm(pst[:, 0:w], qiT[:, hi, :], kiT[:, c0:c0 + w], R=[qiT, kiT], W=[pst])
                        r2 = rl[k_ % 2]; k_ += 1
                        P.act(r2[:, 0:w], pst[:, 0:w], AF.Relu, R=[pst], W=[r2])
                        if hi == 0:
                            P.ts("dve", acc[:, c0:c0 + w], r2[:, 0:w], wi[:, 0:1], None, ALU.mult, R=[r2, wi], W=[acc])
                        else:
                            P.stt("dve", acc[:, c0:c0 + w], r2[:, 0:w], wi[:, hi:hi + 1], acc[:, c0:c0 + w], ALU.mult, ALU.add,
                                  R=[r2, wi, acc], W=[acc])
                if BIS == 7:
                    continue
                P.op("dve", lambda e, Sk=Sk: e.reduce_max(bs[:, 0:1], acc[:, 0:Sk], AX.X), R=[acc], W=[bs])
                P.op("dve", lambda e, Sk=Sk: e.tensor_reduce(bs[:, 1:2], acc[:, 0:Sk], AX.X, ALU.min), R=[acc], W=[bs])
                P.tt("dve", acc[:, Sk - 128:Sk], acc[:, Sk - 128:Sk], NL[:], ALU.add, R=[acc, NL], W=[acc])
                P.tt("dve", bs[:, 2:3], bs[:, 0:1], bs[:, 1:2], ALU.subtract, R=[bs], W=[bs])
                P.ts("dve", bs[:, 2:3], bs[:, 2:3], 1.0001, 1e-6, ALU.mult, ALU.add, R=[bs], W=[bs])
                P.ts("dve", bs[:, 3:4], bs[:, 1:2], -1e-6, None, ALU.add, R=[bs], W=[bs])
                for it in range(1, NIT + 1):
                    sc = 2.0 ** -it
                    P.stt("dve", bs[:, 4:5], bs[:, 2:3], sc, bs[:, 3:4], ALU.mult, ALU.add, R=[bs], W=[bs])
                    P.ts("dve", maskb[:, 0:Sk], acc[:, 0:Sk], bs[:, 4:5], None, ALU.is_ge, ALU.add, accum=bs[:, 5:6],
                         R=[acc, bs], W=[maskb, bs])
                    P.ts("dve", bs[:, 6:7], bs[:, 5:6], KSEL - 0.5, sc, ALU.is_ge, ALU.mult, R=[bs], W=[bs])
                    P.stt("dve", bs[:, 3:4], bs[:, 6:7], bs[:, 2:3], bs[:, 3:4], ALU.mult, ALU.add, R=[bs], W=[bs])
                P.ts("dve", maskb[:, 0:Sk], acc[:, 0:Sk], bs[:, 3:4], None, ALU.is_ge, R=[acc, bs], W=[maskb])
                if BIS == 8:
                    continue
                for h in range(HA):
                    nch = 0
                    for c0 in range(0, Sk, 512):
                        w = min(512, Sk - c0)
                        pst = nps()
                        P.mm(pst[:, 0:w], qT[:, h, :], kT[:, h, c0:c0 + w], R=[qT, kT], W=[pst])
                        P.ts("dve", acc[:, c0:c0 + w], pst[:, 0:w], 1.0, None, ALU.mult, ALU.max, accum=mxc[:, nch:nch + 1],
                             R=[pst], W=[acc, mxc])
                        nch += 1
                    P.op("dve", lambda e, nch=nch: e.reduce_max(bs[:, 7:8], mxc[:, 0:nch], AX.X), R=[mxc], W=[bs])
                    P.ts("dve", bs[:, 7:8], bs[:, 7:8], -1.0, None, ALU.mult, R=[bs], W=[bs])
                    P.act(acc[:, 0:Sk], acc[:, 0:Sk], AF.Exp, bias=bs[:, 7:8], R=[acc, bs], W=[acc])
                    P.tt("pool", acc[:, 0:Sk], acc[:, 0:Sk], maskb[:, 0:Sk], ALU.mult, R=[acc, maskb], W=[acc])
                    P.op("dve", lambda e, Sk=Sk: e.reduce_sum(bs[:, 5:6], acc[:, 0:Sk], AX.X), R=[acc], W=[bs])
                    nj = Sk // 128
                    for j0 in range(0, nj, 4):
                        nn = min(4, nj - j0)
                        pst = nps()
                        for jj in range(nn):
                            P.tr(pst[:, jj * 128:(jj + 1) * 128], acc[:, (j0 + jj) * 128:(j0 + jj + 1) * 128], ident[:], R=[acc, ident], W=[pst])
                        pt = pT[(j0 // 4) % 2]
                        P.cp("act", pt[:, 0:nn, :], v3(pst[:, 0:nn * 128], nn), R=[pst], W=[pt])
                        for jj in range(nn):
                            j = j0 + jj
                            P.mm(p_o[:, 0:128], pt[:, jj, :], vA[:, j, h, :], start=(j == 0), stop=(j == nj - 1), R=[pt, vA], W=[p_o])
                    P.op("dve", lambda e: e.reciprocal(bs[:, 6:7], bs[:, 5:6]), R=[bs], W=[bs])
                    P.ts("dve", o_[:, h * 128:(h + 1) * 128], p_o[:, 0:128], bs[:, 6:7], None, ALU.mult, R=[p_o, bs], W=[o_])
                P.dma("act", mixed[qb * 128:(qb + 1) * 128, 0:G], o_[:], R=[o_], W=[(mixed, qb, 0)])
        psmod[0] = 8

    def ln_pass(src, dst, gname, bname, l, st, extra=None):
        P.barrier()
        gbc = P.sb("lng", [128, DM], F32, st); bbc = P.sb("lnb", [128, DM], F32, st)
        bcast_row(gbc, Wt[gname][l:l + 1, :], DM); bcast_row(bbc, Wt[bname][l:l + 1, :], DM)
        xt2 = [P.sb("lnx%d" % i, [128, DM], F32, st) for i in range(2)]
        junk = P.sb("lnj", [128, DM], F32, st)
        sm = P.sb("lnsm", [128, 8], F32, st)
        for t in range(NT):
            xt = xt2[t % 2]
            P.dma("sp", xt[:], src[t * 128:(t + 1) * 128, :], R=[(src, t)], W=[xt])
            P.act(junk[:], xt[:], AF.Identity, accum=sm[:, 0:1], R=[xt], W=[junk, (sm, 0)])
            P.act(junk[:], xt[:], AF.Square, accum=sm[:, 1:2], R=[xt], W=[junk, (sm, 1)])
            P.ts("dve", sm[:, 2:3], sm[:, 0:1], 1.0 / DM, None, ALU.mult, R=[(sm, 0)], W=[(sm, 2)])
            P.tt("dve", sm[:, 3:4], sm[:, 2:3], sm[:, 2:3], ALU.mult, R=[(sm, 2)], W=[(sm, 3)])
            P.stt("dve", sm[:, 4:5], sm[:, 1:2], 1.0 / DM, sm[:, 3:4], ALU.mult, ALU.subtract, R=[(sm, 1), (sm, 3)], W=[(sm, 4)])
            P.ts("dve", sm[:, 4:5], sm[:, 4:5], EPS, None, ALU.add, R=[(sm, 4)], W=[(sm, 4)])
            P.act(sm[:, 5:6], sm[:, 4:5], AF.Sqrt, R=[(sm, 4)], W=[(sm, 5)])
            P.op("dve", lambda e: e.reciprocal(sm[:, 6:7], sm[:, 5:6]), R=[(sm, 5)], W=[(sm, 6)])
            P.ts("dve", xt[:], xt[:], sm[:, 2:3], sm[:, 6:7], ALU.subtract, ALU.mult, R=[xt, (sm, 2), (sm, 6)], W=[xt])
            P.tt("pool", xt[:], xt[:], gbc[:], ALU.mult, R=[xt, gbc], W=[xt])
            P.tt("dve", xt[:], xt[:], bbc[:], ALU.add, R=[xt, bbc], W=[xt])
            P.dma("act", dst[t * 128:(t + 1) * 128, :], xt[:], R=[xt], W=[(dst, t)])
            if extra is not None:
                extra(t, xt)

    def resid_epilogue(resid, gate_part, dst, st):
        gbc = P.sb("gbc", [128, DM], F32, st)
        bcast_row(gbc, modv.t.ap()[0:1, gate_part * DM:(gate_part + 1) * DM], DM)
        P.ts("dve", gbc[:], gbc[:], 1.0, None, ALU.add, R=[gbc], W=[gbc])
        obs = [P.sb("rob%d" % i, [128, 512], F32, st) for i in range(2)]
        xts = [P.sb("rxt%d" % i, [128, 512], F32, st) for i in range(2)]
        cnt = [0]

        def ep(t0, n0, w, pst, R):
            ob = obs[cnt[0] % 2]; xt = xts[cnt[0] % 2]; cnt[0] += 1
            P.dma("sp", xt[:, 0:w], resid[t0:t0 + 128, n0:n0 + w], R=[(resid, t0 // 128)], W=[xt])
            P.tt("dve", ob[:, 0:w], pst[:, 0:w], gbc[:, n0:n0 + w], ALU.mult, R=R + [gbc], W=[ob])
            P.stt("dve", ob[:, 0:w], xt[:, 0:w], ALPHA, ob[:, 0:w], ALU.mult, ALU.add, R=[xt, ob], W=[ob])
            P.dma("act", dst[t0:t0 + 128, n0:n0 + w], ob[:, 0:w], R=[ob], W=[(dst, t0 // 128)])
        return ep

    def stage_out(l, xin, xo):
        NE = N_EXP * EXP_FF
        with scope() as st:
            wscr, NTL, nt = cast_weights(lambda n0, w: Wt["w_out"][l][:, n0:n0 + w], DM, DM, "wout_bf%d" % l)
            gemm(mixed, DM, wscr, NTL, nt, DM, resid_epilogue(xin, 2, x1, st), None, name="wo")
        gates_d = dscr("gates%d" % l, [S, 32])
        with scope() as st:
            s2 = mod_cols(st, 4, True); sh2 = mod_cols(st, 3, False)
            Wr = P.sb("Wr", [128, KD, 36], F32, st)
            P.dma("sp", Wr[:, :, 0:4], Wt["router_g_w"][l].rearrange("(k p) g -> p k g", p=128), W=[Wr])
            P.dma("sp", Wr[:, :, 4:36], Wt["router_e_w"][l].rearrange("(k p) g -> p k g", p=128), W=[Wr])
            rb = P.sb("rb", [128, 36], F32, st)
            P.dma("sp", rb[:, 0:4], Wt["router_g_b"][l:l + 1, :].to_broadcast([128, 4]), W=[rb])
            P.dma("sp", rb[:, 4:36], Wt["router_e_b"][l:l + 1, :].to_broadcast([128, 32]), W=[rb])
            hT = P.sb("rhT", [128, KD, 128], F32, st)
            lg = P.sb("lg", [128, 36], F32, st)
            w_ = P.sb("rw_", [128, 96], F32, st)
            gt = P.sb("gt", [128, 32], F32, st)

            def router(t, xt):
                for kk in range(KD):
                    if kk % 4 == 0:
                        pst = nps()
                    P.tr(pst[:, (kk % 4) * 128:(kk % 4 + 1) * 128], xt[:, kk * 128:(kk + 1) * 128], ident[:], R=[xt, ident], W=[pst])
                    P.act(hT[:, kk, :], pst[:, (kk % 4) * 128:(kk % 4 + 1) * 128], AF.Identity, scale=s2[:, kk:kk + 1],
                          bias=sh2[:, kk:kk + 1], R=[pst, s2, sh2], W=[hT])
                pst = nps()
                for kk in range(KD):
                    P.mm(pst[:, 0:36], hT[:, kk, :], Wr[:, kk, :], start=(kk == 0), stop=(kk == KD - 1), R=[hT, Wr], W=[pst])
                P.tt("dve", lg[:], pst[:, 0:36], rb[:], ALU.add, R=[pst, rb], W=[lg])
                P.op("dve", lambda e: e.reduce_max(w_[:, 0:1], lg[:, 0:4], AX.X), R=[lg], W=[w_])
                P.ts("dve", w_[:, 4:8], lg[:, 0:4], w_[:, 0:1], None, ALU.is_equal, R=[lg, w_], W=[w_])
                P.ts("dve", w_[:, 1:2], w_[:, 0:1], -1.0, None, ALU.mult, R=[w_], W=[w_])
                P.act(w_[:, 8:12], lg[:, 0:4], AF.Exp, bias=w_[:, 1:2], accum=w_[:, 2:3], R=[lg, w_], W=[w_])
                P.op("dve", lambda e: e.reciprocal(w_[:, 3:4], w_[:, 2:3]), R=[w_], W=[w_])
                P.ts("dve", w_[:, 12:16], w_[:, 4:8], -NEG, NEG, ALU.mult, ALU.add, R=[w_], W=[w_])
                P.tt("dve", w_[:, 16:48].rearrange("p (g e) -> p g e", g=4), lg[:, 4:36].rearrange("p (g e) -> p g e", g=4),
                     w_[:, 12:16].to_broadcast([128, 4, 8]) if False else w_[:, 12:16].rearrange("p (g o) -> p g o", o=1).to_broadcast([128, 4, 8]),
                     ALU.add, R=[lg, w_], W=[w_])
                P.op("dve", lambda e: e.reduce_max(w_[:, 48:49], w_[:, 16:48], AX.X), R=[w_], W=[w_])
                P.ts("dve", w_[:, 56:88], w_[:, 16:48], w_[:, 48:49], None, ALU.is_equal, R=[w_], W=[w_])
                P.stt("dve", gt[:], w_[:, 56:88], NEG, w_[:, 16:48], ALU.mult, ALU.add, R=[w_], W=[gt])
                P.op("dve", lambda e: e.reduce_max(w_[:, 49:50], gt[:], AX.X), R=[gt], W=[w_])
                P.ts("dve", gt[:], gt[:], w_[:, 49:50], None, ALU.is_equal, R=[gt, w_], W=[gt])
                P.tt("dve", w_[:, 50:51], w_[:, 48:49], w_[:, 49:50], ALU.subtract, R=[w_], W=[w_])
                P.act(w_[:, 51:52], w_[:, 50:51], AF.Sigmoid, R=[w_], W=[w_])
                P.ts("dve", w_[:, 52:53], w_[:, 51:52], -1.0, 1.0, ALU.mult, ALU.add, R=[w_], W=[w_])
                P.tt("dve", w_[:, 51:52], w_[:, 51:52], w_[:, 3:4], ALU.mult, R=[w_], W=[w_])
                P.tt("dve", w_[:, 52:53], w_[:, 52:53], w_[:, 3:4], ALU.mult, R=[w_], W=[w_])
                P.ts("dve", gt[:], gt[:], w_[:, 52:53], None, ALU.mult, R=[gt, w_], W=[gt])
                P.stt("dve", gt[:], w_[:, 56:88], w_[:, 51:52], gt[:], ALU.mult, ALU.add, R=[gt, w_], W=[gt])
                P.dma("act", gates_d[t * 128:(t + 1) * 128, :], gt[:], R=[gt], W=[(gates_d, t)])
            ln_pass(x1, x1, "ln1_g", "ln1_b", l, st, extra=router)
        for wn, dst in (("exp_w_gate", hg_d), ("exp_w_up", hu_d)):
            with scope() as st:
                scr = dscr("%s_bf%d" % (wn, l), [NE // 512, 128, KD, 512], BF16)
                for t in range(NE // 512):
                    for e_ in range(2):
                        castq_n[0] += 1
                        P.dma("pool", scr[t][:, :, e_ * 256:(e_ + 1) * 256],
                              Wt[wn][l, 2 * t + e_].rearrange("(kd p) f -> p kd f", p=128), W=[(scr, t), (castq, castq_n[0] % 2)])
                s2 = mod_cols(st, 4, True); sh2 = mod_cols(st, 3, False)

                def prologue(kk, out_ap, ps_ap, R, W, s2=s2, sh2=sh2):
                    P.act(out_ap, ps_ap, AF.Identity, scale=s2[:, kk:kk + 1], bias=sh2[:, kk:kk + 1], R=R + [s2, sh2], W=W)
                gemm(x1, DM, scr, 512, NE // 512, NE, store_epilogue(dst, st), prologue, name="ex")
        P.barrier()
        with scope() as st:
            CW = 2048
            a2 = [P.sb("ea%d" % i, [128, CW], F32, st) for i in range(2)]
            b2 = [P.sb("eb%d" % i, [128, CW], F32, st) for i in range(2)]
            g2 = [P.sb("eg%d" % i, [128, 32], F32, st) for i in range(2)]
            k = 0
            for t in range(NT):
                gtl = g2[t % 2]
                P.dma("sp", gtl[:], gates_d[t * 128:(t + 1) * 128, :], R=[(gates_d, t)], W=[gtl])
                for c0 in range(0, NE, CW):
                    a_, b_ = a2[k % 2], b2[k % 2]; k += 1
                    P.dma("sp", a_[:], hg_d[t * 128:(t + 1) * 128, c0:c0 + CW], R=[hg_d], W=[a_])
                    P.dma("sp", b_[:], hu_d[t * 128:(t + 1) * 128, c0:c0 + CW], R=[hu_d], W=[b_])
                    P.act(a_[:], a_[:], AF.Silu, R=[a_], W=[a_])
                    P.tt("dve", a_[:], a_[:], b_[:], ALU.mult, R=[a_, b_], W=[a_])
                    for e_ in range(CW // 256):
                        ee = c0 // 256 + e_
                        P.ts("pool", a_[:, e_ * 256:(e_ + 1) * 256], a_[:, e_ * 256:(e_ + 1) * 256], gtl[:, ee:ee + 1], None, ALU.mult,
                             R=[a_, gtl], W=[a_])
                    P.dma("act", hg_d[t * 128:(t + 1) * 128, c0:c0 + CW], a_[:], R=[a_], W=[hg_d])
        with scope() as st:
            wscr, NTL, nt = cast_weights(lambda n0, w: Wt["exp_w_down"][l].rearrange("e f d -> (e f) d")[:, n0:n0 + w], NE, DM, "wdn_bf%d" % l)
            gemm(hg_d, NE, wscr, NTL, nt, DM, resid_epilogue(x1, 5, xo, st), None, name="dn")
        with scope() as st:
            ln_pass(xo, xo, "ln2_g", "ln2_b", l, st)

    xin = x_in
    final = []
    for l in range(NL):
        stage_mod(l)
        if STAGES <= 0:
            break
        stage_proj(l, xin)
        if STAGES <= 1:
            break
        if STAGES == 2:
            if not debug:
                with scope() as st:
                    zt = P.sb("zt", [128, DM], F32, st)
                    P.memset("dve", zt[:], 0.0, W=[zt])
                    for t in range(NT):
                        P.dma("sp", mixed[t * 128:(t + 1) * 128, :], zt[:], R=[zt], W=[(mixed, t)])
            xo = y_out
            stage_out(l, xin, xo)
            break
        if "a" in MIX:
            stage_rope_once(l)
            if BIS != 5:
                stage_attn(l)
        if "b" in MIX:
            stage_gdn(l)
        if "c" in MIX:
            stage_gla(l)
        if "d" in MIX:
            stage_ssd(l)
        xo = y_out if l == NL - 1 else (xa if l % 2 == 0 else xb)
        stage_out(l, xin, xo)
        xin = xo
    P.barrier()
    toks = []
    for tl in (y_out, proj, mixed, x1, modv):
        for s, stt_ in tl.st.items():
            if stt_[0] is not None:
                toks.append(stt_[0])
    P.finish(toks, "sp")
    P.emit()
    return nc, c


_CACHE = {}


def kernel(**inputs):
    DM, S, DEPTH, B = 4096, 4096, 4, 2
    if "nc" not in _CACHE:
        _CACHE["nc"] = build(DM, S, 1, DEPTH, debug=False)[0]
    nc = _CACHE["nc"]
    consts = host_consts()
    x = np.ascontiguousarray(np.asarray(inputs["x"], np.float32))
    c = np.asarray(inputs["c"], np.float32)
    pos = np.asarray(inputs["positions"]).astype(np.int32)
    for l in range(DEPTH):
        maps = []
        for b in range(B):
            m = {"x": np.ascontiguousarray(x[b]), "c": np.ascontiguousarray(c[b:b + 1]),
                 "pos": np.ascontiguousarray(pos[b].reshape(S, 1))}
            for n in WEIGHTS:
                m[n] = np.ascontiguousarray(np.asarray(inputs[n])[l:l + 1])
            m.update(consts)
            maps.append(m)
        res = run_bass_kernel_spmd(nc, maps, core_ids=list(range(B)))
        x = np.stack([np.asarray(res.results[b]["y"], np.float32) for b in range(B)], axis=0)
    return x
```

```python
import math
import numpy as np
import concourse.bass as bass
import concourse.mybir as mybir
from concourse.bass_utils import run_bass_kernel_spmd
from contextlib import ExitStack

F32 = mybir.dt.float32
BF16 = mybir.dt.bfloat16
I32 = mybir.dt.int32
ALU = mybir.AluOpType
AF = mybir.ActivationFunctionType
AX = mybir.AxisListType
ENGS = ("pe", "act", "dve", "pool", "sp")
EPOCH = 30000
NEG = -1.0e30


class T:
    def __init__(self, name, h):
        self.name = name
        self.t = h
        self.st = {}

    def __getitem__(self, k):
        return self.t[k]


class Prog:
    def __init__(self, nc, n_dma_slots=32):
        self.nc = nc
        self.es = ExitStack()
        self.ops = {e: [] for e in ENGS}
        self.cnt = {e: 0 for e in ENGS}
        self.known = {e: {} for e in ENGS}
        self.sem = {}
        self.dma_slots = []
        for i in range(n_dma_slots):
            k = ("d", i)
            self.sem[k] = self.es.enter_context(nc.semaphore("d%d" % i))
            self.dma_slots.append([k, 0])
        self.dma_rr = 0
        self.n_t = 0
        self.n_ops = 0

    def _semkey(self, eng):
        k = (eng, self.cnt[eng] // EPOCH)
        if k not in self.sem:
            self.sem[k] = self.es.enter_context(self.nc.semaphore("s_%s_%d" % k))
        return k

    def sb(self, name, shape, dt=F32, stack=None):
        self.n_t += 1
        h = (stack or self.es).enter_context(self.nc.sbuf_tensor("%s_%d" % (name, self.n_t), list(shape), dt))
        return T(name, h)

    def ps(self, name, shape, dt=F32, stack=None):
        self.n_t += 1
        h = (stack or self.es).enter_context(self.nc.psum_tensor("%s_%d" % (name, self.n_t), list(shape), dt))
        t = T(name, h)
        t.psum = True
        return t

    def _fix(self, R, W):
        R2, W2 = [], []
        for a in R:
            t = a if isinstance(a, T) else a[0]
            if getattr(t, "psum", False):
                W2.append(t)
            else:
                R2.append(a)
        for a in W:
            t = a if isinstance(a, T) else a[0]
            W2.append(t if getattr(t, "psum", False) else a)
        return R2, W2

    @staticmethod
    def _norm(acc):
        out = []
        for a in acc:
            if isinstance(a, T):
                out.append((a, (None,)))
            else:
                out.append((a[0], tuple(a[1:]) if len(a) > 1 else (None,)))
        return out

    @staticmethod
    def _subs(t, subs):
        if None in subs:
            return list(t.st.keys()) + ([None] if None not in t.st else [])
        return list(subs) + [None]

    def _deps(self, reads, writes):
        deps = []
        for t, subs in self._norm(reads):
            for s in self._subs(t, subs):
                st = t.st.get(s)
                if st and st[0] is not None:
                    deps.append(st[0])
        for t, subs in self._norm(writes):
            for s in self._subs(t, subs):
                st = t.st.get(s)
                if st:
                    if st[0] is not None:
                        deps.append(st[0])
                    deps.extend(st[1].items())
        return deps

    def _commit(self, reads, writes, tok):
        for t, subs in self._norm(reads):
            for s in subs:
                st = t.st.setdefault(s, [None, {}])
                if st[1].get(tok[0], 0) < tok[1]:
                    st[1][tok[0]] = tok[1]
        for t, subs in self._norm(writes):
            if None in subs:
                t.st = {None: [tok, {}]}
            else:
                for s in subs:
                    t.st[s] = [tok, {}]

    def _waits(self, eng, deps):
        kn = self.known[eng]
        need = {}
        for k, v in deps:
            if k[0] == "pe" and eng == "pe":
                continue
            if kn.get(k, 0) < v:
                need[k] = max(need.get(k, 0), v)
        for k, v in need.items():
            kn[k] = v
        return list(need.items())

    def op(self, eng, fn, R=(), W=()):
        R, W = self._fix(R, W)
        deps = self._deps(R, W)
        waits = self._waits(eng, deps)
        k = self._semkey(eng)
        self.cnt[eng] += 1
        tok = (k, self.cnt[eng] - k[1] * EPOCH)
        self.ops[eng].append((waits, fn, (k, 1)))
        self._commit(R, W, tok)
        self.n_ops += 1
        return tok

    def dma(self, eng, out, in_, R=(), W=(), **kw):
        R, W = self._fix(R, W)
        deps = self._deps(R, W)
        slot = self.dma_slots[self.dma_rr]
        self.dma_rr = (self.dma_rr + 1) % len(self.dma_slots)
        if slot[1] > 0:
            deps.append((slot[0], slot[1]))
        waits = self._waits(eng, deps)
        slot[1] += 16
        tok = (slot[0], slot[1])

        def fn(e, out=out, in_=in_, kw=kw):
            return e.dma_start(out=out, in_=in_, **kw)
        self.ops[eng].append((waits, fn, (slot[0], 16)))
        self._commit(R, W, tok)
        self.n_ops += 1
        return tok

    def barrier(self):
        toks = []
        for e in ENGS:
            if self.cnt[e] > 0:
                k = (e, (self.cnt[e] - 1) // EPOCH)
                toks.append((k, self.cnt[e] - k[1] * EPOCH))
        for slot in self.dma_slots:
            if slot[1] > 0:
                toks.append((slot[0], slot[1]))
        for e in ENGS:
            w = self._waits(e, toks)
            if w:
                self.ops[e].append((w, None, None))

    def finish(self, toks, eng="sp"):
        self.ops[eng].append((self._waits(eng, list(toks)), None, None))

    def emit(self):
        nc = self.nc
        engmap = {"pe": "tensor", "act": "scalar", "dve": "vector", "pool": "gpsimd", "sp": "sync"}
        with nc.Block() as block:
            for e in ENGS:
                ops = self.ops[e]
                if not ops:
                    continue

                def body(engine, ops=ops):
                    for waits, fn, inc in ops:
                        for k, v in waits:
                            engine.wait_ge(self.sem[k], v)
                        if fn is not None:
                            fn(engine).then_inc(self.sem[inc[0]], inc[1])
                getattr(block, engmap[e])(body)
        self.es.close()

    def mm(self, out, lhsT, rhs, start=True, stop=True, R=(), W=()):
        return self.op("pe", lambda e: e.matmul(out, lhsT, rhs, start=start, stop=stop), R, W)

    def tr(self, out, in_, ident, R=(), W=()):
        return self.op("pe", lambda e: e.transpose(out, in_, ident), R, W)

    def act(self, out, in_, func, scale=1.0, bias=0.0, accum=None, R=(), W=()):
        if accum is None:
            return self.op("act", lambda e: e.activation(out, in_, func, scale=scale, bias=bias), R, W)
        return self.op("act", lambda e: e.activation(out, in_, func, scale=scale, bias=bias, accum_out=accum), R, W)

    def ts(self, eng, out, in0, s1, s2, op0, op1=None, accum=None, R=(), W=()):
        if op1 is None:
            op1 = ALU.bypass
        if accum is None:
            return self.op(eng, lambda e: e.tensor_scalar(out, in0, s1, s2, op0, op1), R, W)
        return self.op(eng, lambda e: e.tensor_scalar(out, in0, s1, s2, op0, op1, accum_out=accum), R, W)

    def tt(self, eng, out, in0, in1, op, R=(), W=()):
        return self.op(eng, lambda e: e.tensor_tensor(out, in0, in1, op), R, W)

    def stt(self, eng, out, in0, scalar, in1, op0, op1, R=(), W=()):
        return self.op(eng, lambda e: e.scalar_tensor_tensor(out, in0, scalar, in1, op0, op1), R, W)

    def cp(self, eng, out, in_, R=(), W=()):
        if eng == "act":
            return self.act(out, in_, AF.Copy, R=R, W=W)
        return self.op(eng, lambda e: e.tensor_copy(out, in_), R, W)

    def memset(self, eng, ap, val, W=()):
        return self.op(eng, lambda e: e.memset(ap, val), (), W)


IDX_HEADS, IDX_DIM = 16, 64
N_EXP, EXP_FF, N_GRP, EPG = 32, 256, 4, 8
ROPE_THETA = 10000.0
EPS = 1e-6


def derive(DM, S, DEPTH):
    G = DM // 4
    c = dict(DM=DM, S=S, KD=DM // 128, G=G, HA=G // 128, HB=G // 128, HC=G // 128, HD=G // 64,
             XBC=G + 512, KSEL=min(256, S // 4), NT=S // 128, DMIX=DM, KM=DM // 128)
    widths = (G, G, G, IDX_HEADS * IDX_DIM, IDX_DIM, IDX_HEADS,
              G, G, G, G // 128, G // 128, G,
              G // 2, G // 2, G, 16, G,
              G, G, 256, 256, G // 64)
    names = ("a_q", "a_k", "a_v", "a_qi", "a_ki", "a_wi", "b_q", "b_k", "b_v", "b_beta", "b_a", "b_z",
             "c_q", "c_k", "c_v", "c_gk", "c_g", "d_z", "d_x", "d_b", "d_c", "d_dt")
    off = {}
    o = 0
    for n, w in zip(names, widths):
        off[n] = (o, w)
        o += w
    c["off"] = off
    c["DP"] = o
    c["ALPHA"] = (2.0 * DEPTH) ** 0.25
    return c


WEIGHTS = ("ada_down", "ada_up", "ada_bias", "w_in", "w_out", "idx_kn_g", "idx_kn_b",
           "gdn_conv_w", "gdn_a_log", "gdn_dt_bias", "gdn_norm_w", "gla_w_up", "gla_b_up", "gla_norm_w",
           "ssd_conv_w", "ssd_conv_b", "ssd_a_log", "ssd_dt_bias", "ssd_d", "ssd_norm_w",
           "ln1_g", "ln1_b", "router_g_w", "router_g_b", "router_e_w", "router_e_b",
           "exp_w_gate", "exp_w_up", "exp_w_down", "ln2_g", "ln2_b")


def wshapes(c):
    DM, G = c["DM"], c["G"]
    return dict(ada_down=[DM, 256], ada_up=[256, 6 * DM], ada_bias=[6 * DM], w_in=[DM, c["DP"]], w_out=[DM, DM],
                idx_kn_g=[64], idx_kn_b=[64], gdn_conv_w=[4, 3 * G], gdn_a_log=[c["HB"]], gdn_dt_bias=[c["HB"]],
                gdn_norm_w=[128], gla_w_up=[16, G // 2], gla_b_up=[G // 2], gla_norm_w=[128],
                ssd_conv_w=[4, c["XBC"]], ssd_conv_b=[c["XBC"]], ssd_a_log=[c["HD"]], ssd_dt_bias=[c["HD"]],
                ssd_d=[c["HD"]], ssd_norm_w=[G], ln1_g=[DM], ln1_b=[DM], router_g_w=[DM, 4], router_g_b=[4],
                router_e_w=[DM, 32], router_e_b=[32], exp_w_gate=[32, DM, 256], exp_w_up=[32, DM, 256],
                exp_w_down=[32, 256, DM], ln2_g=[DM], ln2_b=[DM])


def host_consts():
    P = 128
    i = np.arange(P)
    U = (i[:, None] <= i[None, :]).astype(np.float32)
    cs = {"c_ident": np.eye(P, dtype=np.float32), "c_U": U, "c_NU": ((1.0 - U) * NEG).astype(np.float32),
          "c_ones": np.ones((P, P), np.float32), "c_NL": np.ascontiguousarray(((1.0 - U) * NEG).T.astype(np.float32))}
    LM = np.zeros((7, P, P), np.float32)
    for k in range(7):
        b = 1 << k
        r = (i // b)
        LM[k] = ((r[:, None] % 2 == 1) & (r[None, :] == r[:, None] - 1)).astype(np.float32)
    cs["c_LM"] = np.ascontiguousarray(LM.transpose(1, 0, 2))
    cs["c_LMT"] = np.ascontiguousarray(LM.transpose(2, 0, 1))
    sel = np.zeros((32, 32, P), np.float32)
    for e in range(32):
        sel[e, e, :] = 1.0
    cs["c_sel"] = np.ascontiguousarray(sel.transpose(1, 0, 2))
    return cs


def build(DM, S, NL, DEPTH, debug=False, STAGES=9, BIS=0, MIX="abcd"):
    c = derive(DM, S, DEPTH)
    KD, G, NT, DP = c["KD"], c["G"], c["NT"], c["DP"]
    HA, HB, HC, HD, XBC, KSEL = c["HA"], c["HB"], c["HC"], c["HD"], c["XBC"], c["KSEL"]
    off = c["off"]
    ALPHA = c["ALPHA"]
    nc = bass.Bass("TRN2", target_bir_lowering=False)
    P = Prog(nc)
    ES = P.es

    def din(name, shape, dt=F32):
        return T(name, nc.dram_tensor(name, list(shape), dt, kind="ExternalInput"))

    def dscr(name, shape, dt=F32, out=False):
        return T(name, nc.dram_tensor(name, list(shape), dt, kind="ExternalOutput" if out else "Internal"))

    x_in = din("x", [S, DM])
    c_in = din("c", [1, DM])
    pos_in = din("pos", [S, 1], I32)
    Wt = {n: din(n, [NL] + s) for n, s in wshapes(c).items()}
    CS = {n: din(n, list(a.shape)) for n, a in host_consts().items()}
    y_out = dscr("y", [S, DM], out=True)
    xa = dscr("xa", [S, DM]); xb = dscr("xb", [S, DM])
    proj = dscr("proj", [S, DP], out=debug)
    mixed = din("mixed_in", [S, DM]) if (debug and STAGES == 2) else dscr("mixed", [S, DM], out=debug)
    x1 = dscr("x1", [S, DM], out=debug)
    hg_d = dscr("hg", [S, N_EXP * EXP_FF]); hu_d = dscr("hu", [S, N_EXP * EXP_FF])
    modv = dscr("modv", [1, 6 * DM], out=debug)
    rope = dscr("rope", [S, 192], out=debug)

    ident = P.sb("ident", [128, 128]); Uc = P.sb("U", [128, 128]); NUc = P.sb("NU", [128, 128])
    ones = P.sb("ones", [128, 128])
    for tl, nm in ((ident, "c_ident"), (Uc, "c_U"), (NUc, "c_NU"), (ones, "c_ones")):
        P.dma("sp", tl[:], CS[nm][:, :], W=[tl])

    PS = [P.ps("ps%d" % i, [128, 512]) for i in range(8)]
    psrr = [0]

    psmod = [8]

    def nps():
        psrr[0] = (psrr[0] + 1) % psmod[0]
        return PS[psrr[0]]

    evrr = [0]

    def ev_eng():
        evrr[0] ^= 1
        return "act" if evrr[0] else "dve"

    from contextlib import contextmanager

    @contextmanager
    def scope():
        P.barrier()
        with ExitStack() as st:
            yield st
            P.barrier()

    castq = T("castq", None)
    castq_n = [0]

    def col_layout(st, src_row_ap, name):
        rows = P.sb(name + "r", [KD, 128], F32, st)
        P.dma("sp", rows[:], src_row_ap.rearrange("o (k p) -> (o k) p", p=128), W=[rows])
        pst = nps()
        P.tr(pst[:, 0:KD], rows[:], ident[0:KD, 0:KD], R=[rows, ident], W=[pst])
        tl = P.sb(name, [128, KD], F32, st)
        P.cp("dve", tl[:], pst[:, 0:KD], R=[pst], W=[tl])
        return tl

    def cast_weights(src_ap_fn, K, N, name):
        kd = K // 128
        NTL = 512 if kd <= 32 else 256
        NTL = min(NTL, N)
        nt = (N + NTL - 1) // NTL
        scr = dscr(name, [nt, 128, kd, NTL], BF16)
        for t in range(nt):
            w = min(NTL, N - t * NTL)
            src = src_ap_fn(t * NTL, w).rearrange("(kd p) n -> p kd n", p=128)
            for k0 in range(0, kd, 32):
                k1 = min(kd, k0 + 32)
                castq_n[0] += 1
                P.dma("pool", scr[t][:, k0:k1, 0:w], src[:, k0:k1, :], W=[(scr, t), (castq, castq_n[0] % 2)])
        return scr, NTL, nt

    def gemm(A, K, Wscr, NTL, nt, N, epilogue, prologue=None, name="g"):
        kd = K // 128
        TB = min(S, max(128, (32768 // kd) // 128 * 128))
        P.barrier()
        with scope() as st:
            AT = P.sb(name + "AT", [128, kd, TB], BF16, st)
            Wsb = [P.sb(name + "W%d" % i, [128, kd, NTL], BF16, st) for i in range(2)]
            xs = [P.sb(name + "xs%d" % i, [128, 2048], F32, st) for i in range(2)]
            k = 0
            for tb in range(0, S, TB):
                for tt_ in range(TB // 128):
                    t0 = tb + tt_ * 128
                    for c0 in range(0, K, 2048):
                        cw = min(2048, K - c0)
                        xt = xs[k % 2]; k += 1
                        P.dma("sp", xt[:, 0:cw], A[t0:t0 + 128, c0:c0 + cw], R=[A], W=[xt])
                        for j in range(cw // 128):
                            kk = (c0 // 128) + j
                            if j % 4 == 0:
                                pst = nps()
                            P.tr(pst[:, (j % 4) * 128:(j % 4 + 1) * 128], xt[:, j * 128:(j + 1) * 128], ident[:],
                                 R=[xt, ident], W=[(pst, j % 4)])
                            if prologue is None:
                                if j % 4 == 3:
                                    P.cp(ev_eng(), AT[:, kk - 3:kk + 1, tt_ * 128:(tt_ + 1) * 128],
                                         pst[:, :].rearrange("p (a b) -> p a b", a=4), R=[pst], W=[(AT, tt_)])
                            else:
                                prologue(kk, AT[:, kk, tt_ * 128:(tt_ + 1) * 128], pst[:, (j % 4) * 128:(j % 4 + 1) * 128],
                                         [(pst, j % 4)], [(AT, tt_)])
                for t in range(0 if BIS == 3 else (nt - 1 if BIS == 4 else nt)):
                    w = min(NTL, N - t * NTL)
                    ws = Wsb[t % 2]
                    P.dma("sp", ws[:], Wscr[t], R=[(Wscr, t)], W=[ws])
                    for tt_ in range(TB // 128):
                        pst = nps()
                        for kk in range(kd):
                            P.mm(pst[:, 0:NTL], AT[:, kk, tt_ * 128:(tt_ + 1) * 128], ws[:, kk, :], start=(kk == 0),
                                 stop=(kk == kd - 1), R=[(AT, tt_), ws], W=[pst])
                        epilogue(tb + tt_ * 128, t * NTL, w, pst, [pst])

    def store_epilogue(dst, st):
        bufs = [P.sb("ob%d" % i, [128, 512], F32, st) for i in range(3)]
        cnt = [0]

        def ep(t0, n0, w, pst, R):
            ob = bufs[cnt[0] % 3]; cnt[0] += 1
            P.cp(ev_eng(), ob[:, 0:w], pst[:, 0:w], R=R, W=[ob])
            P.dma("act", dst[t0:t0 + 128, n0:n0 + w], ob[:, 0:w], R=[ob], W=[dst])
        return ep

    def bcast_row(dst_tile, src_dram_row_ap, n, eng="sp"):
        P.dma(eng, dst_tile[:, 0:n], src_dram_row_ap.to_broadcast([128, n]), W=[dst_tile])

    def stage_mod(l):
        P.barrier()
        with scope() as st:
            cT = P.sb("cT", [128, KD, 2], F32, st)
            P.memset("dve", cT[:], 0.0, W=[cT])
            cc = col_layout(st, c_in.t.ap(), "ccol")
            P.cp("dve", cT[:, :, 0:1], cc[:].rearrange("p (k o) -> p k o", o=1), R=[cc], W=[cT])
            P.act(cT[:], cT[:], AF.Silu, R=[cT], W=[cT])
            ad = P.sb("ad", [128, KD, 256], F32, st)
            P.dma("sp", ad[:], Wt["ada_down"][l].rearrange("(k p) r -> p k r", p=128), W=[ad])
            vT = P.sb("vT", [128, 2], F32, st)
            for rh in range(2):
                pst = nps()
                for kk in range(KD):
                    P.mm(pst[:, 0:2], ad[:, kk, rh * 128:(rh + 1) * 128], cT[:, kk, :], start=(kk == 0), stop=(kk == KD - 1),
                         R=[ad, cT], W=[pst])
                P.cp("dve", vT[:, rh:rh + 1], pst[:, 0:1], R=[pst], W=[vT])
            au = [P.sb("au%d" % i, [128, 2, 512], F32, st) for i in range(2)]
            bb = [P.sb("bb%d" % i, [1, 512], F32, st) for i in range(2)]
            rw = [P.sb("rw%d" % i, [1, 512], F32, st) for i in range(2)]
            for mc in range(6 * DM // 512):
                a_, b_, r_ = au[mc % 2], bb[mc % 2], rw[mc % 2]
                P.dma("sp", a_[:], Wt["ada_up"][l][:, mc * 512:(mc + 1) * 512].rearrange("(r p) n -> p r n", p=128), W=[a_])
                P.dma("sp", b_[:], Wt["ada_bias"][l:l + 1, mc * 512:(mc + 1) * 512], W=[b_])
                pst = nps()
                for rh in range(2):
                    P.mm(pst[0:1, :], vT[:, rh:rh + 1], a_[:, rh, :], start=(rh == 0), stop=(rh == 1), R=[vT, a_], W=[pst])
                P.tt("dve", r_[:], pst[0:1, :], b_[:], ALU.add, R=[pst, b_], W=[r_])
                P.dma("act", modv[0:1, mc * 512:(mc + 1) * 512], r_[:], R=[r_], W=[modv])

    def mod_cols(st, part, plus_one):
        P.barrier()
        tl = col_layout(st, modv.t.ap()[0:1, part * DM:(part + 1) * DM], "mc%d" % part)
        if plus_one:
            P.ts("dve", tl[:], tl[:], 1.0, None, ALU.add, R=[tl], W=[tl])
        return tl

    def stage_proj(l, xin):
        with scope() as st:
            wscr, NTL, nt = cast_weights(lambda n0, w: Wt["w_in"][l][:, n0:n0 + w], DM, DP, "win_bf%d" % l)
            s1 = mod_cols(st, 1, True)
            sh1 = mod_cols(st, 0, False)

            def prologue(kk, out_ap, ps_ap, R, W):
                P.act(out_ap, ps_ap, AF.Identity, scale=s1[:, kk:kk + 1], bias=sh1[:, kk:kk + 1], R=R + [s1, sh1], W=W)
            if BIS != 1:
                gemm(xin, DM, wscr, NTL, nt, DP, store_epilogue(proj, st), None if BIS in (2, 3, 4) else prologue, name="pj")

    def conv_silu(t, c0, ncols, wbc, bias_bc, win, acc, tmp):
        for i in range(4):
            r0 = t * 128 - 3 + i
            wt = win[i % len(win)]
            if r0 < 0:
                P.memset("pool", wt[:, 0:ncols], 0.0, W=[wt])
                P.dma("sp", wt[-r0:128, 0:ncols], proj[0:128 + r0, c0:c0 + ncols], R=[proj], W=[wt])
            else:
                P.dma("sp", wt[:, 0:ncols], proj[r0:r0 + 128, c0:c0 + ncols], R=[proj], W=[wt])
            if i == 0:
                P.tt("pool", acc[:, 0:ncols], wt[:, 0:ncols], wbc[0][:, 0:ncols], ALU.mult, R=[wt, wbc[0]], W=[acc])
            else:
                P.tt("pool", tmp[:, 0:ncols], wt[:, 0:ncols], wbc[i][:, 0:ncols], ALU.mult, R=[wt, wbc[i]], W=[tmp])
                P.tt("dve", acc[:, 0:ncols], acc[:, 0:ncols], tmp[:, 0:ncols], ALU.add, R=[acc, tmp], W=[acc])
        if bias_bc is not None:
            P.tt("dve", acc[:, 0:ncols], acc[:, 0:ncols], bias_bc[:, 0:ncols], ALU.add, R=[acc, bias_bc], W=[acc])
        P.act(acc[:, 0:ncols], acc[:, 0:ncols], AF.Silu, R=[acc], W=[acc])

    def ldp(tile, t, name, eng="sp"):
        o, w = off[name]
        kw = {"allow_slow_non_contiguous": True} if w == 1 else {}
        P.dma(eng, tile[:, 0:w], proj[t * 128:(t + 1) * 128, o:o + w], R=[proj], W=[tile], **kw)

    def bc3(ap2, h, d):
        return ap2.rearrange("p (h o) -> p h o", o=1).to_broadcast([128, h, d])

    def v3(ap2, h):
        return ap2.rearrange("p (h d) -> p h d", h=h)

    def decayT(g, h0, nh, dst, ug, nug):
        pst = nps()
        for k in range(nh):
            h = h0 + k
            P.ts("pool", ug[:], Uc[:], g[:, h:h + 1], None, ALU.mult, R=[Uc, g], W=[ug])
            P.ts("dve", nug[:], Uc[:], g[:, h:h + 1], -1.0, ALU.mult, ALU.mult, R=[Uc, g], W=[nug])
            sl = pst[:, k * 128:(k + 1) * 128]
            P.mm(sl, ones[:], ug[:], start=True, stop=False, R=[ones, ug], W=[pst])
            P.mm(sl, nug[:], ones[:], start=False, stop=False, R=[nug, ones], W=[pst])
            P.mm(sl, ident[:], NUc[:], start=False, stop=True, R=[ident, NUc], W=[pst])
        P.act(dst[:, 0:nh * 128], pst[:, 0:nh * 128], AF.Exp, R=[pst], W=[dst])

    def stage_ssd(l):
        HG = HD // 2
        with scope() as st:
            wbc = [P.sb("dw%d" % i, [128, XBC], F32, st) for i in range(4)]
            for i in range(4):
                bcast_row(wbc[i], Wt["ssd_conv_w"][l, i:i + 1, :], XBC)
            cb = P.sb("dcb", [128, XBC], F32, st); bcast_row(cb, Wt["ssd_conv_b"][l:l + 1, :], XBC)
            dtb = P.sb("ddtb", [128, HD], F32, st); bcast_row(dtb, Wt["ssd_dt_bias"][l:l + 1, :], HD)
            adec = P.sb("dadec", [128, HD], F32, st); bcast_row(adec, Wt["ssd_a_log"][l:l + 1, :], HD)
            P.act(adec[:], adec[:], AF.Exp, R=[adec], W=[adec])
            P.ts("dve", adec[:], adec[:], -1.0, None, ALU.mult, R=[adec], W=[adec])
            dsk = P.sb("ddsk", [128, HD], F32, st); bcast_row(dsk, Wt["ssd_d"][l:l + 1, :], HD)
            nw = P.sb("dnw", [128, G], F32, st); bcast_row(nw, Wt["ssd_norm_w"][l:l + 1, :], G)
            win = [P.sb("dwin%d" % i, [128, XBC], F32, st) for i in range(4)]
            acc = P.sb("dacc", [128, XBC], F32, st); tmp = P.sb("dtmp", [128, XBC], F32, st)
            zt = P.sb("dz", [128, G], F32, st); dtt = P.sb("ddt", [128, HD], F32, st)
            g = P.sb("dg", [128, HD], F32, st); gc = P.sb("dgc", [128, HD], F32, st); gcl = P.sb("dgcl", [128, HD], F32, st)
            eg = P.sb("deg", [128, HD], F32, st); egl = P.sb("degl", [128, HD], F32, st); gl = P.sb("dgl", [128, HD], F32, st)
            xdt = P.sb("dxdt", [128, G], F32, st)
            BCT = P.sb("dBCT", [128, 4, 128], F32, st)
            CBT = P.sb("dCBT", [128, 2, 128], F32, st)
            ug = P.sb("dug", [128, 128], F32, st); nug = P.sb("dnug", [128, 128], F32, st)
            dec = P.sb("ddec", [128, 512], F32, st)
            ST = P.sb("dST", [128, HD, 128], F32, st)
            Bd = [P.sb("dBd%d" % i, [128, 128], F32, st) for i in range(2)]
            S_ = P.sb("dS", [128, G], F32, st)
            P.memset("dve", S_[:], 0.0, W=[S_])
            o_ = P.sb("do", [128, G], F32, st); t2 = P.sb("dt2", [128, G], F32, st)
            sm = P.sb("dsm", [128, 8], F32, st)
            for t in range(NT):
                conv_silu(t, off["d_x"][0], XBC, wbc, cb, win, acc, tmp)
                ldp(zt, t, "d_z"); ldp(dtt, t, "d_dt")
                P.tt("dve", dtt[:], dtt[:], dtb[:], ALU.add, R=[dtt, dtb], W=[dtt])
                P.act(dtt[:], dtt[:], AF.Exp, R=[dtt], W=[dtt])
                P.act(dtt[:], dtt[:], AF.Ln, bias=1.0, R=[dtt], W=[dtt])
                P.tt("dve", g[:], dtt[:], adec[:], ALU.mult, R=[dtt, adec], W=[g])
                P.tt("dve", v3(xdt[:], HD), v3(acc[:, 0:G], HD), bc3(dtt[:], HD, 64), ALU.mult, R=[acc, dtt], W=[xdt])
                pst = nps()
                P.mm(pst[:, 0:HD], Uc[:], g[:], R=[Uc, g], W=[pst])
                P.mm(pst[:, 64:64 + HD], ones[:], g[:], R=[ones, g], W=[pst])
                P.cp("dve", gc[:], pst[:, 0:HD], R=[pst], W=[gc])
                P.cp("dve", gcl[:], pst[:, 64:64 + HD], R=[pst], W=[gcl])
                P.act(eg[:], gc[:], AF.Exp, R=[gc], W=[eg])
                P.act(gl[:], gcl[:], AF.Exp, R=[gcl], W=[gl])
                P.tt("dve", egl[:], gcl[:], gc[:], ALU.subtract, R=[gcl, gc], W=[egl])
                P.act(egl[:], egl[:], AF.Exp, R=[egl], W=[egl])
                pst = nps()
                for k in range(4):
                    P.tr(pst[:, k * 128:(k + 1) * 128], acc[:, G + k * 128:G + (k + 1) * 128], ident[:], R=[acc, ident], W=[pst])
                P.cp("act", BCT[:], v3(pst[:, :], 4), R=[pst], W=[BCT])
                pst = nps()
                for gr in range(2):
                    P.mm(pst[:, gr * 128:(gr + 1) * 128], BCT[:, gr, :], BCT[:, 2 + gr, :], R=[BCT], W=[pst])
                P.cp("act", CBT[:], v3(pst[:, 0:256], 2), R=[pst], W=[CBT])
                for h0 in range(0, HD, 4):
                    nh = min(4, HD - h0)
                    decayT(g, h0, nh, dec, ug, nug)
                    for k in range(nh):
                        h = h0 + k
                        P.tt("dve" if k % 2 else "pool", ST[:, h, :], dec[:, k * 128:(k + 1) * 128], CBT[:, h // HG, :], ALU.mult,
                             R=[dec, CBT], W=[(ST, h)])
                p_in = [nps() for _ in range((HD * 64 + 511) // 512)]
                for h in range(HD):
                    P.mm(p_in[(h * 64) // 512][:, (h * 64) % 512:(h * 64) % 512 + 64], ST[:, h, :], xdt[:, h * 64:(h + 1) * 64],
                         R=[(ST, h), xdt], W=[p_in[(h * 64) // 512]])
                p_x = [nps() for _ in range(2)]
                for gr in range(2):
                    P.mm(p_x[gr][:, 0:HG * 64], BCT[:, 2 + gr, :], S_[:, gr * HG * 64:(gr + 1) * HG * 64], R=[BCT, S_], W=[p_x[gr]])
                for gr in range(2):
                    cs = slice(gr * HG * 64, (gr + 1) * HG * 64)
                    P.tt("dve", v3(o_[:, cs], HG), v3(p_x[gr][:, 0:HG * 64], HG), bc3(eg[:, gr * HG:(gr + 1) * HG], HG, 64), ALU.mult,
                         R=[p_x[gr], eg], W=[o_])
                for bi, pb in enumerate(p_in):
                    w = min(512, HD * 64 - bi * 512)
                    P.tt("dve", o_[:, bi * 512:bi * 512 + w], o_[:, bi * 512:bi * 512 + w], pb[:, 0:w], ALU.add, R=[o_, pb], W=[o_])
                p_s = [nps() for _ in range((HD * 64 + 511) // 512)]
                for h in range(HD):
                    bd = Bd[h % 2]
                    P.ts("pool" if h % 2 else "dve", bd[:], acc[:, G + (h // HG) * 128:G + (h // HG + 1) * 128], egl[:, h:h + 1], None, ALU.mult,
                         R=[acc, egl], W=[bd])
                    P.mm(p_s[(h * 64) // 512][:, (h * 64) % 512:(h * 64) % 512 + 64], bd[:], xdt[:, h * 64:(h + 1) * 64],
                         R=[bd, xdt], W=[p_s[(h * 64) // 512]])
                P.tt("dve", v3(S_[:], HD), v3(S_[:], HD), bc3(gl[:], HD, 64), ALU.mult, R=[S_, gl], W=[S_])
                for bi, pb in enumerate(p_s):
                    w = min(512, HD * 64 - bi * 512)
                    P.tt("dve", S_[:, bi * 512:bi * 512 + w], S_[:, bi * 512:bi * 512 + w], pb[:, 0:w], ALU.add, R=[S_, pb], W=[S_])
                P.tt("pool", v3(t2[:], HD), v3(acc[:, 0:G], HD), bc3(dsk[:], HD, 64), ALU.mult, R=[acc, dsk], W=[t2])
                P.tt("dve", o_[:], o_[:], t2[:], ALU.add, R=[o_, t2], W=[o_])
                P.act(zt[:], zt[:], AF.Silu, R=[zt], W=[zt])
                P.tt("dve", o_[:], o_[:], zt[:], ALU.mult, R=[o_, zt], W=[o_])
                for gr in range(2):
                    cs = slice(gr * G // 2, (gr + 1) * G // 2)
                    P.act(t2[:, cs], o_[:, cs], AF.Square, accum=sm[:, gr:gr + 1], R=[o_], W=[t2, sm])
                P.ts("dve", sm[:, 2:4], sm[:, 0:2], 2.0 / G, EPS, ALU.mult, ALU.add, R=[sm], W=[sm])
                P.act(sm[:, 4:6], sm[:, 2:4], AF.Sqrt, R=[sm], W=[sm])
                P.op("dve", lambda e: e.reciprocal(sm[:, 6:8], sm[:, 4:6]), R=[sm], W=[sm])
                for gr in range(2):
                    cs = slice(gr * G // 2, (gr + 1) * G // 2)
                    P.ts("dve", o_[:, cs], o_[:, cs], sm[:, 6 + gr:7 + gr], None, ALU.mult, R=[o_, sm], W=[o_])
                P.tt("pool", o_[:], o_[:], nw[:], ALU.mult, R=[o_, nw], W=[o_])
                P.dma("act", mixed[t * 128:(t + 1) * 128, 3 * G:4 * G], o_[:], R=[o_], W=[(mixed, t, 3)])

    def stage_gla(l):
        G2 = G // 2
        with scope() as st:
            wup = P.sb("cwup", [16, G2], F32, st)
            P.dma("sp", wup[:], Wt["gla_w_up"][l], W=[wup])
            bup = P.sb("cbup", [128, G2], F32, st); bcast_row(bup, Wt["gla_b_up"][l:l + 1, :], G2)
            nw = P.sb("cnw", [128, 128], F32, st); bcast_row(nw, Wt["gla_norm_w"][l:l + 1, :], 128)
            q = P.sb("cq", [128, G2], F32, st); k = P.sb("ck", [128, G2], F32, st); v = P.sb("cv", [128, G], F32, st)
            gkr = P.sb("cgkr", [128, 16], F32, st); gz = P.sb("cgz", [128, G], F32, st)
            gkT = P.sb("cgkT", [16, 128], F32, st)
            gk = P.sb("cgk", [128, G2], F32, st); b = P.sb("cb", [128, G2], F32, st)
            e1 = P.sb("ce1", [128, G2], F32, st); e2 = P.sb("ce2", [128, G2], F32, st); e3 = P.sb("ce3", [128, G2], F32, st)
            qe = P.sb("cqe", [128, G2], F32, st); ke = P.sb("cke", [128, G2], F32, st); kd = P.sb("ckd", [128, G2], F32, st)
            qeT = P.sb("cqeT", [64, HC, 128], F32, st); keT = P.sb("ckeT", [64, HC, 128], F32, st)
            ebt = P.sb("cebt", [64, HC, 2], F32, st)
            one2 = P.sb("cone2", [128, 2], F32, st); P.memset("dve", one2[:], 1.0, W=[one2])
            aT = [P.sb("caT%d" % i, [128, 128], F32, st) for i in range(2)]
            S_ = P.sb("cS", [64, HC * 128], F32, st); P.memset("dve", S_[:], 0.0, W=[S_])
            o_ = P.sb("co", [128, G], F32, st); t2 = P.sb("ct2", [128, G], F32, st)
            sm = P.sb("csm", [128, 4 * HC], F32, st)
            for t in range(NT):
                ldp(q, t, "c_q"); ldp(k, t, "c_k"); ldp(v, t, "c_v"); ldp(gkr, t, "c_gk"); ldp(gz, t, "c_g")
                pst = nps()
                P.tr(pst[0:16, 0:128], gkr[:], ident[:], R=[gkr, ident], W=[pst])
                P.cp("act", gkT[:], pst[0:16, 0:128], R=[pst], W=[gkT])
                pst = nps()
                P.mm(pst[:, 0:G2], gkT[:], wup[:], R=[gkT, wup], W=[pst])
                P.tt("dve", gk[:], pst[:, 0:G2], bup[:], ALU.add, R=[pst, bup], W=[gk])
                P.act(gk[:], gk[:], AF.Exp, scale=-1.0, R=[gk], W=[gk])
                P.act(gk[:], gk[:], AF.Ln, bias=1.0, R=[gk], W=[gk])
                P.ts("dve", gk[:], gk[:], -1.0 / 16.0, None, ALU.mult, R=[gk], W=[gk])
                pst = nps(); pst2 = nps()
                P.mm(pst[:, 0:G2], Uc[:], gk[:], R=[Uc, gk], W=[pst])
                P.mm(pst2[:, 0:G2], ones[:], gk[:], R=[ones, gk], W=[pst2])
                P.cp("dve", b[:], pst[:, 0:G2], R=[pst], W=[b])
                P.act(e1[:], b[:], AF.Exp, R=[b], W=[e1])
                P.act(e2[:], b[:], AF.Exp, scale=-1.0, R=[b], W=[e2])
                P.tt("dve", e3[:], pst2[:, 0:G2], b[:], ALU.subtract, R=[pst2, b], W=[e3])
                P.act(e3[:], e3[:], AF.Exp, R=[e3], W=[e3])
                P.stt("dve", qe[:], q[:], 0.125, e1[:], ALU.mult, ALU.mult, R=[q, e1], W=[qe])
                P.tt("pool", ke[:], k[:], e2[:], ALU.mult, R=[k, e2], W=[ke])
                P.tt("pool", kd[:], k[:], e3[:], ALU.mult, R=[k, e3], W=[kd])
                for src, dstT in ((qe, qeT), (ke, keT)):
                    for h0 in range(0, HC, 4):
                        nh = min(4, HC - h0)
                        pst = nps()
                        for kk in range(nh):
                            P.tr(pst[0:64, kk * 128:(kk + 1) * 128], src[:, (h0 + kk) * 64:(h0 + kk + 1) * 64], ident[:], R=[src, ident], W=[pst])
                        P.cp("act", dstT[:, h0:h0 + nh, :], v3(pst[0:64, 0:nh * 128], nh), R=[pst], W=[dstT])
                for h in range(HC):
                    pst = nps()
                    P.mm(pst[0:64, 0:2], gk[:, h * 64:(h + 1) * 64], one2[:], R=[gk, one2], W=[pst])
                    P.act(ebt[:, h, :], pst[0:64, 0:2], AF.Exp, R=[pst], W=[ebt])
                for h0 in range(0, HC, 4):
                    nh = min(4, HC - h0)
                    p_o = nps()
                    for kk in range(nh):
                        h = h0 + kk
                        pst = nps()
                        P.mm(pst[:, 0:128], keT[:, h, :], qeT[:, h, :], R=[keT, qeT], W=[pst])
                        at = aT[h % 2]
                        P.tt("dve", at[:], pst[:, 0:128], Uc[:], ALU.mult, R=[pst, Uc], W=[at])
                        osl = p_o[:, kk * 128:(kk + 1) * 128]
                        P.mm(osl, at[:], v[:, h * 128:(h + 1) * 128], start=True, stop=False, R=[at, v], W=[p_o])
                        P.mm(osl, qeT[:, h, :], S_[:, h * 128:(h + 1) * 128], start=False, stop=True, R=[qeT, S_], W=[p_o])
                    P.cp("act", o_[:, h0 * 128:(h0 + nh) * 128], p_o[:, 0:nh * 128], R=[p_o], W=[o_])
                    for kk in range(nh):
                        h = h0 + kk
                        pst = nps()
                        P.mm(pst[0:64, 0:128], kd[:, h * 64:(h + 1) * 64], v[:, h * 128:(h + 1) * 128], R=[kd, v], W=[pst])
                        P.stt("dve", S_[:, h * 128:(h + 1) * 128], S_[:, h * 128:(h + 1) * 128], ebt[:, h, 0:1], pst[0:64, 0:128],
                              ALU.mult, ALU.add, R=[S_, ebt, pst], W=[S_])
                P.tt("pool", t2[:], o_[:], o_[:], ALU.mult, R=[o_], W=[t2])
                P.op("dve", lambda e: e.reduce_sum(sm[:, 0:HC], v3(t2[:], HC), AX.X), R=[t2], W=[sm])
                P.ts("dve", sm[:, HC:2 * HC], sm[:, 0:HC], 1.0 / 128.0, EPS, ALU.mult, ALU.add, R=[sm], W=[sm])
                P.act(sm[:, 2 * HC:3 * HC], sm[:, HC:2 * HC], AF.Sqrt, R=[sm], W=[sm])
                P.op("dve", lambda e: e.reciprocal(sm[:, 3 * HC:4 * HC], sm[:, 2 * HC:3 * HC]), R=[sm], W=[sm])
                P.tt("dve", v3(o_[:], HC), v3(o_[:], HC), bc3(sm[:, 3 * HC:4 * HC], HC, 128), ALU.mult, R=[o_, sm], W=[o_])
                P.tt("pool", v3(o_[:], HC), v3(o_[:], HC), nw[:].rearrange("p (o d) -> p o d", o=1).to_broadcast([128, HC, 128]), ALU.mult,
                     R=[o_, nw], W=[o_])
                P.act(gz[:], gz[:], AF.Silu, R=[gz], W=[gz])
                P.tt("dve", o_[:], o_[:], gz[:], ALU.mult, R=[o_, gz], W=[o_])
                P.dma("act", mixed[t * 128:(t + 1) * 128, 2 * G:3 * G], o_[:], R=[o_], W=[(mixed, t, 2)])

    def stage_gdn(l):
        with scope() as st:
            CW = 3 * G
            wbc = [P.sb("bw%d" % i, [128, CW], F32, st) for i in range(4)]
            for i in range(4):
                bcast_row(wbc[i], Wt["gdn_conv_w"][l, i:i + 1, :], CW)
            dtb = P.sb("bdtb", [128, HB], F32, st); bcast_row(dtb, Wt["gdn_dt_bias"][l:l + 1, :], HB)
            adec = P.sb("badec", [128, HB], F32, st); bcast_row(adec, Wt["gdn_a_log"][l:l + 1, :], HB)
            P.act(adec[:], adec[:], AF.Exp, R=[adec], W=[adec])
            P.ts("dve", adec[:], adec[:], -1.0, None, ALU.mult, R=[adec], W=[adec])
            nw = P.sb("bnw", [128, 128], F32, st); bcast_row(nw, Wt["gdn_norm_w"][l:l + 1, :], 128)
            LM = P.sb("bLM", [128, 7, 128], F32, st); LMT = P.sb("bLMT", [128, 7, 128], F32, st)
            P.dma("sp", LM[:], CS["c_LM"][:, :, :], W=[LM]); P.dma("sp", LMT[:], CS["c_LMT"][:, :, :], W=[LMT])
            win = [P.sb("bwin%d" % i, [128, CW], F32, st) for i in range(2)]
            acc = P.sb("bacc", [128, CW], F32, st); tmp = P.sb("btmp", [128, CW], F32, st)
            zt = P.sb("bz", [128, G], F32, st); beta = P.sb("bbeta", [128, HB], F32, st); ba = P.sb("bba", [128, HB], F32, st)
            g = P.sb("bg", [128, HB], F32, st); gc = P.sb("bgc", [128, HB], F32, st); gcl = P.sb("bgcl", [128, HB], F32, st)
            eg = P.sb("beg", [128, HB], F32, st); egl = P.sb("begl", [128, HB], F32, st); gl = P.sb("bgl", [128, HB], F32, st)
            rn = P.sb("brn", [128, 4 * 2 * HB], F32, st)
            qn = P.sb("bqn", [128, G], F32, st); kn = P.sb("bkn", [128, G], F32, st)
            kb = P.sb("bkb", [128, G], F32, st); vb = P.sb("bvb", [128, G], F32, st)
            qg = P.sb("bqg", [128, G], F32, st); kbg = P.sb("bkbg", [128, G], F32, st); kdd = P.sb("bkdd", [128, G], F32, st)
            ug = P.sb("bug", [128, 128], F32, st); nug = P.sb("bnug", [128, 128], F32, st)
            dec = P.sb("bdec", [128, 512], F32, st)
            HBUF = []
            for i_ in range(min(4, HB)):
                HBUF.append((P.sb("bTT", [128, 4, 128], F32, st), P.sb("bAQ", [128, 2, 128], F32, st), P.sb("bA", [128, 128], F32, st),
                             P.sb("bLA", [128, 7, 128], F32, st), P.sb("bLAT", [128, 7, 128], F32, st),
                             P.sb("bDE", [128, 2, 128], F32, st), P.sb("bX", [128, 2, 128], F32, st),
                             P.sb("bu0", [128, 128], F32, st), P.sb("bwT", [128, 128], F32, st), P.sb("bu", [128, 128], F32, st)))
            S_ = P.sb("bS", [128, HB * 128], F32, st); P.memset("dve", S_[:], 0.0, W=[S_])
            o_ = P.sb("bo", [128, G], F32, st); t2 = tmp
            sm = P.sb("bsm", [128, 4 * HB], F32, st)
            for t in range(NT):
                conv_silu(t, off["b_q"][0], CW, wbc, None, win, acc, tmp)
                ldp(zt, t, "b_z"); ldp(beta, t, "b_beta"); ldp(ba, t, "b_a")
                P.act(beta[:], beta[:], AF.Sigmoid, R=[beta], W=[beta])
                P.tt("dve", ba[:], ba[:], dtb[:], ALU.add, R=[ba, dtb], W=[ba])
                P.act(ba[:], ba[:], AF.Exp, R=[ba], W=[ba])
                P.act(ba[:], ba[:], AF.Ln, bias=1.0, R=[ba], W=[ba])
                P.tt("dve", g[:], ba[:], adec[:], ALU.mult, R=[ba, adec], W=[g])
                P.tt("pool", tmp[:, 0:2 * G], acc[:, 0:2 * G], acc[:, 0:2 * G], ALU.mult, R=[acc], W=[tmp])
                P.op("dve", lambda e: e.reduce_sum(rn[:, 0:2 * HB], v3(tmp[:, 0:2 * G], 2 * HB), AX.X), R=[tmp], W=[rn])
                P.ts("dve", rn[:, 2 * HB:4 * HB], rn[:, 0:2 * HB], EPS, None, ALU.add, R=[rn], W=[rn])
                P.act(rn[:, 4 * HB:6 * HB], rn[:, 2 * HB:4 * HB], AF.Sqrt, R=[rn], W=[rn])
                P.op("dve", lambda e: e.reciprocal(rn[:, 6 * HB:8 * HB], rn[:, 4 * HB:6 * HB]), R=[rn], W=[rn])
                P.ts("dve", rn[:, 6 * HB:7 * HB], rn[:, 6 * HB:7 * HB], 128.0 ** -0.5, None, ALU.mult, R=[rn], W=[rn])
                P.tt("dve", v3(qn[:], HB), v3(acc[:, 0:G], HB), bc3(rn[:, 6 * HB:7 * HB], HB, 128), ALU.mult, R=[acc, rn], W=[qn])
                P.tt("dve", v3(kn[:], HB), v3(acc[:, G:2 * G], HB), bc3(rn[:, 7 * HB:8 * HB], HB, 128), ALU.mult, R=[acc, rn], W=[kn])
                P.tt("pool", v3(kb[:], HB), v3(kn[:], HB), bc3(beta[:], HB, 128), ALU.mult, R=[kn, beta], W=[kb])
                P.tt("pool", v3(vb[:], HB), v3(acc[:, 2 * G:3 * G], HB), bc3(beta[:], HB, 128), ALU.mult, R=[acc, beta], W=[vb])
                pst = nps()
                P.mm(pst[:, 0:HB], Uc[:], g[:], R=[Uc, g], W=[pst])
                P.mm(pst[:, 64:64 + HB], ones[:], g[:], R=[ones, g], W=[pst])
                P.cp("dve", gc[:], pst[:, 0:HB], R=[pst], W=[gc])
                P.cp("dve", gcl[:], pst[:, 64:64 + HB], R=[pst], W=[gcl])
                P.act(eg[:], gc[:], AF.Exp, R=[gc], W=[eg])
                P.act(gl[:], gcl[:], AF.Exp, R=[gcl], W=[gl])
                P.tt("dve", egl[:], gcl[:], gc[:], ALU.subtract, R=[gcl, gc], W=[egl])
                P.act(egl[:], egl[:], AF.Exp, R=[egl], W=[egl])
                P.tt("dve", v3(qg[:], HB), v3(qn[:], HB), bc3(eg[:], HB, 128), ALU.mult, R=[qn, eg], W=[qg])
                P.tt("pool", v3(kbg[:], HB), v3(kb[:], HB), bc3(eg[:], HB, 128), ALU.mult, R=[kb, eg], W=[kbg])
                P.tt("pool", v3(kdd[:], HB), v3(kn[:], HB), bc3(egl[:], HB, 128), ALU.mult, R=[kn, egl], W=[kdd])
                for h0 in range(0, HB, 4):
                    nh = min(4, HB - h0)
                    decayT(g, h0, nh, dec, ug, nug)

                    def head_gen(h, kk, Bf):
                        TT, AQ, A_, LA, LAT, DE, X, u0, wT, u = Bf
                        hs = slice(h * 128, (h + 1) * 128)
                        pst = nps()
                        for i_, src in enumerate((kn, kb, qn, qg)):
                            P.tr(pst[:, i_ * 128:(i_ + 1) * 128], src[:, hs], ident[:], R=[src, ident], W=[pst])
                        P.cp("act", TT[:], v3(pst[:, :], 4), R=[pst], W=[TT])
                        yield
                        pst = nps()
                        P.mm(pst[:, 0:256], TT[:, 0, :], TT[:, 1:3, :], R=[TT], W=[pst])
                        P.tt("dve", AQ[:], v3(pst[:, 0:256], 2),
                             dec[:, kk * 128:(kk + 1) * 128].rearrange("p (o d) -> p o d", o=1).to_broadcast([128, 2, 128]), ALU.mult,
                             R=[pst, dec], W=[AQ])
                        yield
                        pst = nps()
                        P.tr(pst[:, 0:128], AQ[:, 0, :], ident[:], R=[AQ, ident], W=[pst])
                        P.cp("act", A_[:], pst[:, 0:128], R=[pst], W=[A_])
                        P.tt("dve", LAT[:], LMT[:], AQ[:, 0, :].rearrange("p (o d) -> p o d", o=1).to_broadcast([128, 7, 128]), ALU.mult,
                             R=[LMT, AQ], W=[LAT])
                        yield
                        P.tt("pool", LA[:], LM[:], A_[:].rearrange("p (o d) -> p o d", o=1).to_broadcast([128, 7, 128]), ALU.mult,
                             R=[LM, A_], W=[LA])
                        P.tt("dve", DE[:, 1, :], ident[:], LAT[:, 0, :], ALU.subtract, R=[ident, LAT], W=[DE])
                        yield
                        P.tt("dve", DE[:, 0, :], ident[:], LA[:, 0, :], ALU.subtract, R=[ident, LA], W=[DE])
                        yield
                        for lv in range(1, 7):
                            pst = nps()
                            P.mm(pst[:, 0:128], LAT[:, lv, :], DE[:, 0, :], R=[LAT, DE], W=[pst])
                            P.mm(pst[:, 128:256], LA[:, lv, :], DE[:, 1, :], R=[LA, DE], W=[pst])
                            P.cp("act", X[:], v3(pst[:, 0:256], 2), R=[pst], W=[X])
                            yield
                            pst = nps()
                            P.mm(pst[:, 0:128], DE[:, 1, :], X[:, 0, :], R=[DE, X], W=[pst])
                            P.mm(pst[:, 128:256], DE[:, 0, :], X[:, 1, :], R=[DE, X], W=[pst])
                            P.tt("dve", DE[:], DE[:], v3(pst[:, 0:256], 2), ALU.subtract, R=[DE, pst], W=[DE])
                            yield
                        pst = nps(); pst2 = nps()
                        P.mm(pst[:, 0:128], DE[:, 1, :], vb[:, hs], R=[DE, vb], W=[pst])
                        P.mm(pst2[:, 0:128], kbg[:, hs], DE[:, 1, :], R=[kbg, DE], W=[pst2])
                        P.cp("act", u0[:], pst[:, 0:128], R=[pst], W=[u0])
                        P.cp("dve", wT[:], pst2[:, 0:128], R=[pst2], W=[wT])
                        yield
                        pst = nps()
                        P.mm(pst[:, 0:128], wT[:], S_[:, hs], R=[wT, (S_, h)], W=[pst])
                        P.tt("dve", u[:], u0[:], pst[:, 0:128], ALU.subtract, R=[u0, pst], W=[u])
                        yield
                        pst = nps()
                        P.mm(pst[:, 0:128], TT[:, 3, :], S_[:, hs], start=True, stop=False, R=[TT, (S_, h)], W=[pst])
                        P.mm(pst[:, 0:128], AQ[:, 1, :], u[:], start=False, stop=True, R=[AQ, u], W=[pst])
                        P.cp("act", o_[:, hs], pst[:, 0:128], R=[pst], W=[(o_, h)])
                        pst = nps()
                        P.mm(pst[:, 0:128], kdd[:, hs], u[:], R=[kdd, u], W=[pst])
                        P.stt("dve", S_[:, hs], S_[:, hs], gl[:, h:h + 1], pst[:, 0:128], ALU.mult, ALU.add, R=[(S_, h), gl, pst], W=[(S_, h)])
                        yield

                    gens = [head_gen(h0 + kk, kk, HBUF[kk]) for kk in range(nh)]
                    while gens:
                        for g_ in list(gens):
                            try:
                                next(g_)
                            except StopIteration:
                                gens.remove(g_)
                P.tt("pool", t2[:, 0:G], o_[:], o_[:], ALU.mult, R=[o_], W=[t2])
                P.op("dve", lambda e: e.reduce_sum(sm[:, 0:HB], v3(t2[:, 0:G], HB), AX.X), R=[t2], W=[sm])
                P.ts("dve", sm[:, HB:2 * HB], sm[:, 0:HB], 1.0 / 128.0, EPS, ALU.mult, ALU.add, R=[sm], W=[sm])
                P.act(sm[:, 2 * HB:3 * HB], sm[:, HB:2 * HB], AF.Sqrt, R=[sm], W=[sm])
                P.op("dve", lambda e: e.reciprocal(sm[:, 3 * HB:4 * HB], sm[:, 2 * HB:3 * HB]), R=[sm], W=[sm])
                P.tt("dve", v3(o_[:], HB), v3(o_[:], HB), bc3(sm[:, 3 * HB:4 * HB], HB, 128), ALU.mult, R=[o_, sm], W=[o_])
                P.tt("pool", v3(o_[:], HB), v3(o_[:], HB), nw[:].rearrange("p (o d) -> p o d", o=1).to_broadcast([128, HB, 128]), ALU.mult,
                     R=[o_, nw], W=[o_])
                P.act(zt[:], zt[:], AF.Silu, R=[zt], W=[zt])
                P.tt("dve", o_[:], o_[:], zt[:], ALU.mult, R=[o_, zt], W=[o_])
                P.dma("act", mixed[t * 128:(t + 1) * 128, G:2 * G], o_[:], R=[o_], W=[(mixed, t, 1)])

    def stage_rope_once(l):
        if l != 0:
            return
        TWO_PI = 2.0 * math.pi
        with scope() as st:
            ii = P.sb("rii", [128, 96], I32, st)
            inv = P.sb("rinv", [128, 96], F32, st)
            P.op("pool", lambda e: e.iota(ii[:, 0:64], [[1, 64]], base=0, channel_multiplier=0), W=[ii])
            P.op("pool", lambda e: e.iota(ii[:, 64:96], [[1, 32]], base=0, channel_multiplier=0), W=[ii])
            P.cp("dve", inv[:], ii[:], R=[ii], W=[inv])
            P.act(inv[:, 0:64], inv[:, 0:64], AF.Exp, scale=-2.0 * math.log(ROPE_THETA) / 128.0, R=[inv], W=[inv])
            P.act(inv[:, 64:96], inv[:, 64:96], AF.Exp, scale=-2.0 * math.log(ROPE_THETA) / 64.0, R=[inv], W=[inv])
            pi_ = P.sb("rpi", [128, 1], I32, st); pf = P.sb("rpf", [128, 1], F32, st)
            y = P.sb("ry", [128, 96], F32, st); ki = P.sb("rki", [128, 96], I32, st); kf = P.sb("rkf", [128, 96], F32, st)
            m1 = P.sb("rm1", [128, 96], F32, st)
            tab = P.sb("rtab", [128, 192], F32, st)
            for t in range(NT):
                P.dma("sp", pi_[:], pos_in[t * 128:(t + 1) * 128, :], W=[pi_])
                P.cp("dve", pf[:], pi_[:], R=[pi_], W=[pf])
                for which, shift in ((0, 0.25), (1, 0.0)):
                    P.ts("dve", y[:], inv[:], pf[:, 0:1], 1.0 / TWO_PI, ALU.mult, ALU.mult, R=[inv, pf], W=[y])
                    if shift:
                        P.ts("dve", y[:], y[:], shift, None, ALU.add, R=[y], W=[y])
                    P.cp("dve", ki[:], y[:], R=[y], W=[ki])
                    P.cp("dve", kf[:], ki[:], R=[ki], W=[kf])
                    P.tt("dve", y[:], y[:], kf[:], ALU.subtract, R=[y, kf], W=[y])
                    P.ts("dve", m1[:], y[:], 0.5, None, ALU.is_gt, R=[y], W=[m1])
                    P.tt("dve", y[:], y[:], m1[:], ALU.subtract, R=[y, m1], W=[y])
                    P.ts("dve", m1[:], y[:], -0.5, None, ALU.is_lt, R=[y], W=[m1])
                    P.tt("dve", y[:], y[:], m1[:], ALU.add, R=[y, m1], W=[y])
                    P.act(tab[:, which * 64:which * 64 + 64], y[:, 0:64], AF.Sin, scale=TWO_PI, R=[y], W=[tab])
                    P.act(tab[:, 128 + which * 32:160 + which * 32], y[:, 64:96], AF.Sin, scale=TWO_PI, R=[y], W=[tab])
                P.dma("act", rope[t * 128:(t + 1) * 128, :], tab[:], R=[tab], W=[(rope, t)])

    def rope_apply(dst, src, cs, sn, H, half, t1, t2_):
        def bc(ap):
            return ap.rearrange("p (o d) -> p o d", o=1).to_broadcast([128, H, half])
        s4 = src.rearrange("p (h two d) -> p h two d", h=H, two=2)
        d4 = dst.rearrange("p (h two d) -> p h two d", h=H, two=2)
        a1 = t1.rearrange("p (h d) -> p h d", h=H); a2 = t2_.rearrange("p (h d) -> p h d", h=H)
        return s4, d4, a1, a2, bc(cs), bc(sn)

    def stage_attn(l):
        NIT = 26
        psmod[0] = 7
        p_o = PS[7]
        with scope() as st:
            kT = P.sb("akT", [128, HA, S], BF16, st)
            vA = P.sb("avA", [128, NT, HA, 128], BF16, st)
            kiT = P.sb("akiT", [64, S], BF16, st)
            NL = P.sb("aNL", [128, 128], F32, st); P.dma("sp", NL[:], CS["c_NL"][:, :], W=[NL])
            lng = P.sb("alng", [128, 64], F32, st); lnb = P.sb("alnb", [128, 64], F32, st)
            bcast_row(lng, Wt["idx_kn_g"][l:l + 1, :], 64); bcast_row(lnb, Wt["idx_kn_b"][l:l + 1, :], 64)
            rp = [P.sb("arp%d" % i, [128, 192], F32, st) for i in range(2)]
            xa_ = P.sb("axa", [128, G], F32, st); xr = P.sb("axr", [128, G], F32, st)
            t1 = P.sb("at1", [128, 512], F32, st); t2_ = P.sb("at2", [128, 512], F32, st)
            qi = P.sb("aqi", [128, 1024], F32, st)
            vt = qi
            kit = P.sb("akit", [128, 64], F32, st); kir = P.sb("akir", [128, 64], F32, st)
            sm = P.sb("asm", [128, 16], F32, st)

            def do_rope(dst, src, H, half, cs, sn, W_):
                s4, d4, a1, a2, cb, sb_ = rope_apply(dst, src, cs, sn, H, half, t1[:, 0:H * half], t2_[:, 0:H * half])
                P.tt("dve", a1, s4[:, :, 0, :], cb, ALU.mult, R=[W_[0], W_[2]], W=[t1])
                P.tt("pool", a2, s4[:, :, 1, :], sb_, ALU.mult, R=[W_[0], W_[2]], W=[t2_])
                P.tt("dve", d4[:, :, 0, :], a1, a2, ALU.subtract, R=[t1, t2_], W=[W_[1]])
                P.tt("dve", a1, s4[:, :, 1, :], cb, ALU.mult, R=[W_[0], W_[2]], W=[t1])
                P.tt("pool", a2, s4[:, :, 0, :], sb_, ALU.mult, R=[W_[0], W_[2]], W=[t2_])
                P.tt("dve", d4[:, :, 1, :], a1, a2, ALU.add, R=[t1, t2_], W=[W_[1]])

            for t in range(NT):
                r_ = rp[t % 2]
                P.dma("sp", r_[:], rope[t * 128:(t + 1) * 128, :], R=[(rope, t)], W=[r_])
                ldp(xa_, t, "a_k"); ldp(vt, t, "a_v"); ldp(kit, t, "a_ki")
                do_rope(xr[:], xa_[:], HA, 64, r_[:, 0:64], r_[:, 64:128], (xa_, xr, r_))
                for h0 in range(0, HA, 4):
                    nh = min(4, HA - h0)
                    pst = nps()
                    for kk in range(nh):
                        P.tr(pst[:, kk * 128:(kk + 1) * 128], xr[:, (h0 + kk) * 128:(h0 + kk + 1) * 128], ident[:], R=[xr, ident], W=[pst])
                    P.cp("act", kT[:, h0:h0 + nh, t * 128:(t + 1) * 128], v3(pst[:, 0:nh * 128], nh), R=[pst], W=[(kT, t)])
                P.cp("pool", vA[:, t, :, :], v3(vt[:, 0:G], HA), R=[vt], W=[(vA, t)])
                P.act(kir[:], kit[:], AF.Identity, accum=sm[:, 0:1], R=[kit], W=[kir, sm])
                P.act(kir[:], kit[:], AF.Square, accum=sm[:, 1:2], R=[kit], W=[kir, sm])
                P.ts("dve", sm[:, 2:3], sm[:, 0:1], 1.0 / 64.0, None, ALU.mult, R=[sm], W=[sm])
                P.tt("dve", sm[:, 3:4], sm[:, 2:3], sm[:, 2:3], ALU.mult, R=[sm], W=[sm])
                P.stt("dve", sm[:, 4:5], sm[:, 1:2], 1.0 / 64.0, sm[:, 3:4], ALU.mult, ALU.subtract, R=[sm], W=[sm])
                P.ts("dve", sm[:, 4:5], sm[:, 4:5], EPS, None, ALU.add, R=[sm], W=[sm])
                P.act(sm[:, 5:6], sm[:, 4:5], AF.Sqrt, R=[sm], W=[sm])
                P.op("dve", lambda e: e.reciprocal(sm[:, 6:7], sm[:, 5:6]), R=[sm], W=[sm])
                P.ts("dve", kit[:], kit[:], sm[:, 2:3], sm[:, 6:7], ALU.subtract, ALU.mult, R=[kit, sm], W=[kit])
                P.tt("dve", kit[:], kit[:], lng[:], ALU.mult, R=[kit, lng], W=[kit])
                P.tt("dve", kit[:], kit[:], lnb[:], ALU.add, R=[kit, lnb], W=[kit])
                do_rope(kir[:], kit[:], 1, 32, r_[:, 128:160], r_[:, 160:192], (kit, kir, r_))
                pst = nps()
                P.tr(pst[0:64, 0:128], kir[:], ident[:], R=[kir, ident], W=[pst])
                P.cp("act", kiT[:, t * 128:(t + 1) * 128], pst[0:64, 0:128], R=[pst], W=[(kiT, t)])
            if BIS == 6:
                NTQ = 0
            else:
                NTQ = NT
            qT = P.sb("aqT", [128, HA, 128], BF16, st)
            qir = P.sb("aqir", [128, 1024], F32, st)
            qiT = P.sb("aqiT", [64, 16, 128], BF16, st)
            wi = P.sb("awi", [128, 16], F32, st)
            acc = P.sb("aacc", [128, S], F32, st)
            maskb = P.sb("amask", [128, S], BF16, st)
            rl = [P.sb("arl%d" % i, [128, 512], F32, st) for i in range(2)]
            bs = P.sb("abs", [128, 8], F32, st)
            mxc = P.sb("amxc", [128, 16], F32, st)
            pT = [P.sb("apT%d" % i, [128, 4, 128], BF16, st) for i in range(2)]
            o_ = xa_
            for qb in range(NTQ):
                Sk = (qb + 1) * 128
                r_ = rp[qb % 2]
                P.dma("sp", r_[:], rope[qb * 128:(qb + 1) * 128, :], R=[(rope, qb)], W=[r_])
                ldp(xa_, qb, "a_q"); ldp(qi, qb, "a_qi"); ldp(wi, qb, "a_wi")
                do_rope(xr[:], xa_[:], HA, 64, r_[:, 0:64], r_[:, 64:128], (xa_, xr, r_))
                for h0 in range(0, HA, 4):
                    nh = min(4, HA - h0)
                    pst = nps()
                    for kk in range(nh):
                        P.tr(pst[:, kk * 128:(kk + 1) * 128], xr[:, (h0 + kk) * 128:(h0 + kk + 1) * 128], ident[:], R=[xr, ident], W=[pst])
                    P.act(qT[:, h0:h0 + nh, :], v3(pst[:, 0:nh * 128], nh), AF.Copy, scale=128.0 ** -0.5, R=[pst], W=[qT])
                do_rope(qir[:], qi[:], 16, 32, r_[:, 128:160], r_[:, 160:192], (qi, qir, r_))
                for h0 in range(0, 16, 4):
                    pst = nps()
                    for kk in range(4):
                        P.tr(pst[0:64, kk * 128:(kk + 1) * 128], qir[:, (h0 + kk) * 64:(h0 + kk + 1) * 64], ident[:], R=[qir, ident], W=[pst])
                    P.cp("act", qiT[:, h0:h0 + 4, :], v3(pst[0:64, 0:512], 4), R=[pst], W=[qiT])
                P.ts("dve", wi[:], wi[:], 1.0 / 32.0, None, ALU.mult, R=[wi], W=[wi])
                k_ = 0
                for c0 in range(0, Sk, 512):
                    w = min(512, Sk - c0)
                    for hi in range(16):
                        pst = nps()
                        P.mm(pst[:, 0:w], qiT[:, hi, :], kiT[:, c0:c0 + w], R=[qiT, kiT], W=[pst])
                        r2 = rl[k_ % 2]; k_ += 1
                        P.act(r2[:, 0:w], pst[:, 0:w], AF.Relu, R=[pst], W=[r2])
                        if hi == 0:
                            P.ts("dve", acc[:, c0:c0 + w], r2[:, 0:w], wi[:, 0:1], None, ALU.mult, R=[r2, wi], W=[acc])
                        else:
                            P.stt("dve", acc[:, c0:c0 + w], r2[:, 0:w], wi[:, hi:hi + 1], acc[:, c0:c0 + w], ALU.mult, ALU.add,
                                  R=[r2, wi, acc], W=[acc])
                if BIS == 7:
                    continue
                P.op("dve", lambda e, Sk=Sk: e.reduce_max(bs[:, 0:1], acc[:, 0:Sk], AX.X), R=[acc], W=[bs])
                P.op("dve", lambda e, Sk=Sk: e.tensor_reduce(bs[:, 1:2], acc[:, 0:Sk], AX.X, ALU.min), R=[acc], W=[bs])
                P.tt("dve", acc[:, Sk - 128:Sk], acc[:, Sk - 128:Sk], NL[:], ALU.add, R=[acc, NL], W=[acc])
                P.tt("dve", bs[:, 2:3], bs[:, 0:1], bs[:, 1:2], ALU.subtract, R=[bs], W=[bs])
                P.ts("dve", bs[:, 2:3], bs[:, 2:3], 1.0001, 1e-6, ALU.mult, ALU.add, R=[bs], W=[bs])
                P.ts("dve", bs[:, 3:4], bs[:, 1:2], -1e-6, None, ALU.add, R=[bs], W=[bs])
                for it in range(1, NIT + 1):
                    sc = 2.0 ** -it
                    P.stt("dve", bs[:, 4:5], bs[:, 2:3], sc, bs[:, 3:4], ALU.mult, ALU.add, R=[bs], W=[bs])
                    P.ts("dve", maskb[:, 0:Sk], acc[:, 0:Sk], bs[:, 4:5], None, ALU.is_ge, ALU.add, accum=bs[:, 5:6],
                         R=[acc, bs], W=[maskb, bs])
                    P.ts("dve", bs[:, 6:7], bs[:, 5:6], KSEL - 0.5, sc, ALU.is_ge, ALU.mult, R=[bs], W=[bs])
                    P.stt("dve", bs[:, 3:4], bs[:, 6:7], bs[:, 2:3], bs[:, 3:4], ALU.mult, ALU.add, R=[bs], W=[bs])
                P.ts("dve", maskb[:, 0:Sk], acc[:, 0:Sk], bs[:, 3:4], None, ALU.is_ge, R=[acc, bs], W=[maskb])
                if BIS == 8:
                    continue
                for h in range(HA):
                    nch = 0
                    for c0 in range(0, Sk, 512):
                        w = min(512, Sk - c0)
                        pst = nps()
                        P.mm(pst[:, 0:w], qT[:, h, :], kT[:, h, c0:c0 + w], R=[qT, kT], W=[pst])
                        P.ts("dve", acc[:, c0:c0 + w], pst[:, 0:w], 1.0, None, ALU.mult, ALU.max, accum=mxc[:, nch:nch + 1],
                             R=[pst], W=[acc, mxc])
                        nch += 1
                    P.op("dve", lambda e, nch=nch: e.reduce_max(bs[:, 7:8], mxc[:, 0:nch], AX.X), R=[mxc], W=[bs])
                    P.ts("dve", bs[:, 7:8], bs[:, 7:8], -1.0, None, ALU.mult, R=[bs], W=[bs])
                    P.act(acc[:, 0:Sk], acc[:, 0:Sk], AF.Exp, bias=bs[:, 7:8], R=[acc, bs], W=[acc])
                    P.tt("pool", acc[:, 0:Sk], acc[:, 0:Sk], maskb[:, 0:Sk], ALU.mult, R=[acc, maskb], W=[acc])
                    P.op("dve", lambda e, Sk=Sk: e.reduce_sum(bs[:, 5:6], acc[:, 0:Sk], AX.X), R=[acc], W=[bs])
                    nj = Sk // 128
                    for j0 in range(0, nj, 4):
                        nn = min(4, nj - j0)
                        pst = nps()
                        for jj in range(nn):
                            P.tr(pst[:, jj * 128:(jj + 1) * 128], acc[:, (j0 + jj) * 128:(j0 + jj + 1) * 128], ident[:], R=[acc, ident], W=[pst])
                        pt = pT[(j0 // 4) % 2]
                        P.cp("act", pt[:, 0:nn, :], v3(pst[:, 0:nn * 128], nn), R=[pst], W=[pt])
                        for jj in range(nn):
                            j = j0 + jj
                            P.mm(p_o[:, 0:128], pt[:, jj, :], vA[:, j, h, :], start=(j == 0), stop=(j == nj - 1), R=[pt, vA], W=[p_o])
                    P.op("dve", lambda e: e.reciprocal(bs[:, 6:7], bs[:, 5:6]), R=[bs], W=[bs])
                    P.ts("dve", o_[:, h * 128:(h + 1) * 128], p_o[:, 0:128], bs[:, 6:7], None, ALU.mult, R=[p_o, bs], W=[o_])
                P.dma("act", mixed[qb * 128:(qb + 1) * 128, 0:G], o_[:], R=[o_], W=[(mixed, qb, 0)])
        psmod[0] = 8

    def ln_pass(src, dst, gname, bname, l, st, extra=None):
        P.barrier()
        gbc = P.sb("lng", [128, DM], F32, st); bbc = P.sb("lnb", [128, DM], F32, st)
        bcast_row(gbc, Wt[gname][l:l + 1, :], DM); bcast_row(bbc, Wt[bname][l:l + 1, :], DM)
        xt2 = [P.sb("lnx%d" % i, [128, DM], F32, st) for i in range(2)]
        junk = P.sb("lnj", [128, DM], F32, st)
        sm = P.sb("lnsm", [128, 8], F32, st)
        for t in range(NT):
            xt = xt2[t % 2]
            P.dma("sp", xt[:], src[t * 128:(t + 1) * 128, :], R=[(src, t)], W=[xt])
            P.act(junk[:], xt[:], AF.Identity, accum=sm[:, 0:1], R=[xt], W=[junk, (sm, 0)])
            P.act(junk[:], xt[:], AF.Square, accum=sm[:, 1:2], R=[xt], W=[junk, (sm, 1)])
            P.ts("dve", sm[:, 2:3], sm[:, 0:1], 1.0 / DM, None, ALU.mult, R=[(sm, 0)], W=[(sm, 2)])
            P.tt("dve", sm[:, 3:4], sm[:, 2:3], sm[:, 2:3], ALU.mult, R=[(sm, 2)], W=[(sm, 3)])
            P.stt("dve", sm[:, 4:5], sm[:, 1:2], 1.0 / DM, sm[:, 3:4], ALU.mult, ALU.subtract, R=[(sm, 1), (sm, 3)], W=[(sm, 4)])
            P.ts("dve", sm[:, 4:5], sm[:, 4:5], EPS, None, ALU.add, R=[(sm, 4)], W=[(sm, 4)])
            P.act(sm[:, 5:6], sm[:, 4:5], AF.Sqrt, R=[(sm, 4)], W=[(sm, 5)])
            P.op("dve", lambda e: e.reciprocal(sm[:, 6:7], sm[:, 5:6]), R=[(sm, 5)], W=[(sm, 6)])
            P.ts("dve", xt[:], xt[:], sm[:, 2:3], sm[:, 6:7], ALU.subtract, ALU.mult, R=[xt, (sm, 2), (sm, 6)], W=[xt])
            P.tt("pool", xt[:], xt[:], gbc[:], ALU.mult, R=[xt, gbc], W=[xt])
            P.tt("dve", xt[:], xt[:], bbc[:], ALU.add, R=[xt, bbc], W=[xt])
            P.dma("act", dst[t * 128:(t + 1) * 128, :], xt[:], R=[xt], W=[(dst, t)])
            if extra is not None:
                extra(t, xt)

    def resid_epilogue(resid, gate_part, dst, st):
        gbc = P.sb("gbc", [128, DM], F32, st)
        bcast_row(gbc, modv.t.ap()[0:1, gate_part * DM:(gate_part + 1) * DM], DM)
        P.ts("dve", gbc[:], gbc[:], 1.0, None, ALU.add, R=[gbc], W=[gbc])
        obs = [P.sb("rob%d" % i, [128, 512], F32, st) for i in range(2)]
        xts = [P.sb("rxt%d" % i, [128, 512], F32, st) for i in range(2)]
        cnt = [0]

        def ep(t0, n0, w, pst, R):
            ob = obs[cnt[0] % 2]; xt = xts[cnt[0] % 2]; cnt[0] += 1
            P.dma("sp", xt[:, 0:w], resid[t0:t0 + 128, n0:n0 + w], R=[(resid, t0 // 128)], W=[xt])
            P.tt("dve", ob[:, 0:w], pst[:, 0:w], gbc[:, n0:n0 + w], ALU.mult, R=R + [gbc], W=[ob])
            P.stt("dve", ob[:, 0:w], xt[:, 0:w], ALPHA, ob[:, 0:w], ALU.mult, ALU.add, R=[xt, ob], W=[ob])
            P.dma("act", dst[t0:t0 + 128, n0:n0 + w], ob[:, 0:w], R=[ob], W=[(dst, t0 // 128)])
        return ep

    def stage_out(l, xin, xo):
        NE = N_EXP * EXP_FF
        with scope() as st:
            wscr, NTL, nt = cast_weights(lambda n0, w: Wt["w_out"][l][:, n0:n0 + w], DM, DM, "wout_bf%d" % l)
            gemm(mixed, DM, wscr, NTL, nt, DM, resid_epilogue(xin, 2, x1, st), None, name="wo")
        gates_d = dscr("gates%d" % l, [S, 32])
        with scope() as st:
            s2 = mod_cols(st, 4, True); sh2 = mod_cols(st, 3, False)
            Wr = P.sb("Wr", [128, KD, 36], F32, st)
            P.dma("sp", Wr[:, :, 0:4], Wt["router_g_w"][l].rearrange("(k p) g -> p k g", p=128), W=[Wr])
            P.dma("sp", Wr[:, :, 4:36], Wt["router_e_w"][l].rearrange("(k p) g -> p k g", p=128), W=[Wr])
            rb = P.sb("rb", [128, 36], F32, st)
            P.dma("sp", rb[:, 0:4], Wt["router_g_b"][l:l + 1, :].to_broadcast([128, 4]), W=[rb])
            P.dma("sp", rb[:, 4:36], Wt["router_e_b"][l:l + 1, :].to_broadcast([128, 32]), W=[rb])
            hT = P.sb("rhT", [128, KD, 128], F32, st)
            lg = P.sb("lg", [128, 36], F32, st)
            w_ = P.sb("rw_", [128, 96], F32, st)
            gt = P.sb("gt", [128, 32], F32, st)

            def router(t, xt):
                for kk in range(KD):
                    if kk % 4 == 0:
                        pst = nps()
                    P.tr(pst[:, (kk % 4) * 128:(kk % 4 + 1) * 128], xt[:, kk * 128:(kk + 1) * 128], ident[:], R=[xt, ident], W=[pst])
                    P.act(hT[:, kk, :], pst[:, (kk % 4) * 128:(kk % 4 + 1) * 128], AF.Identity, scale=s2[:, kk:kk + 1],
                          bias=sh2[:, kk:kk + 1], R=[pst, s2, sh2], W=[hT])
                pst = nps()
                for kk in range(KD):
                    P.mm(pst[:, 0:36], hT[:, kk, :], Wr[:, kk, :], start=(kk == 0), stop=(kk == KD - 1), R=[hT, Wr], W=[pst])
                P.tt("dve", lg[:], pst[:, 0:36], rb[:], ALU.add, R=[pst, rb], W=[lg])
                P.op("dve", lambda e: e.reduce_max(w_[:, 0:1], lg[:, 0:4], AX.X), R=[lg], W=[w_])
                P.ts("dve", w_[:, 4:8], lg[:, 0:4], w_[:, 0:1], None, ALU.is_equal, R=[lg, w_], W=[w_])
                P.ts("dve", w_[:, 1:2], w_[:, 0:1], -1.0, None, ALU.mult, R=[w_], W=[w_])
                P.act(w_[:, 8:12], lg[:, 0:4], AF.Exp, bias=w_[:, 1:2], accum=w_[:, 2:3], R=[lg, w_], W=[w_])
                P.op("dve", lambda e: e.reciprocal(w_[:, 3:4], w_[:, 2:3]), R=[w_], W=[w_])
                P.ts("dve", w_[:, 12:16], w_[:, 4:8], -NEG, NEG, ALU.mult, ALU.add, R=[w_], W=[w_])
                P.tt("dve", w_[:, 16:48].rearrange("p (g e) -> p g e", g=4), lg[:, 4:36].rearrange("p (g e) -> p g e", g=4),
                     w_[:, 12:16].to_broadcast([128, 4, 8]) if False else w_[:, 12:16].rearrange("p (g o) -> p g o", o=1).to_broadcast([128, 4, 8]),
                     ALU.add, R=[lg, w_], W=[w_])
                P.op("dve", lambda e: e.reduce_max(w_[:, 48:49], w_[:, 16:48], AX.X), R=[w_], W=[w_])
                P.ts("dve", w_[:, 56:88], w_[:, 16:48], w_[:, 48:49], None, ALU.is_equal, R=[w_], W=[w_])
                P.stt("dve", gt[:], w_[:, 56:88], NEG, w_[:, 16:48], ALU.mult, ALU.add, R=[w_], W=[gt])
                P.op("dve", lambda e: e.reduce_max(w_[:, 49:50], gt[:], AX.X), R=[gt], W=[w_])
                P.ts("dve", gt[:], gt[:], w_[:, 49:50], None, ALU.is_equal, R=[gt, w_], W=[gt])
                P.tt("dve", w_[:, 50:51], w_[:, 48:49], w_[:, 49:50], ALU.subtract, R=[w_], W=[w_])
                P.act(w_[:, 51:52], w_[:, 50:51], AF.Sigmoid, R=[w_], W=[w_])
                P.ts("dve", w_[:, 52:53], w_[:, 51:52], -1.0, 1.0, ALU.mult, ALU.add, R=[w_], W=[w_])
                P.tt("dve", w_[:, 51:52], w_[:, 51:52], w_[:, 3:4], ALU.mult, R=[w_], W=[w_])
                P.tt("dve", w_[:, 52:53], w_[:, 52:53], w_[:, 3:4], ALU.mult, R=[w_], W=[w_])
                P.ts("dve", gt[:], gt[:], w_[:, 52:53], None, ALU.mult, R=[gt, w_], W=[gt])
                P.stt("dve", gt[:], w_[:, 56:88], w_[:, 51:52], gt[:], ALU.mult, ALU.add, R=[gt, w_], W=[gt])
                P.dma("act", gates_d[t * 128:(t + 1) * 128, :], gt[:], R=[gt], W=[(gates_d, t)])
            ln_pass(x1, x1, "ln1_g", "ln1_b", l, st, extra=router)
        for wn, dst in (("exp_w_gate", hg_d), ("exp_w_up", hu_d)):
            with scope() as st:
                scr = dscr("%s_bf%d" % (wn, l), [NE // 512, 128, KD, 512], BF16)
                for t in range(NE // 512):
                    for e_ in range(2):
                        castq_n[0] += 1
                        P.dma("pool", scr[t][:, :, e_ * 256:(e_ + 1) * 256],
                              Wt[wn][l, 2 * t + e_].rearrange("(kd p) f -> p kd f", p=128), W=[(scr, t), (castq, castq_n[0] % 2)])
                s2 = mod_cols(st, 4, True); sh2 = mod_cols(st, 3, False)

                def prologue(kk, out_ap, ps_ap, R, W, s2=s2, sh2=sh2):
                    P.act(out_ap, ps_ap, AF.Identity, scale=s2[:, kk:kk + 1], bias=sh2[:, kk:kk + 1], R=R + [s2, sh2], W=W)
                gemm(x1, DM, scr, 512, NE // 512, NE, store_epilogue(dst, st), prologue, name="ex")
        P.barrier()
        with scope() as st:
            CW = 2048
            a2 = [P.sb("ea%d" % i, [128, CW], F32, st) for i in range(2)]
            b2 = [P.sb("eb%d" % i, [128, CW], F32, st) for i in range(2)]
            g2 = [P.sb("eg%d" % i, [128, 32], F32, st) for i in range(2)]
            k = 0
            for t in range(NT):
                gtl = g2[t % 2]
                P.dma("sp", gtl[:], gates_d[t * 128:(t + 1) * 128, :], R=[(gates_d, t)], W=[gtl])
                for c0 in range(0, NE, CW):
                    a_, b_ = a2[k % 2], b2[k % 2]; k += 1
                    P.dma("sp", a_[:], hg_d[t * 128:(t + 1) * 128, c0:c0 + CW], R=[hg_d], W=[a_])
                    P.dma("sp", b_[:], hu_d[t * 128:(t + 1) * 128, c0:c0 + CW], R=[hu_d], W=[b_])
                    P.act(a_[:], a_[:], AF.Silu, R=[a_], W=[a_])
                    P.tt("dve", a_[:], a_[:], b_[:], ALU.mult, R=[a_, b_], W=[a_])
                    for e_ in range(CW // 256):
                        ee = c0 // 256 + e_
                        P.ts("pool", a_[:, e_ * 256:(e_ + 1) * 256], a_[:, e_ * 256:(e_ + 1) * 256], gtl[:, ee:ee + 1], None, ALU.mult,
                             R=[a_, gtl], W=[a_])
                    P.dma("act", hg_d[t * 128:(t + 1) * 128, c0:c0 + CW], a_[:], R=[a_], W=[hg_d])
        with scope() as st:
            wscr, NTL, nt = cast_weights(lambda n0, w: Wt["exp_w_down"][l].rearrange("e f d -> (e f) d")[:, n0:n0 + w], NE, DM, "wdn_bf%d" % l)
            gemm(hg_d, NE, wscr, NTL, nt, DM, resid_epilogue(x1, 5, xo, st), None, name="dn")
        with scope() as st:
            ln_pass(xo, xo, "ln2_g", "ln2_b", l, st)

    xin = x_in
    final = []
    for l in range(NL):
        stage_mod(l)
        if STAGES <= 0:
            break
        stage_proj(l, xin)
        if STAGES <= 1:
            break
        if STAGES == 2:
            if not debug:
                with scope() as st:
                    zt = P.sb("zt", [128, DM], F32, st)
                    P.memset("dve", zt[:], 0.0, W=[zt])
                    for t in range(NT):
                        P.dma("sp", mixed[t * 128:(t + 1) * 128, :], zt[:], R=[zt], W=[(mixed, t)])
            xo = y_out
            stage_out(l, xin, xo)
            break
        if "a" in MIX:
            stage_rope_once(l)
            if BIS != 5:
                stage_attn(l)
        if "b" in MIX:
            stage_gdn(l)
        if "c" in MIX:
            stage_gla(l)
        if "d" in MIX:
            stage_ssd(l)
        xo = y_out if l == NL - 1 else (xa if l % 2 == 0 else xb)
        stage_out(l, xin, xo)
        xin = xo
    P.barrier()
    toks = []
    for tl in (y_out, proj, mixed, x1, modv):
        for s, stt_ in tl.st.items():
            if stt_[0] is not None:
                toks.append(stt_[0])
    P.finish(toks, "sp")
    P.emit()
    return nc, c


_CACHE = {}


def kernel(**inputs):
    DM, S, DEPTH, B = 4096, 4096, 4, 2
    if "nc" not in _CACHE:
        _CACHE["nc"] = build(DM, S, DEPTH, DEPTH, debug=False)[0]
    nc = _CACHE["nc"]
    consts = host_consts()
    x = np.asarray(inputs["x"], np.float32)
    c = np.asarray(inputs["c"], np.float32)
    pos = np.asarray(inputs["positions"]).astype(np.int32)
    maps = []
    for b in range(B):
        m = {"x": np.ascontiguousarray(x[b]), "c": np.ascontiguousarray(c[b:b + 1]),
             "pos": np.ascontiguousarray(pos[b].reshape(S, 1))}
        for n in WEIGHTS:
            m[n] = np.ascontiguousarray(np.asarray(inputs[n], np.float32))
        m.update(consts)
        maps.append(m)
    res = run_bass_kernel_spmd(nc, maps, core_ids=list(range(B)))
    return np.stack([np.asarray(res.results[b]["y"], np.float32) for b in range(B)], axis=0)
```

```python
import math
import numpy as np
import concourse.bass as bass
import concourse.mybir as mybir
from concourse.bass_utils import run_bass_kernel_spmd
from contextlib import ExitStack

F32 = mybir.dt.float32
BF16 = mybir.dt.bfloat16
I32 = mybir.dt.int32
ALU = mybir.AluOpType
AF = mybir.ActivationFunctionType
AX = mybir.AxisListType
ENGS = ("pe", "act", "dve", "pool", "sp")
EPOCH = 30000
NEG = -1.0e30


class T:
    def __init__(self, name, h):
        self.name = name
        self.t = h
        self.st = {}

    def __getitem__(self, k):
        return self.t[k]


class Prog:
    def __init__(self, nc, n_dma_slots=32):
        self.nc = nc
        self.es = ExitStack()
        self.ops = {e: [] for e in ENGS}
        self.cnt = {e: 0 for e in ENGS}
        self.known = {e: {} for e in ENGS}
        self.sem = {}
        self.dma_slots = []
        for i in range(n_dma_slots):
            k = ("d", i)
            self.sem[k] = self.es.enter_context(nc.semaphore("d%d" % i))
            self.dma_slots.append([k, 0])
        self.dma_rr = 0
        self.n_t = 0
        self.n_ops = 0

    def _semkey(self, eng):
        k = (eng, self.cnt[eng] // EPOCH)
        if k not in self.sem:
            self.sem[k] = self.es.enter_context(self.nc.semaphore("s_%s_%d" % k))
        return k

    def sb(self, name, shape, dt=F32, stack=None):
        self.n_t += 1
        h = (stack or self.es).enter_context(self.nc.sbuf_tensor("%s_%d" % (name, self.n_t), list(shape), dt))
        return T(name, h)

    def ps(self, name, shape, dt=F32, stack=None):
        self.n_t += 1
        h = (stack or self.es).enter_context(self.nc.psum_tensor("%s_%d" % (name, self.n_t), list(shape), dt))
        t = T(name, h)
        t.psum = True
        return t

    def _fix(self, R, W):
        R2, W2 = [], []
        for a in R:
            t = a if isinstance(a, T) else a[0]
            if getattr(t, "psum", False):
                W2.append(t)
            else:
                R2.append(a)
        for a in W:
            t = a if isinstance(a, T) else a[0]
            W2.append(t if getattr(t, "psum", False) else a)
        return R2, W2

    @staticmethod
    def _norm(acc):
        out = []
        for a in acc:
            if isinstance(a, T):
                out.append((a, (None,)))
            else:
                out.append((a[0], tuple(a[1:]) if len(a) > 1 else (None,)))
        return out

    @staticmethod
    def _subs(t, subs):
        if None in subs:
            return list(t.st.keys()) + ([None] if None not in t.st else [])
        return list(subs) + [None]

    def _deps(self, reads, writes):
        deps = []
        for t, subs in self._norm(reads):
            for s in self._subs(t, subs):
                st = t.st.get(s)
                if st and st[0] is not None:
                    deps.append(st[0])
        for t, subs in self._norm(writes):
            for s in self._subs(t, subs):
                st = t.st.get(s)
                if st:
                    if st[0] is not None:
                        deps.append(st[0])
                    deps.extend(st[1].items())
        return deps

    def _commit(self, reads, writes, tok):
        for t, subs in self._norm(reads):
            for s in subs:
                st = t.st.setdefault(s, [None, {}])
                if st[1].get(tok[0], 0) < tok[1]:
                    st[1][tok[0]] = tok[1]
        for t, subs in self._norm(writes):
            if None in subs:
                t.st = {None: [tok, {}]}
            else:
                for s in subs:
                    t.st[s] = [tok, {}]

    def _waits(self, eng, deps):
        kn = self.known[eng]
        need = {}
        for k, v in deps:
            if k[0] == "pe" and eng == "pe":
                continue
            if kn.get(k, 0) < v:
                need[k] = max(need.get(k, 0), v)
        for k, v in need.items():
            kn[k] = v
        return list(need.items())

    def op(self, eng, fn, R=(), W=()):
        R, W = self._fix(R, W)
        deps = self._deps(R, W)
        waits = self._waits(eng, deps)
        k = self._semkey(eng)
        self.cnt[eng] += 1
        tok = (k, self.cnt[eng] - k[1] * EPOCH)
        self.ops[eng].append((waits, fn, (k, 1)))
        self._commit(R, W, tok)
        self.n_ops += 1
        return tok

    def dma(self, eng, out, in_, R=(), W=(), **kw):
        R, W = self._fix(R, W)
        deps = self._deps(R, W)
        slot = self.dma_slots[self.dma_rr]
        self.dma_rr = (self.dma_rr + 1) % len(self.dma_slots)
        if slot[1] > 0:
            deps.append((slot[0], slot[1]))
        waits = self._waits(eng, deps)
        slot[1] += 16
        tok = (slot[0], slot[1])

        def fn(e, out=out, in_=in_, kw=kw):
            return e.dma_start(out=out, in_=in_, **kw)
        self.ops[eng].append((waits, fn, (slot[0], 16)))
        self._commit(R, W, tok)
        self.n_ops += 1
        return tok

    def barrier(self):
        toks = []
        for e in ENGS:
            if self.cnt[e] > 0:
                k = (e, (self.cnt[e] - 1) // EPOCH)
                toks.append((k, self.cnt[e] - k[1] * EPOCH))
        for slot in self.dma_slots:
            if slot[1] > 0:
                toks.append((slot[0], slot[1]))
        for e in ENGS:
            w = self._waits(e, toks)
            if w:
                self.ops[e].append((w, None, None))

    def finish(self, toks, eng="sp"):
        self.ops[eng].append((self._waits(eng, list(toks)), None, None))

    def emit(self):
        nc = self.nc
        engmap = {"pe": "tensor", "act": "scalar", "dve": "vector", "pool": "gpsimd", "sp": "sync"}
        with nc.Block() as block:
            for e in ENGS:
                ops = self.ops[e]
                if not ops:
                    continue

                def body(engine, ops=ops):
                    for waits, fn, inc in ops:
                        for k, v in waits:
                            engine.wait_ge(self.sem[k], v)
                        if fn is not None:
                            fn(engine).then_inc(self.sem[inc[0]], inc[1])
                getattr(block, engmap[e])(body)
        self.es.close()

    def mm(self, out, lhsT, rhs, start=True, stop=True, R=(), W=()):
        return self.op("pe", lambda e: e.matmul(out, lhsT, rhs, start=start, stop=stop), R, W)

    def tr(self, out, in_, ident, R=(), W=()):
        return self.op("pe", lambda e: e.transpose(out, in_, ident), R, W)

    def act(self, out, in_, func, scale=1.0, bias=0.0, accum=None, R=(), W=()):
        if accum is None:
            return self.op("act", lambda e: e.activation(out, in_, func, scale=scale, bias=bias), R, W)
        return self.op("act", lambda e: e.activation(out, in_, func, scale=scale, bias=bias, accum_out=accum), R, W)

    def ts(self, eng, out, in0, s1, s2, op0, op1=None, accum=None, R=(), W=()):
        if op1 is None:
            op1 = ALU.bypass
        if accum is None:
            return self.op(eng, lambda e: e.tensor_scalar(out, in0, s1, s2, op0, op1), R, W)
        return self.op(eng, lambda e: e.tensor_scalar(out, in0, s1, s2, op0, op1, accum_out=accum), R, W)

    def tt(self, eng, out, in0, in1, op, R=(), W=()):
        return self.op(eng, lambda e: e.tensor_tensor(out, in0, in1, op), R, W)

    def stt(self, eng, out, in0, scalar, in1, op0, op1, R=(), W=()):
        return self.op(eng, lambda e: e.scalar_tensor_tensor(out, in0, scalar, in1, op0, op1), R, W)

    def cp(self, eng, out, in_, R=(), W=()):
        if eng == "act":
            return self.act(out, in_, AF.Copy, R=R, W=W)
        return self.op(eng, lambda e: e.tensor_copy(out, in_), R, W)

    def memset(self, eng, ap, val, W=()):
        return self.op(eng, lambda e: e.memset(ap, val), (), W)


IDX_HEADS, IDX_DIM = 16, 64
N_EXP, EXP_FF, N_GRP, EPG = 32, 256, 4, 8
ROPE_THETA = 10000.0
EPS = 1e-6


def derive(DM, S, DEPTH):
    G = DM // 4
    c = dict(DM=DM, S=S, KD=DM // 128, G=G, HA=G // 128, HB=G // 128, HC=G // 128, HD=G // 64,
             XBC=G + 512, KSEL=min(256, S // 4), NT=S // 128, DMIX=DM, KM=DM // 128)
    widths = (G, G, G, IDX_HEADS * IDX_DIM, IDX_DIM, IDX_HEADS,
              G, G, G, G // 128, G // 128, G,
              G // 2, G // 2, G, 16, G,
              G, G, 256, 256, G // 64)
    names = ("a_q", "a_k", "a_v", "a_qi", "a_ki", "a_wi", "b_q", "b_k", "b_v", "b_beta", "b_a", "b_z",
             "c_q", "c_k", "c_v", "c_gk", "c_g", "d_z", "d_x", "d_b", "d_c", "d_dt")
    off = {}
    o = 0
    for n, w in zip(names, widths):
        off[n] = (o, w)
        o += w
    c["off"] = off
    c["DP"] = o
    c["ALPHA"] = (2.0 * DEPTH) ** 0.25
    return c


WEIGHTS = ("ada_down", "ada_up", "ada_bias", "w_in", "w_out", "idx_kn_g", "idx_kn_b",
           "gdn_conv_w", "gdn_a_log", "gdn_dt_bias", "gdn_norm_w", "gla_w_up", "gla_b_up", "gla_norm_w",
           "ssd_conv_w", "ssd_conv_b", "ssd_a_log", "ssd_dt_bias", "ssd_d", "ssd_norm_w",
           "ln1_g", "ln1_b", "router_g_w", "router_g_b", "router_e_w", "router_e_b",
           "exp_w_gate", "exp_w_up", "exp_w_down", "ln2_g", "ln2_b")


def wshapes(c):
    DM, G = c["DM"], c["G"]
    return dict(ada_down=[DM, 256], ada_up=[256, 6 * DM], ada_bias=[6 * DM], w_in=[DM, c["DP"]], w_out=[DM, DM],
                idx_kn_g=[64], idx_kn_b=[64], gdn_conv_w=[4, 3 * G], gdn_a_log=[c["HB"]], gdn_dt_bias=[c["HB"]],
                gdn_norm_w=[128], gla_w_up=[16, G // 2], gla_b_up=[G // 2], gla_norm_w=[128],
                ssd_conv_w=[4, c["XBC"]], ssd_conv_b=[c["XBC"]], ssd_a_log=[c["HD"]], ssd_dt_bias=[c["HD"]],
                ssd_d=[c["HD"]], ssd_norm_w=[G], ln1_g=[DM], ln1_b=[DM], router_g_w=[DM, 4], router_g_b=[4],
                router_e_w=[DM, 32], router_e_b=[32], exp_w_gate=[32, DM, 256], exp_w_up=[32, DM, 256],
                exp_w_down=[32, 256, DM], ln2_g=[DM], ln2_b=[DM])


def host_consts():
    P = 128
    i = np.arange(P)
    U = (i[:, None] <= i[None, :]).astype(np.float32)
    cs = {"c_ident": np.eye(P, dtype=np.float32), "c_U": U, "c_NU": ((1.0 - U) * NEG).astype(np.float32),
          "c_ones": np.ones((P, P), np.float32), "c_NL": np.ascontiguousarray(((1.0 - U) * NEG).T.astype(np.float32))}
    LM = np.zeros((7, P, P), np.float32)
    for k in range(7):
        b = 1 << k
        r = (i // b)
        LM[k] = ((r[:, None] % 2 == 1) & (r[None, :] == r[:, None] - 1)).astype(np.float32)
    cs["c_LM"] = np.ascontiguousarray(LM.transpose(1, 0, 2))
    cs["c_LMT"] = np.ascontiguousarray(LM.transpose(2, 0, 1))
    sel = np.zeros((32, 32, P), np.float32)
    for e in range(32):
        sel[e, e, :] = 1.0
    cs["c_sel"] = np.ascontiguousarray(sel.transpose(1, 0, 2))
    return cs


def build(DM, S, NL, DEPTH, debug=False, STAGES=9, BIS=0, MIX="abcd"):
    c = derive(DM, S, DEPTH)
    KD, G, NT, DP = c["KD"], c["G"], c["NT"], c["DP"]
    HA, HB, HC, HD, XBC, KSEL = c["HA"], c["HB"], c["HC"], c["HD"], c["XBC"], c["KSEL"]
    off = c["off"]
    ALPHA = c["ALPHA"]
    nc = bass.Bass("TRN2", target_bir_lowering=False)
    P = Prog(nc)
    ES = P.es

    def din(name, shape, dt=F32):
        return T(name, nc.dram_tensor(name, list(shape), dt, kind="ExternalInput"))

    def dscr(name, shape, dt=F32, out=False):
        return T(name, nc.dram_tensor(name, list(shape), dt, kind="ExternalOutput" if out else "Internal"))

    x_in = din("x", [S, DM])
    c_in = din("c", [1, DM])
    pos_in = din("pos", [S, 1], I32)
    Wt = {n: din(n, [NL] + s) for n, s in wshapes(c).items()}
    CS = {n: din(n, list(a.shape)) for n, a in host_consts().items()}
    y_out = dscr("y", [S, DM], out=True)
    xa = dscr("xa", [S, DM]); xb = dscr("xb", [S, DM])
    proj = dscr("proj", [S, DP], out=debug)
    mixed = din("mixed_in", [S, DM]) if (debug and STAGES == 2) else dscr("mixed", [S, DM], out=debug)
    x1 = dscr("x1", [S, DM], out=debug)
    hg_d = dscr("hg", [S, N_EXP * EXP_FF]); hu_d = dscr("hu", [S, N_EXP * EXP_FF])
    modv = dscr("modv", [1, 6 * DM], out=debug)
    rope = dscr("rope", [S, 192], out=debug)

    ident = P.sb("ident", [128, 128]); Uc = P.sb("U", [128, 128]); NUc = P.sb("NU", [128, 128])
    ones = P.sb("ones", [128, 128])
    for tl, nm in ((ident, "c_ident"), (Uc, "c_U"), (NUc, "c_NU"), (ones, "c_ones")):
        P.dma("sp", tl[:], CS[nm][:, :], W=[tl])

    PS = [P.ps("ps%d" % i, [128, 512]) for i in range(8)]
    psrr = [0]

    psmod = [8]

    def nps():
        psrr[0] = (psrr[0] + 1) % psmod[0]
        return PS[psrr[0]]

    evrr = [0]

    def ev_eng():
        evrr[0] ^= 1
        return "act" if evrr[0] else "dve"

    from contextlib import contextmanager

    @contextmanager
    def scope():
        P.barrier()
        with ExitStack() as st:
            yield st
            P.barrier()

    castq = T("castq", None)
    castq_n = [0]

    def col_layout(st, src_row_ap, name):
        rows = P.sb(name + "r", [KD, 128], F32, st)
        P.dma("sp", rows[:], src_row_ap.rearrange("o (k p) -> (o k) p", p=128), W=[rows])
        pst = nps()
        P.tr(pst[:, 0:KD], rows[:], ident[0:KD, 0:KD], R=[rows, ident], W=[pst])
        tl = P.sb(name, [128, KD], F32, st)
        P.cp("dve", tl[:], pst[:, 0:KD], R=[pst], W=[tl])
        return tl

    def cast_weights(src_ap_fn, K, N, name):
        kd = K // 128
        NTL = 512 if kd <= 32 else 256
        NTL = min(NTL, N)
        nt = (N + NTL - 1) // NTL
        scr = dscr(name, [nt, 128, kd, NTL], BF16)
        for t in range(nt):
            w = min(NTL, N - t * NTL)
            src = src_ap_fn(t * NTL, w).rearrange("(kd p) n -> p kd n", p=128)
            for k0 in range(0, kd, 32):
                k1 = min(kd, k0 + 32)
                castq_n[0] += 1
                P.dma("pool", scr[t][:, k0:k1, 0:w], src[:, k0:k1, :], W=[(scr, t), (castq, castq_n[0] % 2)])
        return scr, NTL, nt

    def gemm(A, K, Wscr, NTL, nt, N, epilogue, prologue=None, name="g"):
        kd = K // 128
        TB = min(S, max(128, (32768 // kd) // 128 * 128))
        P.barrier()
        with scope() as st:
            AT = P.sb(name + "AT", [128, kd, TB], BF16, st)
            Wsb = [P.sb(name + "W%d" % i, [128, kd, NTL], BF16, st) for i in range(2)]
            xs = [P.sb(name + "xs%d" % i, [128, 2048], F32, st) for i in range(2)]
            k = 0
            for tb in range(0, S, TB):
                for tt_ in range(TB // 128):
                    t0 = tb + tt_ * 128
                    for c0 in range(0, K, 2048):
                        cw = min(2048, K - c0)
                        xt = xs[k % 2]; k += 1
                        P.dma("sp", xt[:, 0:cw], A[t0:t0 + 128, c0:c0 + cw], R=[A], W=[xt])
                        for j in range(cw // 128):
                            kk = (c0 // 128) + j
                            if j % 4 == 0:
                                pst = nps()
                            P.tr(pst[:, (j % 4) * 128:(j % 4 + 1) * 128], xt[:, j * 128:(j + 1) * 128], ident[:],
                                 R=[xt, ident], W=[(pst, j % 4)])
                            if prologue is None:
                                if j % 4 == 3:
                                    P.cp(ev_eng(), AT[:, kk - 3:kk + 1, tt_ * 128:(tt_ + 1) * 128],
                                         pst[:, :].rearrange("p (a b) -> p a b", a=4), R=[pst], W=[(AT, tt_)])
                            else:
                                prologue(kk, AT[:, kk, tt_ * 128:(tt_ + 1) * 128], pst[:, (j % 4) * 128:(j % 4 + 1) * 128],
                                         [(pst, j % 4)], [(AT, tt_)])
                for t in range(0 if BIS == 3 else (nt - 1 if BIS == 4 else nt)):
                    w = min(NTL, N - t * NTL)
                    ws = Wsb[t % 2]
                    P.dma("sp", ws[:], Wscr[t], R=[(Wscr, t)], W=[ws])
                    for tt_ in range(TB // 128):
                        pst = nps()
                        for kk in range(kd):
                            P.mm(pst[:, 0:NTL], AT[:, kk, tt_ * 128:(tt_ + 1) * 128], ws[:, kk, :], start=(kk == 0),
                                 stop=(kk == kd - 1), R=[(AT, tt_), ws], W=[pst])
                        epilogue(tb + tt_ * 128, t * NTL, w, pst, [pst])

    def store_epilogue(dst, st):
        bufs = [P.sb("ob%d" % i, [128, 512], F32, st) for i in range(3)]
        cnt = [0]

        def ep(t0, n0, w, pst, R):
            ob = bufs[cnt[0] % 3]; cnt[0] += 1
            P.cp(ev_eng(), ob[:, 0:w], pst[:, 0:w], R=R, W=[ob])
            P.dma("act", dst[t0:t0 + 128, n0:n0 + w], ob[:, 0:w], R=[ob], W=[dst])
        return ep

    def bcast_row(dst_tile, src_dram_row_ap, n, eng="sp"):
        P.dma(eng, dst_tile[:, 0:n], src_dram_row_ap.to_broadcast([128, n]), W=[dst_tile])

    def stage_mod(l):
        P.barrier()
        with scope() as st:
            cT = P.sb("cT", [128, KD, 2], F32, st)
            P.memset("dve", cT[:], 0.0, W=[cT])
            cc = col_layout(st, c_in.t.ap(), "ccol")
            P.cp("dve", cT[:, :, 0:1], cc[:].rearrange("p (k o) -> p k o", o=1), R=[cc], W=[cT])
            P.act(cT[:], cT[:], AF.Silu, R=[cT], W=[cT])
            ad = P.sb("ad", [128, KD, 256], F32, st)
            P.dma("sp", ad[:], Wt["ada_down"][l].rearrange("(k p) r -> p k r", p=128), W=[ad])
            vT = P.sb("vT", [128, 2], F32, st)
            for rh in range(2):
                pst = nps()
                for kk in range(KD):
                    P.mm(pst[:, 0:2], ad[:, kk, rh * 128:(rh + 1) * 128], cT[:, kk, :], start=(kk == 0), stop=(kk == KD - 1),
                         R=[ad, cT], W=[pst])
                P.cp("dve", vT[:, rh:rh + 1], pst[:, 0:1], R=[pst], W=[vT])
            au = [P.sb("au%d" % i, [128, 2, 512], F32, st) for i in range(2)]
            bb = [P.sb("bb%d" % i, [1, 512], F32, st) for i in range(2)]
            rw = [P.sb("rw%d" % i, [1, 512], F32, st) for i in range(2)]
            for mc in range(6 * DM // 512):
                a_, b_, r_ = au[mc % 2], bb[mc % 2], rw[mc % 2]
                P.dma("sp", a_[:], Wt["ada_up"][l][:, mc * 512:(mc + 1) * 512].rearrange("(r p) n -> p r n", p=128), W=[a_])
                P.dma("sp", b_[:], Wt["ada_bias"][l:l + 1, mc * 512:(mc + 1) * 512], W=[b_])
                pst = nps()
                for rh in range(2):
                    P.mm(pst[0:1, :], vT[:, rh:rh + 1], a_[:, rh, :], start=(rh == 0), stop=(rh == 1), R=[vT, a_], W=[pst])
                P.tt("dve", r_[:], pst[0:1, :], b_[:], ALU.add, R=[pst, b_], W=[r_])
                P.dma("act", modv[0:1, mc * 512:(mc + 1) * 512], r_[:], R=[r_], W=[modv])

    def mod_cols(st, part, plus_one):
        P.barrier()
        tl = col_layout(st, modv.t.ap()[0:1, part * DM:(part + 1) * DM], "mc%d" % part)
        if plus_one:
            P.ts("dve", tl[:], tl[:], 1.0, None, ALU.add, R=[tl], W=[tl])
        return tl

    def stage_proj(l, xin):
        with scope() as st:
            wscr, NTL, nt = cast_weights(lambda n0, w: Wt["w_in"][l][:, n0:n0 + w], DM, DP, "win_bf%d" % l)
            s1 = mod_cols(st, 1, True)
            sh1 = mod_cols(st, 0, False)

            def prologue(kk, out_ap, ps_ap, R, W):
                P.act(out_ap, ps_ap, AF.Identity, scale=s1[:, kk:kk + 1], bias=sh1[:, kk:kk + 1], R=R + [s1, sh1], W=W)
            if BIS != 1:
                gemm(xin, DM, wscr, NTL, nt, DP, store_epilogue(proj, st), None if BIS in (2, 3, 4) else prologue, name="pj")

    def conv_silu(t, c0, ncols, wbc, bias_bc, win, acc, tmp):
        for i in range(4):
            r0 = t * 128 - 3 + i
            wt = win[i % len(win)]
            if r0 < 0:
                P.memset("pool", wt[:, 0:ncols], 0.0, W=[wt])
                P.dma("sp", wt[-r0:128, 0:ncols], proj[0:128 + r0, c0:c0 + ncols], R=[proj], W=[wt])
            else:
                P.dma("sp", wt[:, 0:ncols], proj[r0:r0 + 128, c0:c0 + ncols], R=[proj], W=[wt])
            if i == 0:
                P.tt("pool", acc[:, 0:ncols], wt[:, 0:ncols], wbc[0][:, 0:ncols], ALU.mult, R=[wt, wbc[0]], W=[acc])
            else:
                P.tt("pool", tmp[:, 0:ncols], wt[:, 0:ncols], wbc[i][:, 0:ncols], ALU.mult, R=[wt, wbc[i]], W=[tmp])
                P.tt("dve", acc[:, 0:ncols], acc[:, 0:ncols], tmp[:, 0:ncols], ALU.add, R=[acc, tmp], W=[acc])
        if bias_bc is not None:
            P.tt("dve", acc[:, 0:ncols], acc[:, 0:ncols], bias_bc[:, 0:ncols], ALU.add, R=[acc, bias_bc], W=[acc])
        P.act(acc[:, 0:ncols], acc[:, 0:ncols], AF.Silu, R=[acc], W=[acc])

    def ldp(tile, t, name, eng="sp"):
        o, w = off[name]
        kw = {"allow_slow_non_contiguous": True} if w == 1 else {}
        P.dma(eng, tile[:, 0:w], proj[t * 128:(t + 1) * 128, o:o + w], R=[proj], W=[tile], **kw)

    def bc3(ap2, h, d):
        return ap2.rearrange("p (h o) -> p h o", o=1).to_broadcast([128, h, d])

    def v3(ap2, h):
        return ap2.rearrange("p (h d) -> p h d", h=h)

    def decayT(g, h0, nh, dst, ug, nug):
        pst = nps()
        for k in range(nh):
            h = h0 + k
            ug_ = ug[h % len(ug)]; nug_ = nug[h % len(nug)]
            P.ts("pool", ug_[:], Uc[:], g[:, h:h + 1], None, ALU.mult, R=[Uc, g], W=[ug_])
            P.ts("dve", nug_[:], Uc[:], g[:, h:h + 1], -1.0, ALU.mult, ALU.mult, R=[Uc, g], W=[nug_])
            sl = pst[:, k * 128:(k + 1) * 128]
            P.mm(sl, ones[:], ug_[:], start=True, stop=False, R=[ones, ug_], W=[pst])
            P.mm(sl, nug_[:], ones[:], start=False, stop=False, R=[nug_, ones], W=[pst])
            P.mm(sl, ident[:], NUc[:], start=False, stop=True, R=[ident, NUc], W=[pst])
        P.act(dst[:, 0:nh * 128], pst[:, 0:nh * 128], AF.Exp, R=[pst], W=[dst])

    def stage_ssd(l):
        HG = HD // 2
        with scope() as st:
            wbc = [P.sb("dw%d" % i, [128, XBC], F32, st) for i in range(4)]
            for i in range(4):
                bcast_row(wbc[i], Wt["ssd_conv_w"][l, i:i + 1, :], XBC)
            cb = P.sb("dcb", [128, XBC], F32, st); bcast_row(cb, Wt["ssd_conv_b"][l:l + 1, :], XBC)
            dtb = P.sb("ddtb", [128, HD], F32, st); bcast_row(dtb, Wt["ssd_dt_bias"][l:l + 1, :], HD)
            adec = P.sb("dadec", [128, HD], F32, st); bcast_row(adec, Wt["ssd_a_log"][l:l + 1, :], HD)
            P.act(adec[:], adec[:], AF.Exp, R=[adec], W=[adec])
            P.ts("dve", adec[:], adec[:], -1.0, None, ALU.mult, R=[adec], W=[adec])
            dsk = P.sb("ddsk", [128, HD], F32, st); bcast_row(dsk, Wt["ssd_d"][l:l + 1, :], HD)
            nw = P.sb("dnw", [128, G], F32, st); bcast_row(nw, Wt["ssd_norm_w"][l:l + 1, :], G)
            win = [P.sb("dwin%d" % i, [128, XBC], F32, st) for i in range(4)]
            acc = P.sb("dacc", [128, XBC], F32, st); tmp = P.sb("dtmp", [128, XBC], F32, st)
            zt = P.sb("dz", [128, G], F32, st); dtt = P.sb("ddt", [128, HD], F32, st)
            g = P.sb("dg", [128, HD], F32, st); gc = P.sb("dgc", [128, HD], F32, st); gcl = P.sb("dgcl", [128, HD], F32, st)
            eg = P.sb("deg", [128, HD], F32, st); egl = P.sb("degl", [128, HD], F32, st); gl = P.sb("dgl", [128, HD], F32, st)
            xdt = P.sb("dxdt", [128, G], F32, st)
            BCT = P.sb("dBCT", [128, 4, 128], F32, st)
            CBT = P.sb("dCBT", [128, 2, 128], F32, st)
            ug = [P.sb("dug", [128, 128], F32, st) for _ in range(2)]; nug = [P.sb("dnug", [128, 128], F32, st) for _ in range(2)]
            decs = [P.sb("ddec", [128, 512], F32, st) for _ in range(2)]
            ST = P.sb("dST", [128, HD, 128], F32, st)
            Bd = [P.sb("dBd%d" % i, [128, 128], F32, st) for i in range(2)]
            S_ = P.sb("dS", [128, G], F32, st)
            P.memset("dve", S_[:], 0.0, W=[S_])
            o_ = P.sb("do", [128, G], F32, st); t2 = P.sb("dt2", [128, G], F32, st)
            sm = P.sb("dsm", [128, 8], F32, st)
            for t in range(NT):
                conv_silu(t, off["d_x"][0], XBC, wbc, cb, win, acc, tmp)
                ldp(zt, t, "d_z"); ldp(dtt, t, "d_dt")
                P.tt("dve", dtt[:], dtt[:], dtb[:], ALU.add, R=[dtt, dtb], W=[dtt])
                P.act(dtt[:], dtt[:], AF.Exp, R=[dtt], W=[dtt])
                P.act(dtt[:], dtt[:], AF.Ln, bias=1.0, R=[dtt], W=[dtt])
                P.tt("dve", g[:], dtt[:], adec[:], ALU.mult, R=[dtt, adec], W=[g])
                P.tt("dve", v3(xdt[:], HD), v3(acc[:, 0:G], HD), bc3(dtt[:], HD, 64), ALU.mult, R=[acc, dtt], W=[xdt])
                pst = nps()
                P.mm(pst[:, 0:HD], Uc[:], g[:], R=[Uc, g], W=[pst])
                P.mm(pst[:, 64:64 + HD], ones[:], g[:], R=[ones, g], W=[pst])
                P.cp("dve", gc[:], pst[:, 0:HD], R=[pst], W=[gc])
                P.cp("dve", gcl[:], pst[:, 64:64 + HD], R=[pst], W=[gcl])
                P.act(eg[:], gc[:], AF.Exp, R=[gc], W=[eg])
                P.act(gl[:], gcl[:], AF.Exp, R=[gcl], W=[gl])
                P.tt("dve", egl[:], gcl[:], gc[:], ALU.subtract, R=[gcl, gc], W=[egl])
                P.act(egl[:], egl[:], AF.Exp, R=[egl], W=[egl])
                pst = nps()
                for k in range(4):
                    P.tr(pst[:, k * 128:(k + 1) * 128], acc[:, G + k * 128:G + (k + 1) * 128], ident[:], R=[acc, ident], W=[pst])
                P.cp("act", BCT[:], v3(pst[:, :], 4), R=[pst], W=[BCT])
                pst = nps()
                for gr in range(2):
                    P.mm(pst[:, gr * 128:(gr + 1) * 128], BCT[:, gr, :], BCT[:, 2 + gr, :], R=[BCT], W=[pst])
                P.cp("act", CBT[:], v3(pst[:, 0:256], 2), R=[pst], W=[CBT])
                for h0 in range(0, HD, 4):
                    nh = min(4, HD - h0)
                    dec = decs[(h0 // 4) % 2]
                    decayT(g, h0, nh, dec, ug, nug)
                    for k in range(nh):
                        h = h0 + k
                        P.tt("dve" if k % 2 else "pool", ST[:, h, :], dec[:, k * 128:(k + 1) * 128], CBT[:, h // HG, :], ALU.mult,
                             R=[dec, CBT], W=[(ST, h)])
                p_in = [nps() for _ in range((HD * 64 + 511) // 512)]
                for h in range(HD):
                    P.mm(p_in[(h * 64) // 512][:, (h * 64) % 512:(h * 64) % 512 + 64], ST[:, h, :], xdt[:, h * 64:(h + 1) * 64],
                         R=[(ST, h), xdt], W=[p_in[(h * 64) // 512]])
                p_x = [nps() for _ in range(2)]
                for gr in range(2):
                    P.mm(p_x[gr][:, 0:HG * 64], BCT[:, 2 + gr, :], S_[:, gr * HG * 64:(gr + 1) * HG * 64], R=[BCT, S_], W=[p_x[gr]])
                for gr in range(2):
                    cs = slice(gr * HG * 64, (gr + 1) * HG * 64)
                    P.tt("dve", v3(o_[:, cs], HG), v3(p_x[gr][:, 0:HG * 64], HG), bc3(eg[:, gr * HG:(gr + 1) * HG], HG, 64), ALU.mult,
                         R=[p_x[gr], eg], W=[o_])
                for bi, pb in enumerate(p_in):
                    w = min(512, HD * 64 - bi * 512)
                    P.tt("dve", o_[:, bi * 512:bi * 512 + w], o_[:, bi * 512:bi * 512 + w], pb[:, 0:w], ALU.add, R=[o_, pb], W=[o_])
                p_s = [nps() for _ in range((HD * 64 + 511) // 512)]
                for h in range(HD):
                    bd = Bd[h % 2]
                    P.ts("pool" if h % 2 else "dve", bd[:], acc[:, G + (h // HG) * 128:G + (h // HG + 1) * 128], egl[:, h:h + 1], None, ALU.mult,
                         R=[acc, egl], W=[bd])
                    P.mm(p_s[(h * 64) // 512][:, (h * 64) % 512:(h * 64) % 512 + 64], bd[:], xdt[:, h * 64:(h + 1) * 64],
                         R=[bd, xdt], W=[p_s[(h * 64) // 512]])
                P.tt("dve", v3(S_[:], HD), v3(S_[:], HD), bc3(gl[:], HD, 64), ALU.mult, R=[S_, gl], W=[S_])
                for bi, pb in enumerate(p_s):
                    w = min(512, HD * 64 - bi * 512)
                    P.tt("dve", S_[:, bi * 512:bi * 512 + w], S_[:, bi * 512:bi * 512 + w], pb[:, 0:w], ALU.add, R=[S_, pb], W=[S_])
                P.tt("pool", v3(t2[:], HD), v3(acc[:, 0:G], HD), bc3(dsk[:], HD, 64), ALU.mult, R=[acc, dsk], W=[t2])
                P.tt("dve", o_[:], o_[:], t2[:], ALU.add, R=[o_, t2], W=[o_])
                P.act(zt[:], zt[:], AF.Silu, R=[zt], W=[zt])
                P.tt("dve", o_[:], o_[:], zt[:], ALU.mult, R=[o_, zt], W=[o_])
                for gr in range(2):
                    cs = slice(gr * G // 2, (gr + 1) * G // 2)
                    P.act(t2[:, cs], o_[:, cs], AF.Square, accum=sm[:, gr:gr + 1], R=[o_], W=[t2, sm])
                P.ts("dve", sm[:, 2:4], sm[:, 0:2], 2.0 / G, EPS, ALU.mult, ALU.add, R=[sm], W=[sm])
                P.act(sm[:, 4:6], sm[:, 2:4], AF.Sqrt, R=[sm], W=[sm])
                P.op("dve", lambda e: e.reciprocal(sm[:, 6:8], sm[:, 4:6]), R=[sm], W=[sm])
                for gr in range(2):
                    cs = slice(gr * G // 2, (gr + 1) * G // 2)
                    P.ts("dve", o_[:, cs], o_[:, cs], sm[:, 6 + gr:7 + gr], None, ALU.mult, R=[o_, sm], W=[o_])
                P.tt("pool", o_[:], o_[:], nw[:], ALU.mult, R=[o_, nw], W=[o_])
                P.dma("act", mixed[t * 128:(t + 1) * 128, 3 * G:4 * G], o_[:], R=[o_], W=[(mixed, t, 3)])

    def stage_gla(l):
        G2 = G // 2
        with scope() as st:
            wup = P.sb("cwup", [16, G2], F32, st)
            P.dma("sp", wup[:], Wt["gla_w_up"][l], W=[wup])
            bup = P.sb("cbup", [128, G2], F32, st); bcast_row(bup, Wt["gla_b_up"][l:l + 1, :], G2)
            nw = P.sb("cnw", [128, 128], F32, st); bcast_row(nw, Wt["gla_norm_w"][l:l + 1, :], 128)
            q = P.sb("cq", [128, G2], F32, st); k = P.sb("ck", [128, G2], F32, st); v = P.sb("cv", [128, G], F32, st)
            gkr = P.sb("cgkr", [128, 16], F32, st); gz = P.sb("cgz", [128, G], F32, st)
            gkT = P.sb("cgkT", [16, 128], F32, st)
            gk = P.sb("cgk", [128, G2], F32, st); b = P.sb("cb", [128, G2], F32, st)
            e1 = P.sb("ce1", [128, G2], F32, st); e2 = P.sb("ce2", [128, G2], F32, st); e3 = P.sb("ce3", [128, G2], F32, st)
            qe = P.sb("cqe", [128, G2], F32, st); ke = P.sb("cke", [128, G2], F32, st); kd = P.sb("ckd", [128, G2], F32, st)
            qeT = P.sb("cqeT", [64, HC, 128], F32, st); keT = P.sb("ckeT", [64, HC, 128], F32, st)
            ebt = P.sb("cebt", [64, HC, 2], F32, st)
            one2 = P.sb("cone2", [128, 2], F32, st); P.memset("dve", one2[:], 1.0, W=[one2])
            aT = [P.sb("caT%d" % i, [128, 128], F32, st) for i in range(2)]
            S_ = P.sb("cS", [64, HC * 128], F32, st); P.memset("dve", S_[:], 0.0, W=[S_])
            o_ = P.sb("co", [128, G], F32, st); t2 = P.sb("ct2", [128, G], F32, st)
            sm = P.sb("csm", [128, 4 * HC], F32, st)
            for t in range(NT):
                ldp(q, t, "c_q"); ldp(k, t, "c_k"); ldp(v, t, "c_v"); ldp(gkr, t, "c_gk"); ldp(gz, t, "c_g")
                pst = nps()
                P.tr(pst[0:16, 0:128], gkr[:], ident[:], R=[gkr, ident], W=[pst])
                P.cp("act", gkT[:], pst[0:16, 0:128], R=[pst], W=[gkT])
                pst = nps()
                P.mm(pst[:, 0:G2], gkT[:], wup[:], R=[gkT, wup], W=[pst])
                P.tt("dve", gk[:], pst[:, 0:G2], bup[:], ALU.add, R=[pst, bup], W=[gk])
                P.act(gk[:], gk[:], AF.Exp, scale=-1.0, R=[gk], W=[gk])
                P.act(gk[:], gk[:], AF.Ln, bias=1.0, R=[gk], W=[gk])
                P.ts("dve", gk[:], gk[:], -1.0 / 16.0, None, ALU.mult, R=[gk], W=[gk])
                pst = nps(); pst2 = nps()
                P.mm(pst[:, 0:G2], Uc[:], gk[:], R=[Uc, gk], W=[pst])
                P.mm(pst2[:, 0:G2], ones[:], gk[:], R=[ones, gk], W=[pst2])
                P.cp("dve", b[:], pst[:, 0:G2], R=[pst], W=[b])
                P.act(e1[:], b[:], AF.Exp, R=[b], W=[e1])
                P.act(e2[:], b[:], AF.Exp, scale=-1.0, R=[b], W=[e2])
                P.tt("dve", e3[:], pst2[:, 0:G2], b[:], ALU.subtract, R=[pst2, b], W=[e3])
                P.act(e3[:], e3[:], AF.Exp, R=[e3], W=[e3])
                P.stt("dve", qe[:], q[:], 0.125, e1[:], ALU.mult, ALU.mult, R=[q, e1], W=[qe])
                P.tt("pool", ke[:], k[:], e2[:], ALU.mult, R=[k, e2], W=[ke])
                P.tt("pool", kd[:], k[:], e3[:], ALU.mult, R=[k, e3], W=[kd])
                for src, dstT in ((qe, qeT), (ke, keT)):
                    for h0 in range(0, HC, 4):
                        nh = min(4, HC - h0)
                        pst = nps()
                        for kk in range(nh):
                            P.tr(pst[0:64, kk * 128:(kk + 1) * 128], src[:, (h0 + kk) * 64:(h0 + kk + 1) * 64], ident[:], R=[src, ident], W=[pst])
                        P.cp("act", dstT[:, h0:h0 + nh, :], v3(pst[0:64, 0:nh * 128], nh), R=[pst], W=[dstT])
                for h in range(HC):
                    pst = nps()
                    P.mm(pst[0:64, 0:2], gk[:, h * 64:(h + 1) * 64], one2[:], R=[gk, one2], W=[pst])
                    P.act(ebt[:, h, :], pst[0:64, 0:2], AF.Exp, R=[pst], W=[ebt])
                for h0 in range(0, HC, 4):
                    nh = min(4, HC - h0)
                    p_o = nps()
                    for kk in range(nh):
                        h = h0 + kk
                        pst = nps()
                        P.mm(pst[:, 0:128], keT[:, h, :], qeT[:, h, :], R=[keT, qeT], W=[pst])
                        at = aT[h % 2]
                        P.tt("dve", at[:], pst[:, 0:128], Uc[:], ALU.mult, R=[pst, Uc], W=[at])
                        osl = p_o[:, kk * 128:(kk + 1) * 128]
                        P.mm(osl, at[:], v[:, h * 128:(h + 1) * 128], start=True, stop=False, R=[at, v], W=[p_o])
                        P.mm(osl, qeT[:, h, :], S_[:, h * 128:(h + 1) * 128], start=False, stop=True, R=[qeT, S_], W=[p_o])
                    P.cp("act", o_[:, h0 * 128:(h0 + nh) * 128], p_o[:, 0:nh * 128], R=[p_o], W=[o_])
                    for kk in range(nh):
                        h = h0 + kk
                        pst = nps()
                        P.mm(pst[0:64, 0:128], kd[:, h * 64:(h + 1) * 64], v[:, h * 128:(h + 1) * 128], R=[kd, v], W=[pst])
                        P.stt("dve", S_[:, h * 128:(h + 1) * 128], S_[:, h * 128:(h + 1) * 128], ebt[:, h, 0:1], pst[0:64, 0:128],
                              ALU.mult, ALU.add, R=[S_, ebt, pst], W=[S_])
                P.tt("pool", t2[:], o_[:], o_[:], ALU.mult, R=[o_], W=[t2])
                P.op("dve", lambda e: e.reduce_sum(sm[:, 0:HC], v3(t2[:], HC), AX.X), R=[t2], W=[sm])
                P.ts("dve", sm[:, HC:2 * HC], sm[:, 0:HC], 1.0 / 128.0, EPS, ALU.mult, ALU.add, R=[sm], W=[sm])
                P.act(sm[:, 2 * HC:3 * HC], sm[:, HC:2 * HC], AF.Sqrt, R=[sm], W=[sm])
                P.op("dve", lambda e: e.reciprocal(sm[:, 3 * HC:4 * HC], sm[:, 2 * HC:3 * HC]), R=[sm], W=[sm])
                P.tt("dve", v3(o_[:], HC), v3(o_[:], HC), bc3(sm[:, 3 * HC:4 * HC], HC, 128), ALU.mult, R=[o_, sm], W=[o_])
                P.tt("pool", v3(o_[:], HC), v3(o_[:], HC), nw[:].rearrange("p (o d) -> p o d", o=1).to_broadcast([128, HC, 128]), ALU.mult,
                     R=[o_, nw], W=[o_])
                P.act(gz[:], gz[:], AF.Silu, R=[gz], W=[gz])
                P.tt("dve", o_[:], o_[:], gz[:], ALU.mult, R=[o_, gz], W=[o_])
                P.dma("act", mixed[t * 128:(t + 1) * 128, 2 * G:3 * G], o_[:], R=[o_], W=[(mixed, t, 2)])

    def stage_gdn(l):
        with scope() as st:
            CW = 3 * G
            wbc = [P.sb("bw%d" % i, [128, CW], F32, st) for i in range(4)]
            for i in range(4):
                bcast_row(wbc[i], Wt["gdn_conv_w"][l, i:i + 1, :], CW)
            dtb = P.sb("bdtb", [128, HB], F32, st); bcast_row(dtb, Wt["gdn_dt_bias"][l:l + 1, :], HB)
            adec = P.sb("badec", [128, HB], F32, st); bcast_row(adec, Wt["gdn_a_log"][l:l + 1, :], HB)
            P.act(adec[:], adec[:], AF.Exp, R=[adec], W=[adec])
            P.ts("dve", adec[:], adec[:], -1.0, None, ALU.mult, R=[adec], W=[adec])
            nw = P.sb("bnw", [128, 128], F32, st); bcast_row(nw, Wt["gdn_norm_w"][l:l + 1, :], 128)
            LM = P.sb("bLM", [128, 7, 128], F32, st); LMT = P.sb("bLMT", [128, 7, 128], F32, st)
            P.dma("sp", LM[:], CS["c_LM"][:, :, :], W=[LM]); P.dma("sp", LMT[:], CS["c_LMT"][:, :, :], W=[LMT])
            win = [P.sb("bwin%d" % i, [128, CW], F32, st) for i in range(2)]
            acc = P.sb("bacc", [128, CW], F32, st); tmp = P.sb("btmp", [128, CW], F32, st)
            zt = P.sb("bz", [128, G], F32, st); beta = P.sb("bbeta", [128, HB], F32, st); ba = P.sb("bba", [128, HB], F32, st)
            g = P.sb("bg", [128, HB], F32, st); gc = P.sb("bgc", [128, HB], F32, st); gcl = P.sb("bgcl", [128, HB], F32, st)
            eg = P.sb("beg", [128, HB], F32, st); egl = P.sb("begl", [128, HB], F32, st); gl = P.sb("bgl", [128, HB], F32, st)
            rn = P.sb("brn", [128, 4 * 2 * HB], F32, st)
            qn = P.sb("bqn", [128, G], F32, st); kn = P.sb("bkn", [128, G], F32, st)
            kb = P.sb("bkb", [128, G], F32, st); vb = P.sb("bvb", [128, G], F32, st)
            qg = P.sb("bqg", [128, G], F32, st); kbg = P.sb("bkbg", [128, G], F32, st); kdd = P.sb("bkdd", [128, G], F32, st)
            ug = [P.sb("bug", [128, 128], F32, st) for _ in range(2)]; nug = [P.sb("bnug", [128, 128], F32, st) for _ in range(2)]
            dec = P.sb("bdec", [128, 512], F32, st)
            HBUF = []
            for i_ in range(min(4, HB)):
                HBUF.append((P.sb("bTT", [128, 4, 128], F32, st), P.sb("bAQ", [128, 2, 128], F32, st), P.sb("bA", [128, 128], F32, st),
                             P.sb("bLA", [128, 7, 128], F32, st), P.sb("bLAT", [128, 7, 128], F32, st),
                             P.sb("bDE", [128, 2, 128], F32, st), P.sb("bX", [128, 2, 128], F32, st),
                             P.sb("bu0", [128, 128], F32, st), P.sb("bwT", [128, 128], F32, st), P.sb("bu", [128, 128], F32, st)))
            S_ = P.sb("bS", [128, HB * 128], F32, st); P.memset("dve", S_[:], 0.0, W=[S_])
            o_ = P.sb("bo", [128, G], F32, st); t2 = tmp
            sm = P.sb("bsm", [128, 4 * HB], F32, st)
            for t in range(NT):
                conv_silu(t, off["b_q"][0], CW, wbc, None, win, acc, tmp)
                ldp(zt, t, "b_z"); ldp(beta, t, "b_beta"); ldp(ba, t, "b_a")
                P.act(beta[:], beta[:], AF.Sigmoid, R=[beta], W=[beta])
                P.tt("dve", ba[:], ba[:], dtb[:], ALU.add, R=[ba, dtb], W=[ba])
                P.act(ba[:], ba[:], AF.Exp, R=[ba], W=[ba])
                P.act(ba[:], ba[:], AF.Ln, bias=1.0, R=[ba], W=[ba])
                P.tt("dve", g[:], ba[:], adec[:], ALU.mult, R=[ba, adec], W=[g])
                P.tt("pool", tmp[:, 0:2 * G], acc[:, 0:2 * G], acc[:, 0:2 * G], ALU.mult, R=[acc], W=[tmp])
                P.op("dve", lambda e: e.reduce_sum(rn[:, 0:2 * HB], v3(tmp[:, 0:2 * G], 2 * HB), AX.X), R=[tmp], W=[rn])
                P.ts("dve", rn[:, 2 * HB:4 * HB], rn[:, 0:2 * HB], EPS, None, ALU.add, R=[rn], W=[rn])
                P.act(rn[:, 4 * HB:6 * HB], rn[:, 2 * HB:4 * HB], AF.Sqrt, R=[rn], W=[rn])
                P.op("dve", lambda e: e.reciprocal(rn[:, 6 * HB:8 * HB], rn[:, 4 * HB:6 * HB]), R=[rn], W=[rn])
                P.ts("dve", rn[:, 6 * HB:7 * HB], rn[:, 6 * HB:7 * HB], 128.0 ** -0.5, None, ALU.mult, R=[rn], W=[rn])
                P.tt("dve", v3(qn[:], HB), v3(acc[:, 0:G], HB), bc3(rn[:, 6 * HB:7 * HB], HB, 128), ALU.mult, R=[acc, rn], W=[qn])
                P.tt("dve", v3(kn[:], HB), v3(acc[:, G:2 * G], HB), bc3(rn[:, 7 * HB:8 * HB], HB, 128), ALU.mult, R=[acc, rn], W=[kn])
                P.tt("pool", v3(kb[:], HB), v3(kn[:], HB), bc3(beta[:], HB, 128), ALU.mult, R=[kn, beta], W=[kb])
                P.tt("pool", v3(vb[:], HB), v3(acc[:, 2 * G:3 * G], HB), bc3(beta[:], HB, 128), ALU.mult, R=[acc, beta], W=[vb])
                pst = nps()
                P.mm(pst[:, 0:HB], Uc[:], g[:], R=[Uc, g], W=[pst])
                P.mm(pst[:, 64:64 + HB], ones[:], g[:], R=[ones, g], W=[pst])
                P.cp("dve", gc[:], pst[:, 0:HB], R=[pst], W=[gc])
                P.cp("dve", gcl[:], pst[:, 64:64 + HB], R=[pst], W=[gcl])
                P.act(eg[:], gc[:], AF.Exp, R=[gc], W=[eg])
                P.act(gl[:], gcl[:], AF.Exp, R=[gcl], W=[gl])
                P.tt("dve", egl[:], gcl[:], gc[:], ALU.subtract, R=[gcl, gc], W=[egl])
                P.act(egl[:], egl[:], AF.Exp, R=[egl], W=[egl])
                P.tt("dve", v3(qg[:], HB), v3(qn[:], HB), bc3(eg[:], HB, 128), ALU.mult, R=[qn, eg], W=[qg])
                P.tt("pool", v3(kbg[:], HB), v3(kb[:], HB), bc3(eg[:], HB, 128), ALU.mult, R=[kb, eg], W=[kbg])
                P.tt("pool", v3(kdd[:], HB), v3(kn[:], HB), bc3(egl[:], HB, 128), ALU.mult, R=[kn, egl], W=[kdd])
                for h0 in range(0, HB, 4):
                    nh = min(4, HB - h0)
                    decayT(g, h0, nh, dec, ug, nug)

                    def head_gen(h, kk, Bf):
                        TT, AQ, A_, LA, LAT, DE, X, u0, wT, u = Bf
                        hs = slice(h * 128, (h + 1) * 128)
                        pst = nps()
                        for i_, src in enumerate((kn, kb, qn, qg)):
                            P.tr(pst[:, i_ * 128:(i_ + 1) * 128], src[:, hs], ident[:], R=[src, ident], W=[pst])
                        P.cp("act", TT[:], v3(pst[:, :], 4), R=[pst], W=[TT])
                        yield
                        pst = nps()
                        P.mm(pst[:, 0:256], TT[:, 0, :], TT[:, 1:3, :], R=[TT], W=[pst])
                        P.tt("dve", AQ[:], v3(pst[:, 0:256], 2),
                             dec[:, kk * 128:(kk + 1) * 128].rearrange("p (o d) -> p o d", o=1).to_broadcast([128, 2, 128]), ALU.mult,
                             R=[pst, dec], W=[AQ])
                        yield
                        pst = nps()
                        P.tr(pst[:, 0:128], AQ[:, 0, :], ident[:], R=[AQ, ident], W=[pst])
                        P.cp("act", A_[:], pst[:, 0:128], R=[pst], W=[A_])
                        P.tt("dve", LAT[:], LMT[:], AQ[:, 0, :].rearrange("p (o d) -> p o d", o=1).to_broadcast([128, 7, 128]), ALU.mult,
                             R=[LMT, AQ], W=[LAT])
                        yield
                        P.tt("pool", LA[:], LM[:], A_[:].rearrange("p (o d) -> p o d", o=1).to_broadcast([128, 7, 128]), ALU.mult,
                             R=[LM, A_], W=[LA])
                        P.tt("dve", DE[:, 1, :], ident[:], LAT[:, 0, :], ALU.subtract, R=[ident, LAT], W=[DE])
                        yield
                        P.tt("dve", DE[:, 0, :], ident[:], LA[:, 0, :], ALU.subtract, R=[ident, LA], W=[DE])
                        yield
                        for lv in range(1, 7):
                            pst = nps()
                            P.mm(pst[:, 0:128], LAT[:, lv, :], DE[:, 0, :], R=[LAT, DE], W=[pst])
                            P.mm(pst[:, 128:256], LA[:, lv, :], DE[:, 1, :], R=[LA, DE], W=[pst])
                            P.cp("act", X[:], v3(pst[:, 0:256], 2), R=[pst], W=[X])
                            yield
                            pst = nps()
                            P.mm(pst[:, 0:128], DE[:, 1, :], X[:, 0, :], R=[DE, X], W=[pst])
                            P.mm(pst[:, 128:256], DE[:, 0, :], X[:, 1, :], R=[DE, X], W=[pst])
                            P.tt("dve", DE[:], DE[:], v3(pst[:, 0:256], 2), ALU.subtract, R=[DE, pst], W=[DE])
                            yield
                        pst = nps(); pst2 = nps()
                        P.mm(pst[:, 0:128], DE[:, 1, :], vb[:, hs], R=[DE, vb], W=[pst])
                        P.mm(pst2[:, 0:128], kbg[:, hs], DE[:, 1, :], R=[kbg, DE], W=[pst2])
                        P.cp("act", u0[:], pst[:, 0:128], R=[pst], W=[u0])
                        P.cp("dve", wT[:], pst2[:, 0:128], R=[pst2], W=[wT])
                        yield
                        pst = nps()
                        P.mm(pst[:, 0:128], wT[:], S_[:, hs], R=[wT, (S_, h)], W=[pst])
                        P.tt("dve", u[:], u0[:], pst[:, 0:128], ALU.subtract, R=[u0, pst], W=[u])
                        yield
                        pst = nps()
                        P.mm(pst[:, 0:128], TT[:, 3, :], S_[:, hs], start=True, stop=False, R=[TT, (S_, h)], W=[pst])
                        P.mm(pst[:, 0:128], AQ[:, 1, :], u[:], start=False, stop=True, R=[AQ, u], W=[pst])
                        P.cp("act", o_[:, hs], pst[:, 0:128], R=[pst], W=[(o_, h)])
                        pst = nps()
                        P.mm(pst[:, 0:128], kdd[:, hs], u[:], R=[kdd, u], W=[pst])
                        P.stt("dve", S_[:, hs], S_[:, hs], gl[:, h:h + 1], pst[:, 0:128], ALU.mult, ALU.add, R=[(S_, h), gl, pst], W=[(S_, h)])
                        yield

                    gens = [head_gen(h0 + kk, kk, HBUF[kk]) for kk in range(nh)]
                    while gens:
                        for g_ in list(gens):
                            try:
                                next(g_)
                            except StopIteration:
                                gens.remove(g_)
                P.tt("pool", t2[:, 0:G], o_[:], o_[:], ALU.mult, R=[o_], W=[t2])
                P.op("dve", lambda e: e.reduce_sum(sm[:, 0:HB], v3(t2[:, 0:G], HB), AX.X), R=[t2], W=[sm])
                P.ts("dve", sm[:, HB:2 * HB], sm[:, 0:HB], 1.0 / 128.0, EPS, ALU.mult, ALU.add, R=[sm], W=[sm])
                P.act(sm[:, 2 * HB:3 * HB], sm[:, HB:2 * HB], AF.Sqrt, R=[sm], W=[sm])
                P.op("dve", lambda e: e.reciprocal(sm[:, 3 * HB:4 * HB], sm[:, 2 * HB:3 * HB]), R=[sm], W=[sm])
                P.tt("dve", v3(o_[:], HB), v3(o_[:], HB), bc3(sm[:, 3 * HB:4 * HB], HB, 128), ALU.mult, R=[o_, sm], W=[o_])
                P.tt("pool", v3(o_[:], HB), v3(o_[:], HB), nw[:].rearrange("p (o d) -> p o d", o=1).to_broadcast([128, HB, 128]), ALU.mult,
                     R=[o_, nw], W=[o_])
                P.act(zt[:], zt[:], AF.Silu, R=[zt], W=[zt])
                P.tt("dve", o_[:], o_[:], zt[:], ALU.mult, R=[o_, zt], W=[o_])
                P.dma("act", mixed[t * 128:(t + 1) * 128, G:2 * G], o_[:], R=[o_], W=[(mixed, t, 1)])

    def stage_rope_once(l):
        if l != 0:
            return
        TWO_PI = 2.0 * math.pi
        with scope() as st:
            ii = P.sb("rii", [128, 96], I32, st)
            inv = P.sb("rinv", [128, 96], F32, st)
            P.op("pool", lambda e: e.iota(ii[:, 0:64], [[1, 64]], base=0, channel_multiplier=0), W=[ii])
            P.op("pool", lambda e: e.iota(ii[:, 64:96], [[1, 32]], base=0, channel_multiplier=0), W=[ii])
            P.cp("dve", inv[:], ii[:], R=[ii], W=[inv])
            P.act(inv[:, 0:64], inv[:, 0:64], AF.Exp, scale=-2.0 * math.log(ROPE_THETA) / 128.0, R=[inv], W=[inv])
            P.act(inv[:, 64:96], inv[:, 64:96], AF.Exp, scale=-2.0 * math.log(ROPE_THETA) / 64.0, R=[inv], W=[inv])
            pi_ = P.sb("rpi", [128, 1], I32, st); pf = P.sb("rpf", [128, 1], F32, st)
            y = P.sb("ry", [128, 96], F32, st); ki = P.sb("rki", [128, 96], I32, st); kf = P.sb("rkf", [128, 96], F32, st)
            m1 = P.sb("rm1", [128, 96], F32, st)
            tab = P.sb("rtab", [128, 192], F32, st)
            for t in range(NT):
                P.dma("sp", pi_[:], pos_in[t * 128:(t + 1) * 128, :], W=[pi_])
                P.cp("dve", pf[:], pi_[:], R=[pi_], W=[pf])
                for which, shift in ((0, 0.25), (1, 0.0)):
                    P.ts("dve", y[:], inv[:], pf[:, 0:1], 1.0 / TWO_PI, ALU.mult, ALU.mult, R=[inv, pf], W=[y])
                    if shift:
                        P.ts("dve", y[:], y[:], shift, None, ALU.add, R=[y], W=[y])
                    P.cp("dve", ki[:], y[:], R=[y], W=[ki])
                    P.cp("dve", kf[:], ki[:], R=[ki], W=[kf])
                    P.tt("dve", y[:], y[:], kf[:], ALU.subtract, R=[y, kf], W=[y])
                    P.ts("dve", m1[:], y[:], 0.5, None, ALU.is_gt, R=[y], W=[m1])
                    P.tt("dve", y[:], y[:], m1[:], ALU.subtract, R=[y, m1], W=[y])
                    P.ts("dve", m1[:], y[:], -0.5, None, ALU.is_lt, R=[y], W=[m1])
                    P.tt("dve", y[:], y[:], m1[:], ALU.add, R=[y, m1], W=[y])
                    P.act(tab[:, which * 64:which * 64 + 64], y[:, 0:64], AF.Sin, scale=TWO_PI, R=[y], W=[tab])
                    P.act(tab[:, 128 + which * 32:160 + which * 32], y[:, 64:96], AF.Sin, scale=TWO_PI, R=[y], W=[tab])
                P.dma("act", rope[t * 128:(t + 1) * 128, :], tab[:], R=[tab], W=[(rope, t)])

    def rope_apply(dst, src, cs, sn, H, half, t1, t2_):
        def bc(ap):
            return ap.rearrange("p (o d) -> p o d", o=1).to_broadcast([128, H, half])
        s4 = src.rearrange("p (h two d) -> p h two d", h=H, two=2)
        d4 = dst.rearrange("p (h two d) -> p h two d", h=H, two=2)
        a1 = t1.rearrange("p (h d) -> p h d", h=H); a2 = t2_.rearrange("p (h d) -> p h d", h=H)
        return s4, d4, a1, a2, bc(cs), bc(sn)

    def stage_attn(l):
        NIT = 26
        psmod[0] = 7
        p_o = PS[7]
        with scope() as st:
            kT = P.sb("akT", [128, HA, S], BF16, st)
            vA = P.sb("avA", [128, NT, HA, 128], BF16, st)
            kiT = P.sb("akiT", [64, S], BF16, st)
            NL = P.sb("aNL", [128, 128], F32, st); P.dma("sp", NL[:], CS["c_NL"][:, :], W=[NL])
            lng = P.sb("alng", [128, 64], F32, st); lnb = P.sb("alnb", [128, 64], F32, st)
            bcast_row(lng, Wt["idx_kn_g"][l:l + 1, :], 64); bcast_row(lnb, Wt["idx_kn_b"][l:l + 1, :], 64)
            rp = [P.sb("arp%d" % i, [128, 192], F32, st) for i in range(2)]
            xa_ = P.sb("axa", [128, G], F32, st); xr = P.sb("axr", [128, G], F32, st)
            t1 = P.sb("at1", [128, 512], F32, st); t2_ = P.sb("at2", [128, 512], F32, st)
            qi = P.sb("aqi", [128, 1024], F32, st)
            vt = qi
            kit = P.sb("akit", [128, 64], F32, st); kir = P.sb("akir", [128, 64], F32, st)
            sm = P.sb("asm", [128, 16], F32, st)

            def do_rope(dst, src, H, half, cs, sn, W_):
                s4, d4, a1, a2, cb, sb_ = rope_apply(dst, src, cs, sn, H, half, t1[:, 0:H * half], t2_[:, 0:H * half])
                P.tt("dve", a1, s4[:, :, 0, :], cb, ALU.mult, R=[W_[0], W_[2]], W=[t1])
                P.tt("pool", a2, s4[:, :, 1, :], sb_, ALU.mult, R=[W_[0], W_[2]], W=[t2_])
                P.tt("dve", d4[:, :, 0, :], a1, a2, ALU.subtract, R=[t1, t2_], W=[W_[1]])
                P.tt("dve", a1, s4[:, :, 1, :], cb, ALU.mult, R=[W_[0], W_[2]], W=[t1])
                P.tt("pool", a2, s4[:, :, 0, :], sb_, ALU.mult, R=[W_[0], W_[2]], W=[t2_])
                P.tt("dve", d4[:, :, 1, :], a1, a2, ALU.add, R=[t1, t2_], W=[W_[1]])

            for t in range(NT):
                r_ = rp[t % 2]
                P.dma("sp", r_[:], rope[t * 128:(t + 1) * 128, :], R=[(rope, t)], W=[r_])
                ldp(xa_, t, "a_k"); ldp(vt, t, "a_v"); ldp(kit, t, "a_ki")
                do_rope(xr[:], xa_[:], HA, 64, r_[:, 0:64], r_[:, 64:128], (xa_, xr, r_))
                for h0 in range(0, HA, 4):
                    nh = min(4, HA - h0)
                    pst = nps()
                    for kk in range(nh):
                        P.tr(pst[:, kk * 128:(kk + 1) * 128], xr[:, (h0 + kk) * 128:(h0 + kk + 1) * 128], ident[:], R=[xr, ident], W=[pst])
                    P.cp("act", kT[:, h0:h0 + nh, t * 128:(t + 1) * 128], v3(pst[:, 0:nh * 128], nh), R=[pst], W=[(kT, t)])
                P.cp("pool", vA[:, t, :, :], v3(vt[:, 0:G], HA), R=[vt], W=[(vA, t)])
                P.act(kir[:], kit[:], AF.Identity, accum=sm[:, 0:1], R=[kit], W=[kir, sm])
                P.act(kir[:], kit[:], AF.Square, accum=sm[:, 1:2], R=[kit], W=[kir, sm])
                P.ts("dve", sm[:, 2:3], sm[:, 0:1], 1.0 / 64.0, None, ALU.mult, R=[sm], W=[sm])
                P.tt("dve", sm[:, 3:4], sm[:, 2:3], sm[:, 2:3], ALU.mult, R=[sm], W=[sm])
                P.stt("dve", sm[:, 4:5], sm[:, 1:2], 1.0 / 64.0, sm[:, 3:4], ALU.mult, ALU.subtract, R=[sm], W=[sm])
                P.ts("dve", sm[:, 4:5], sm[:, 4:5], EPS, None, ALU.add, R=[sm], W=[sm])
                P.act(sm[:, 5:6], sm[:, 4:5], AF.Sqrt, R=[sm], W=[sm])
                P.op("dve", lambda e: e.reciprocal(sm[:, 6:7], sm[:, 5:6]), R=[sm], W=[sm])
                P.ts("dve", kit[:], kit[:], sm[:, 2:3], sm[:, 6:7], ALU.subtract, ALU.mult, R=[kit, sm], W=[kit])
                P.tt("dve", kit[:], kit[:], lng[:], ALU.mult, R=[kit, lng], W=[kit])
                P.tt("dve", kit[:], kit[:], lnb[:], ALU.add, R=[kit, lnb], W=[kit])
                do_rope(kir[:], kit[:], 1, 32, r_[:, 128:160], r_[:, 160:192], (kit, kir, r_))
                pst = nps()
                P.tr(pst[0:64, 0:128], kir[:], ident[:], R=[kir, ident], W=[pst])
                P.cp("act", kiT[:, t * 128:(t + 1) * 128], pst[0:64, 0:128], R=[pst], W=[(kiT, t)])
            if BIS == 6:
                NTQ = 0
            else:
                NTQ = NT
            qT = P.sb("aqT", [128, HA, 128], BF16, st)
            qir = P.sb("aqir", [128, 1024], F32, st)
            qiT = P.sb("aqiT", [64, 16, 128], BF16, st)
            wi = P.sb("awi", [128, 16], F32, st)
            acc = P.sb("aacc", [128, S], F32, st)
            acc2 = acc
            maskb = P.sb("amask", [128, S], BF16, st)
            rl = [P.sb("arl%d" % i, [128, 512], F32, st) for i in range(2)]
            bs = P.sb("abs", [128, 8], F32, st)
            mxcs = [P.sb("amxc", [128, 16], F32, st) for _ in range(2)]
            bsh = [P.sb("absh", [128, 4], F32, st) for _ in range(2)]
            pT = [P.sb("apT%d" % i, [128, 4, 128], BF16, st) for i in range(2)]
            o_ = xa_
            for qb in range(NTQ):
                Sk = (qb + 1) * 128
                r_ = rp[qb % 2]
                P.dma("sp", r_[:], rope[qb * 128:(qb + 1) * 128, :], R=[(rope, qb)], W=[r_])
                ldp(xa_, qb, "a_q"); ldp(qi, qb, "a_qi"); ldp(wi, qb, "a_wi")
                do_rope(xr[:], xa_[:], HA, 64, r_[:, 0:64], r_[:, 64:128], (xa_, xr, r_))
                for h0 in range(0, HA, 4):
                    nh = min(4, HA - h0)
                    pst = nps()
                    for kk in range(nh):
                        P.tr(pst[:, kk * 128:(kk + 1) * 128], xr[:, (h0 + kk) * 128:(h0 + kk + 1) * 128], ident[:], R=[xr, ident], W=[pst])
                    P.act(qT[:, h0:h0 + nh, :], v3(pst[:, 0:nh * 128], nh), AF.Copy, scale=128.0 ** -0.5, R=[pst], W=[qT])
                do_rope(qir[:], qi[:], 16, 32, r_[:, 128:160], r_[:, 160:192], (qi, qir, r_))
                for h0 in range(0, 16, 4):
                    pst = nps()
                    for kk in range(4):
                        P.tr(pst[0:64, kk * 128:(kk + 1) * 128], qir[:, (h0 + kk) * 64:(h0 + kk + 1) * 64], ident[:], R=[qir, ident], W=[pst])
                    P.cp("act", qiT[:, h0:h0 + 4, :], v3(pst[0:64, 0:512], 4), R=[pst], W=[qiT])
                P.ts("dve", wi[:], wi[:], 1.0 / 32.0, None, ALU.mult, R=[wi], W=[wi])
                k_ = 0
                for c0 in range(0, Sk, 512):
                    w = min(512, Sk - c0)
                    for hi in range(16):
                        pst = nps()
                        P.mm(pst[:, 0:w], qiT[:, hi, :], kiT[:, c0:c0 + w], R=[qiT, kiT], W=[pst])
                        r2 = rl[k_ % 2]; k_ += 1
                        P.act(r2[:, 0:w], pst[:, 0:w], AF.Relu, R=[pst], W=[r2])
                        if hi == 0:
                            P.ts("dve", acc[:, c0:c0 + w], r2[:, 0:w], wi[:, 0:1], None, ALU.mult, R=[r2, wi], W=[acc])
                        else:
                            P.stt("dve", acc[:, c0:c0 + w], r2[:, 0:w], wi[:, hi:hi + 1], acc[:, c0:c0 + w], ALU.mult, ALU.add,
                                  R=[r2, wi, acc], W=[acc])
                if BIS == 7:
                    continue
                P.op("dve", lambda e, Sk=Sk: e.reduce_max(bs[:, 0:1], acc[:, 0:Sk], AX.X), R=[acc], W=[bs])
                P.op("dve", lambda e, Sk=Sk: e.tensor_reduce(bs[:, 1:2], acc[:, 0:Sk], AX.X, ALU.min), R=[acc], W=[bs])
                P.tt("dve", acc[:, Sk - 128:Sk], acc[:, Sk - 128:Sk], NL[:], ALU.add, R=[acc, NL], W=[acc])
                P.tt("dve", bs[:, 2:3], bs[:, 0:1], bs[:, 1:2], ALU.subtract, R=[bs], W=[bs])
                P.ts("dve", bs[:, 2:3], bs[:, 2:3], 1.0001, 1e-6, ALU.mult, ALU.add, R=[bs], W=[bs])
                P.ts("dve", bs[:, 3:4], bs[:, 1:2], -1e-6, None, ALU.add, R=[bs], W=[bs])
                for it in range(1, NIT + 1):
                    sc = 2.0 ** -it
                    P.stt("dve", bs[:, 4:5], bs[:, 2:3], sc, bs[:, 3:4], ALU.mult, ALU.add, R=[bs], W=[bs])
                    P.ts("dve", maskb[:, 0:Sk], acc[:, 0:Sk], bs[:, 4:5], None, ALU.is_ge, ALU.add, accum=bs[:, 5:6],
                         R=[acc, bs], W=[maskb, bs])
                    P.ts("dve", bs[:, 6:7], bs[:, 5:6], KSEL - 0.5, sc, ALU.is_ge, ALU.mult, R=[bs], W=[bs])
                    P.stt("dve", bs[:, 3:4], bs[:, 6:7], bs[:, 2:3], bs[:, 3:4], ALU.mult, ALU.add, R=[bs], W=[bs])
                P.ts("dve", maskb[:, 0:Sk], acc[:, 0:Sk], bs[:, 3:4], None, ALU.is_ge, R=[acc, bs], W=[maskb])
                if BIS == 8:
                    continue
                acc_idx = acc
                for h in range(HA):
                    acc = acc_idx if h % 2 == 0 else acc2
                    hb = bsh[h % 2]; mxc = mxcs[h % 2]
                    nch = 0
                    for c0 in range(0, Sk, 512):
                        w = min(512, Sk - c0)
                        pst = nps()
                        P.mm(pst[:, 0:w], qT[:, h, :], kT[:, h, c0:c0 + w], R=[qT, kT], W=[pst])
                        P.ts("dve", acc[:, c0:c0 + w], pst[:, 0:w], 1.0, None, ALU.mult, ALU.max, accum=mxc[:, nch:nch + 1],
                             R=[pst], W=[acc, mxc])
                        nch += 1
                    P.op("dve", lambda e, nch=nch, hb=hb, mxc=mxc: e.reduce_max(hb[:, 0:1], mxc[:, 0:nch], AX.X), R=[mxc], W=[hb])
                    P.ts("dve", hb[:, 0:1], hb[:, 0:1], -1.0, None, ALU.mult, R=[hb], W=[hb])
                    P.act(acc[:, 0:Sk], acc[:, 0:Sk], AF.Exp, bias=hb[:, 0:1], R=[acc, hb], W=[acc])
                    P.tt("pool", acc[:, 0:Sk], acc[:, 0:Sk], maskb[:, 0:Sk], ALU.mult, R=[acc, maskb], W=[acc])
                    P.op("dve", lambda e, Sk=Sk, acc=acc, hb=hb: e.reduce_sum(hb[:, 1:2], acc[:, 0:Sk], AX.X), R=[acc], W=[hb])
                    nj = Sk // 128
                    for j0 in range(0, nj, 4):
                        nn = min(4, nj - j0)
                        pst = nps()
                        for jj in range(nn):
                            P.tr(pst[:, jj * 128:(jj + 1) * 128], acc[:, (j0 + jj) * 128:(j0 + jj + 1) * 128], ident[:], R=[acc, ident], W=[pst])
                        pt = pT[(j0 // 4) % 2]
                        P.cp("act", pt[:, 0:nn, :], v3(pst[:, 0:nn * 128], nn), R=[pst], W=[pt])
                        for jj in range(nn):
                            j = j0 + jj
                            P.mm(p_o[:, 0:128], pt[:, jj, :], vA[:, j, h, :], start=(j == 0), stop=(j == nj - 1), R=[pt, vA], W=[p_o])
                    P.op("dve", lambda e, hb=hb: e.reciprocal(hb[:, 2:3], hb[:, 1:2]), R=[hb], W=[hb])
                    P.ts("dve", o_[:, h * 128:(h + 1) * 128], p_o[:, 0:128], hb[:, 2:3], None, ALU.mult, R=[p_o, hb], W=[(o_, h)])
                acc = acc_idx
                P.dma("act", mixed[qb * 128:(qb + 1) * 128, 0:G], o_[:], R=[o_], W=[(mixed, qb, 0)])
        psmod[0] = 8

    def ln_pass(src, dst, gname, bname, l, st, extra=None):
        P.barrier()
        gbc = P.sb("lng", [128, DM], F32, st); bbc = P.sb("lnb", [128, DM], F32, st)
        bcast_row(gbc, Wt[gname][l:l + 1, :], DM); bcast_row(bbc, Wt[bname][l:l + 1, :], DM)
        xt2 = [P.sb("lnx%d" % i, [128, DM], F32, st) for i in range(2)]
        junk = P.sb("lnj", [128, DM], F32, st)
        sm = P.sb("lnsm", [128, 8], F32, st)
        for t in range(NT):
            xt = xt2[t % 2]
            P.dma("sp", xt[:], src[t * 128:(t + 1) * 128, :], R=[(src, t)], W=[xt])
            P.act(junk[:], xt[:], AF.Identity, accum=sm[:, 0:1], R=[xt], W=[junk, (sm, 0)])
            P.act(junk[:], xt[:], AF.Square, accum=sm[:, 1:2], R=[xt], W=[junk, (sm, 1)])
            P.ts("dve", sm[:, 2:3], sm[:, 0:1], 1.0 / DM, None, ALU.mult, R=[(sm, 0)], W=[(sm, 2)])
            P.tt("dve", sm[:, 3:4], sm[:, 2:3], sm[:, 2:3], ALU.mult, R=[(sm, 2)], W=[(sm, 3)])
            P.stt("dve", sm[:, 4:5], sm[:, 1:2], 1.0 / DM, sm[:, 3:4], ALU.mult, ALU.subtract, R=[(sm, 1), (sm, 3)], W=[(sm, 4)])
            P.ts("dve", sm[:, 4:5], sm[:, 4:5], EPS, None, ALU.add, R=[(sm, 4)], W=[(sm, 4)])
            P.act(sm[:, 5:6], sm[:, 4:5], AF.Sqrt, R=[(sm, 4)], W=[(sm, 5)])
            P.op("dve", lambda e: e.reciprocal(sm[:, 6:7], sm[:, 5:6]), R=[(sm, 5)], W=[(sm, 6)])
            P.ts("dve", xt[:], xt[:], sm[:, 2:3], sm[:, 6:7], ALU.subtract, ALU.mult, R=[xt, (sm, 2), (sm, 6)], W=[xt])
            P.tt("pool", xt[:], xt[:], gbc[:], ALU.mult, R=[xt, gbc], W=[xt])
            P.tt("dve", xt[:], xt[:], bbc[:], ALU.add, R=[xt, bbc], W=[xt])
            P.dma("act", dst[t * 128:(t + 1) * 128, :], xt[:], R=[xt], W=[(dst, t)])
            if extra is not None:
                extra(t, xt)

    def resid_epilogue(resid, gate_part, dst, st):
        gbc = P.sb("gbc", [128, DM], F32, st)
        bcast_row(gbc, modv.t.ap()[0:1, gate_part * DM:(gate_part + 1) * DM], DM)
        P.ts("dve", gbc[:], gbc[:], 1.0, None, ALU.add, R=[gbc], W=[gbc])
        obs = [P.sb("rob%d" % i, [128, 512], F32, st) for i in range(2)]
        xts = [P.sb("rxt%d" % i, [128, 512], F32, st) for i in range(2)]
        cnt = [0]

        def ep(t0, n0, w, pst, R):
            ob = obs[cnt[0] % 2]; xt = xts[cnt[0] % 2]; cnt[0] += 1
            P.dma("sp", xt[:, 0:w], resid[t0:t0 + 128, n0:n0 + w], R=[(resid, t0 // 128)], W=[xt])
            P.tt("dve", ob[:, 0:w], pst[:, 0:w], gbc[:, n0:n0 + w], ALU.mult, R=R + [gbc], W=[ob])
            P.stt("dve", ob[:, 0:w], xt[:, 0:w], ALPHA, ob[:, 0:w], ALU.mult, ALU.add, R=[xt, ob], W=[ob])
            P.dma("act", dst[t0:t0 + 128, n0:n0 + w], ob[:, 0:w], R=[ob], W=[(dst, t0 // 128)])
        return ep

    def stage_out(l, xin, xo):
        NE = N_EXP * EXP_FF
        with scope() as st:
            wscr, NTL, nt = cast_weights(lambda n0, w: Wt["w_out"][l][:, n0:n0 + w], DM, DM, "wout_bf%d" % l)
            gemm(mixed, DM, wscr, NTL, nt, DM, resid_epilogue(xin, 2, x1, st), None, name="wo")
        gates_d = dscr("gates%d" % l, [S, 32])
        with scope() as st:
            s2 = mod_cols(st, 4, True); sh2 = mod_cols(st, 3, False)
            Wr = P.sb("Wr", [128, KD, 36], F32, st)
            P.dma("sp", Wr[:, :, 0:4], Wt["router_g_w"][l].rearrange("(k p) g -> p k g", p=128), W=[Wr])
            P.dma("sp", Wr[:, :, 4:36], Wt["router_e_w"][l].rearrange("(k p) g -> p k g", p=128), W=[Wr])
            rb = P.sb("rb", [128, 36], F32, st)
            P.dma("sp", rb[:, 0:4], Wt["router_g_b"][l:l + 1, :].to_broadcast([128, 4]), W=[rb])
            P.dma("sp", rb[:, 4:36], Wt["router_e_b"][l:l + 1, :].to_broadcast([128, 32]), W=[rb])
            hT = P.sb("rhT", [128, KD, 128], F32, st)
            lg = P.sb("lg", [128, 36], F32, st)
            w_ = P.sb("rw_", [128, 96], F32, st)
            gt = P.sb("gt", [128, 32], F32, st)

            def router(t, xt):
                for kk in range(KD):
                    if kk % 4 == 0:
                        pst = nps()
                    P.tr(pst[:, (kk % 4) * 128:(kk % 4 + 1) * 128], xt[:, kk * 128:(kk + 1) * 128], ident[:], R=[xt, ident], W=[pst])
                    P.act(hT[:, kk, :], pst[:, (kk % 4) * 128:(kk % 4 + 1) * 128], AF.Identity, scale=s2[:, kk:kk + 1],
                          bias=sh2[:, kk:kk + 1], R=[pst, s2, sh2], W=[hT])
                pst = nps()
                for kk in range(KD):
                    P.mm(pst[:, 0:36], hT[:, kk, :], Wr[:, kk, :], start=(kk == 0), stop=(kk == KD - 1), R=[hT, Wr], W=[pst])
                P.tt("dve", lg[:], pst[:, 0:36], rb[:], ALU.add, R=[pst, rb], W=[lg])
                P.op("dve", lambda e: e.reduce_max(w_[:, 0:1], lg[:, 0:4], AX.X), R=[lg], W=[w_])
                P.ts("dve", w_[:, 4:8], lg[:, 0:4], w_[:, 0:1], None, ALU.is_equal, R=[lg, w_], W=[w_])
                P.ts("dve", w_[:, 1:2], w_[:, 0:1], -1.0, None, ALU.mult, R=[w_], W=[w_])
                P.act(w_[:, 8:12], lg[:, 0:4], AF.Exp, bias=w_[:, 1:2], accum=w_[:, 2:3], R=[lg, w_], W=[w_])
                P.op("dve", lambda e: e.reciprocal(w_[:, 3:4], w_[:, 2:3]), R=[w_], W=[w_])
                P.ts("dve", w_[:, 12:16], w_[:, 4:8], -NEG, NEG, ALU.mult, ALU.add, R=[w_], W=[w_])
                P.tt("dve", w_[:, 16:48].rearrange("p (g e) -> p g e", g=4), lg[:, 4:36].rearrange("p (g e) -> p g e", g=4),
                     w_[:, 12:16].to_broadcast([128, 4, 8]) if False else w_[:, 12:16].rearrange("p (g o) -> p g o", o=1).to_broadcast([128, 4, 8]),
                     ALU.add, R=[lg, w_], W=[w_])
                P.op("dve", lambda e: e.reduce_max(w_[:, 48:49], w_[:, 16:48], AX.X), R=[w_], W=[w_])
                P.ts("dve", w_[:, 56:88], w_[:, 16:48], w_[:, 48:49], None, ALU.is_equal, R=[w_], W=[w_])
                P.stt("dve", gt[:], w_[:, 56:88], NEG, w_[:, 16:48], ALU.mult, ALU.add, R=[w_], W=[gt])
                P.op("dve", lambda e: e.reduce_max(w_[:, 49:50], gt[:], AX.X), R=[gt], W=[w_])
                P.ts("dve", gt[:], gt[:], w_[:, 49:50], None, ALU.is_equal, R=[gt, w_], W=[gt])
                P.tt("dve", w_[:, 50:51], w_[:, 48:49], w_[:, 49:50], ALU.subtract, R=[w_], W=[w_])
                P.act(w_[:, 51:52], w_[:, 50:51], AF.Sigmoid, R=[w_], W=[w_])
                P.ts("dve", w_[:, 52:53], w_[:, 51:52], -1.0, 1.0, ALU.mult, ALU.add, R=[w_], W=[w_])
                P.tt("dve", w_[:, 51:52], w_[:, 51:52], w_[:, 3:4], ALU.mult, R=[w_], W=[w_])
                P.tt("dve", w_[:, 52:53], w_[:, 52:53], w_[:, 3:4], ALU.mult, R=[w_], W=[w_])
                P.ts("dve", gt[:], gt[:], w_[:, 52:53], None, ALU.mult, R=[gt, w_], W=[gt])
                P.stt("dve", gt[:], w_[:, 56:88], w_[:, 51:52], gt[:], ALU.mult, ALU.add, R=[gt, w_], W=[gt])
                P.dma("act", gates_d[t * 128:(t + 1) * 128, :], gt[:], R=[gt], W=[(gates_d, t)])
            ln_pass(x1, x1, "ln1_g", "ln1_b", l, st, extra=router)
        for wn in ("exp_w_gate", "exp_w_up"):
            with scope() as st:
                scr = dscr("%s_bf%d" % (wn, l), [NE // 512, 128, KD, 512], BF16)
                for t in range(NE // 512):
                    for e_ in range(2):
                        castq_n[0] += 1
                        P.dma("pool", scr[t][:, :, e_ * 256:(e_ + 1) * 256],
                              Wt[wn][l, 2 * t + e_].rearrange("(kd p) f -> p kd f", p=128), W=[(scr, t), (castq, castq_n[0] % 2)])
                s2 = mod_cols(st, 4, True); sh2 = mod_cols(st, 3, False)

                def prologue(kk, out_ap, ps_ap, R, W, s2=s2, sh2=sh2):
                    P.act(out_ap, ps_ap, AF.Identity, scale=s2[:, kk:kk + 1], bias=sh2[:, kk:kk + 1], R=R + [s2, sh2], W=W)
                if wn == "exp_w_gate":
                    ep = store_epilogue(hg_d, st)
                else:
                    hgb = [P.sb("hgb%d" % i, [128, 512], F32, st) for i in range(2)]
                    obb = [P.sb("obb%d" % i, [128, 512], F32, st) for i in range(2)]
                    gtb = [P.sb("gtb%d" % i, [128, 32], F32, st) for i in range(2)]
                    cnt = [0]

                    def ep(t0, n0, w, pst, R, hgb=hgb, obb=obb, gtb=gtb, cnt=cnt):
                        hb_, ob_, gt_ = hgb[cnt[0] % 2], obb[cnt[0] % 2], gtb[cnt[0] % 2]; cnt[0] += 1
                        key = ("blk", t0 // 128, n0 // 512)
                        P.dma("sp", hb_[:, 0:w], hg_d[t0:t0 + 128, n0:n0 + w], R=[(hg_d, key)], W=[hb_])
                        P.dma("sp", gt_[:], gates_d[t0:t0 + 128, :], R=[(gates_d, t0 // 128)], W=[gt_])
                        P.act(hb_[:, 0:w], hb_[:, 0:w], AF.Silu, R=[hb_], W=[hb_])
                        P.tt("dve", ob_[:, 0:w], hb_[:, 0:w], pst[:, 0:w], ALU.mult, R=R + [hb_], W=[ob_])
                        for e_ in range(w // 256):
                            ee = n0 // 256 + e_
                            P.ts("pool", ob_[:, e_ * 256:(e_ + 1) * 256], ob_[:, e_ * 256:(e_ + 1) * 256], gt_[:, ee:ee + 1], None, ALU.mult,
                                 R=[ob_, gt_], W=[ob_])
                        P.dma("act", hg_d[t0:t0 + 128, n0:n0 + w], ob_[:, 0:w], R=[ob_], W=[(hg_d, key)])
                gemm(x1, DM, scr, 512, NE // 512, NE, ep, prologue, name="ex")
        with scope() as st:
            wscr, NTL, nt = cast_weights(lambda n0, w: Wt["exp_w_down"][l].rearrange("e f d -> (e f) d")[:, n0:n0 + w], NE, DM, "wdn_bf%d" % l)
            gemm(hg_d, NE, wscr, NTL, nt, DM, resid_epilogue(x1, 5, xo, st), None, name="dn")
        with scope() as st:
            ln_pass(xo, xo, "ln2_g", "ln2_b", l, st)

    xin = x_in
    final = []
    for l in range(NL):
        stage_mod(l)
        if STAGES <= 0:
            break
        stage_proj(l, xin)
        if STAGES <= 1:
            break
        if STAGES == 2:
            if not debug:
                with scope() as st:
                    zt = P.sb("zt", [128, DM], F32, st)
                    P.memset("dve", zt[:], 0.0, W=[zt])
                    for t in range(NT):
                        P.dma("sp", mixed[t * 128:(t + 1) * 128, :], zt[:], R=[zt], W=[(mixed, t)])
            xo = y_out
            stage_out(l, xin, xo)
            break
        if "a" in MIX:
            stage_rope_once(l)
            if BIS != 5:
                stage_attn(l)
        if "b" in MIX:
            stage_gdn(l)
        if "c" in MIX:
            stage_gla(l)
        if "d" in MIX:
            stage_ssd(l)
        xo = y_out if l == NL - 1 else (xa if l % 2 == 0 else xb)
        stage_out(l, xin, xo)
        xin = xo
    P.barrier()
    toks = []
    for tl in (y_out, proj, mixed, x1, modv):
        for s, stt_ in tl.st.items():
            if stt_[0] is not None:
                toks.append(stt_[0])
    P.finish(toks, "sp")
    P.emit()
    return nc, c


_CACHE = {}


def kernel(**inputs):
    DM, S, DEPTH, B = 4096, 4096, 4, 2
    if "nc" not in _CACHE:
        _CACHE["nc"] = build(DM, S, DEPTH, DEPTH, debug=False)[0]
    nc = _CACHE["nc"]
    consts = host_consts()
    x = np.asarray(inputs["x"], np.float32)
    c = np.asarray(inputs["c"], np.float32)
    pos = np.asarray(inputs["positions"]).astype(np.int32)
    maps = []
    for b in range(B):
        m = {"x": np.ascontiguousarray(x[b]), "c": np.ascontiguousarray(c[b:b + 1]),
             "pos": np.ascontiguousarray(pos[b].reshape(S, 1))}
        for n in WEIGHTS:
            m[n] = np.ascontiguousarray(np.asarray(inputs[n], np.float32))
        m.update(consts)
        maps.append(m)
    res = run_bass_kernel_spmd(nc, maps, core_ids=list(range(B)))
    return np.stack([np.asarray(res.results[b]["y"], np.float32) for b in range(B)], axis=0)
```
